# Optimizing a Trainium2 kernel written in Bass

```python
import jax
import jax.numpy as jnp
from jax import lax
import numpy as np

D_MODEL = 1024
BATCH = 8
SEQ = 4096
DEPTH = 4

GRID_W = 64
CTX_LEN = 256
HEAD_DIM = 64
NA_HEADS = 4
WIN_H = 8
WIN_W = 16
GQA_Q_HEADS = 8
GQA_KV_HEADS = 2
GQA_GROUP = GQA_Q_HEADS // GQA_KV_HEADS
MLA_HEADS = 4
MLA_Q_RANK = 256
MLA_KV_RANK = 128
MLA_NOPE_DIM = 64
MLA_ROPE_DIM = 32
MLA_V_DIM = 64
ROPE_THETA = 10000.0
Q_BLOCK = 128
N_BRANCHES = 3
FFN_DIM = 2816
N_EXPERTS = 8
TOP_K = 2
EXPERT_DIM = 2816
EXPERT_ROW_BLOCK = 256
NORM_EPS = 1e-6
N_DENSE = (DEPTH + 1) // 2
N_MOE = DEPTH // 2

NA_WIDTH = NA_HEADS * HEAD_DIM
GQA_WIDTH = GQA_Q_HEADS * HEAD_DIM
MLA_WIDTH = MLA_HEADS * MLA_V_DIM
KV_SIZES = (NA_WIDTH, NA_WIDTH, GQA_KV_HEADS * HEAD_DIM, GQA_KV_HEADS * HEAD_DIM, MLA_KV_RANK, MLA_ROPE_DIM)
Q_SIZES = (NA_WIDTH, GQA_WIDTH, MLA_Q_RANK)
KV_WIDTH = sum(KV_SIZES)
Q_WIDTH = sum(Q_SIZES)
GATE_WIDTH = N_BRANCHES * D_MODEL
IN_WIDTH = KV_WIDTH + Q_WIDTH + GATE_WIDTH
NA_SCALE = HEAD_DIM ** -0.5
GQA_SCALE = HEAD_DIM ** -0.5
MLA_SCALE = (MLA_NOPE_DIM + MLA_ROPE_DIM) ** -0.5

kernel_name = 'hybrid_na_gqa_mla_moe_diffusion_trunk'


def split_last(t, sizes):
    return jnp.split(t, [int(s) for s in np.cumsum(sizes)[:-1]], axis=-1)


def rms_norm(x, g):
    xf = x.astype(jnp.float32)
    y = xf * lax.rsqrt(jnp.mean(xf * xf, axis=-1, keepdims=True) + NORM_EPS)
    return (y * g.astype(jnp.float32)).astype(x.dtype)


def modulate(h, shift, scale):
    return h * (1 + scale) + shift


def rope_2d(x, rows, cols):
    half = x.shape[-1] // 2
    n_freq = half // 2
    inv = jnp.power(ROPE_THETA, -jnp.arange(n_freq, dtype=jnp.float32) / n_freq)
    ang = jnp.concatenate([rows[:, None] * inv, cols[:, None] * inv], axis=-1)
    cos = jnp.cos(ang)[None, :, None, :]
    sin = jnp.sin(ang)[None, :, None, :]
    xf = x.astype(jnp.float32)
    x1, x2 = xf[..., :half], xf[..., half:]
    return jnp.concatenate([x1 * cos - x2 * sin, x2 * cos + x1 * sin], axis=-1).astype(x.dtype)


def attend(q, k, v, scale):
    s = jnp.einsum('bqhgd,bkhd->bhgqk', q, k).astype(jnp.float32) * scale
    p = jax.nn.softmax(s, axis=-1).astype(v.dtype)
    return jnp.einsum('bhgqk,bkhd->bqhgd', p, v)


def blocked_attention(q, k_lat, v_lat, k_ctx, v_ctx, scale):
    B, S = q.shape[:2]
    k = jnp.concatenate([k_ctx, k_lat], axis=1)
    v = jnp.concatenate([v_ctx, v_lat], axis=1)
    qb = jnp.moveaxis(q.reshape(B, S // Q_BLOCK, Q_BLOCK, *q.shape[2:]), 1, 0)
    o = lax.map(lambda q_blk: attend(q_blk, k, v, scale), qb)
    return jnp.moveaxis(o, 0, 1).reshape(B, S, -1)


def neighbourhood_attention(q, k, v, k_ctx, v_ctx, rpb, n_rows):
    B, S, H, d = q.shape
    wh = min(WIN_H, n_rows)
    r = jnp.arange(n_rows)
    j = jnp.arange(GRID_W)
    row0 = jnp.clip(r - wh // 2, 0, n_rows - wh)
    key_rows = row0[:, None] + jnp.arange(wh)[None, :]
    col0 = jnp.clip(j - WIN_W // 2, 0, GRID_W - WIN_W)
    in_win = (j[None, :] >= col0[:, None]) & (j[None, :] < col0[:, None] + WIN_W)
    dr = key_rows - r[:, None]
    dc = jnp.clip(j[None, :] - j[:, None], -(WIN_W - 1), WIN_W - 1)
    bias = rpb[:, dr[:, :, None, None] + WIN_H - 1, dc[None, None] + WIN_W - 1]
    bias = bias.transpose(1, 0, 3, 2, 4).astype(jnp.float32)
    qg = q.reshape(B, n_rows, GRID_W, H, d)
    kg = k.reshape(B, n_rows, GRID_W, H, d)[:, key_rows]
    vg = v.reshape(B, n_rows, GRID_W, H, d)[:, key_rows]
    s_nb = jnp.einsum('brqhd,brikhd->brhqik', qg, kg).astype(jnp.float32) * NA_SCALE + bias[None]
    s_nb = jnp.where(in_win[:, None, :], s_nb, -jnp.inf)
    s_ctx = jnp.einsum('brqhd,bchd->brhqc', qg, k_ctx).astype(jnp.float32) * NA_SCALE
    n_nb = wh * GRID_W
    s = jnp.concatenate([s_nb.reshape(B, n_rows, H, GRID_W, n_nb), s_ctx], axis=-1)
    p = jax.nn.softmax(s, axis=-1).astype(v.dtype)
    p_nb = p[..., :n_nb].reshape(B, n_rows, H, GRID_W, wh, GRID_W)
    p_ctx = p[..., n_nb:]
    o = jnp.einsum('brhqik,brikhd->brqhd', p_nb, vg) + jnp.einsum('brhqc,bchd->brqhd', p_ctx, v_ctx)
    return o.reshape(B, S, H * d)


def mixer_kv(p_kv, k_norm, kv_lora_norm, w_ukv, rows, cols):
    B, L = p_kv.shape[:2]
    k_na, v_na, k_g, v_g, c_kv, k_rope = split_last(p_kv, KV_SIZES)
    k_na = k_na.reshape(B, L, NA_HEADS, HEAD_DIM)
    v_na = v_na.reshape(B, L, NA_HEADS, HEAD_DIM)
    k_g = rms_norm(k_g.reshape(B, L, GQA_KV_HEADS, HEAD_DIM), k_norm)
    v_g = v_g.reshape(B, L, GQA_KV_HEADS, HEAD_DIM)
    kv_up = (rms_norm(c_kv, kv_lora_norm) @ w_ukv).reshape(B, L, MLA_HEADS, MLA_NOPE_DIM + MLA_V_DIM)
    k_nope, v_m = kv_up[..., :MLA_NOPE_DIM], kv_up[..., MLA_NOPE_DIM:]
    k_rope = k_rope.reshape(B, L, 1, MLA_ROPE_DIM)
    if rows is not None:
        k_g = rope_2d(k_g, rows, cols)
        k_rope = rope_2d(k_rope, rows, cols)
    k_m = jnp.concatenate([k_nope, jnp.broadcast_to(k_rope, (B, L, MLA_HEADS, MLA_ROPE_DIM))], axis=-1)
    return k_na, v_na, k_g, v_g, k_m, v_m


def mixer_q(p_q, q_norm, q_lora_norm, w_uq, rows, cols):
    B, L = p_q.shape[:2]
    q_na, q_g, c_q = split_last(p_q, Q_SIZES)
    q_na = q_na.reshape(B, L, NA_HEADS, HEAD_DIM)
    q_g = rms_norm(q_g.reshape(B, L, GQA_Q_HEADS, HEAD_DIM), q_norm)
    q_up = (rms_norm(c_q, q_lora_norm) @ w_uq).reshape(B, L, MLA_HEADS, MLA_NOPE_DIM + MLA_ROPE_DIM)
    q_nope, q_rope = q_up[..., :MLA_NOPE_DIM], q_up[..., MLA_NOPE_DIM:]
    if rows is not None:
        q_g = rope_2d(q_g, rows, cols)
        q_rope = rope_2d(q_rope, rows, cols)
    q_g = q_g.reshape(B, L, GQA_KV_HEADS, GQA_GROUP, HEAD_DIM)
    q_m = jnp.concatenate([q_nope, q_rope], axis=-1)
    return q_na, q_g, q_m


def merge_branches(y_na, y_gqa, y_mla, p_gate, w_o_na, w_o_gqa, w_o_mla, w_out):
    g_na, g_gqa, g_mla = jnp.split(jax.nn.sigmoid(p_gate), N_BRANCHES, axis=-1)
    m = g_na * (y_na @ w_o_na) + g_gqa * (y_gqa @ w_o_gqa) + g_mla * (y_mla @ w_o_mla)
    return m @ w_out


def swiglu(h, w_gu, w_dn):
    gate, up = jnp.split(h @ w_gu, 2, axis=-1)
    return (jax.nn.silu(gate) * up) @ w_dn


def moe_swiglu(h, w_router, w_gu, w_dn):
    B, L, D = h.shape
    n = B * L
    n_assign = n * TOP_K
    hf = h.reshape(n, D)
    logits = (hf @ w_router).astype(jnp.float32)
    top_logit, top_e = lax.top_k(logits, TOP_K)
    top_w = jax.nn.softmax(top_logit, axis=-1)
    e_flat = top_e.reshape(-1)
    tok_flat = jnp.repeat(jnp.arange(n, dtype=jnp.int32), TOP_K)
    order = jnp.argsort(e_flat)
    e_sorted = e_flat[order]
    counts = jnp.bincount(e_flat, length=N_EXPERTS)
    padded = (counts + EXPERT_ROW_BLOCK - 1) // EXPERT_ROW_BLOCK * EXPERT_ROW_BLOCK
    pad_end = jnp.cumsum(padded)
    pad_start = pad_end - padded
    raw_start = jnp.cumsum(counts) - counts
    dest = pad_start[e_sorted] + jnp.arange(n_assign) - raw_start[e_sorted]
    n_blocks = -(-(n_assign + N_EXPERTS * (EXPERT_ROW_BLOCK - 1)) // EXPERT_ROW_BLOCK)
    n_rows = n_blocks * EXPERT_ROW_BLOCK
    src_tok = jnp.zeros((n_rows,), jnp.int32).at[dest].set(tok_flat[order])
    row_w = jnp.zeros((n_rows,), jnp.float32).at[dest].set(top_w.reshape(-1)[order])
    block_start = jnp.arange(n_blocks) * EXPERT_ROW_BLOCK
    block_expert = jnp.minimum(jnp.sum(block_start[:, None] >= pad_end[None, :], axis=-1), N_EXPERTS - 1)
    xb = hf[src_tok].reshape(n_blocks, EXPERT_ROW_BLOCK, D)
    yb = lax.map(lambda a: swiglu(a[0], w_gu[a[1]], w_dn[a[1]]), (xb, block_expert))
    yb = yb.reshape(n_rows, D) * row_w[:, None].astype(h.dtype)
    return jax.ops.segment_sum(yb, src_tok, num_segments=n).reshape(B, L, D)


def channel_mixer(t, layer, w_ffn_gu, w_ffn_dn, w_router, w_moe_gu, w_moe_dn):
    j = layer // 2
    if layer % 2 == 0:
        return swiglu(t, w_ffn_gu[j], w_ffn_dn[j])
    return moe_swiglu(t, w_router[j], w_moe_gu[j], w_moe_dn[j])


def setup_inputs(seed: int = 0) -> dict:
    key = jax.random.key(seed)
    ks = jax.random.split(key, 26)
    f32 = jnp.float32
    D = D_MODEL

    def nrm(k, shape, scale):
        return jax.random.normal(k, shape, f32) * scale

    def gain(k, shape):
        return 1.0 + 0.02 * jax.random.normal(k, shape, f32)

    return {
        'x': nrm(ks[0], (BATCH, SEQ, D), 1.0),
        'c': nrm(ks[1], (BATCH, D), 1.0),
        'ctx': nrm(ks[2], (BATCH, CTX_LEN, D), 1.0),
        'c_ctx': nrm(ks[3], (D,), 1.0),
        'w_ada': nrm(ks[4], (DEPTH, D, 6 * D), 0.5 * D ** -0.5),
        'b_ada': nrm(ks[5], (DEPTH, 6 * D), 0.02),
        'norm_mix': gain(ks[6], (DEPTH, D)),
        'norm_ffn': gain(ks[7], (DEPTH, D)),
        'w_in': nrm(ks[8], (DEPTH, D, IN_WIDTH), D ** -0.5),
        'q_norm_gqa': gain(ks[9], (DEPTH, HEAD_DIM)),
        'k_norm_gqa': gain(ks[10], (DEPTH, HEAD_DIM)),
        'q_lora_norm': gain(ks[11], (DEPTH, MLA_Q_RANK)),
        'kv_lora_norm': gain(ks[12], (DEPTH, MLA_KV_RANK)),
        'w_uq': nrm(ks[13], (DEPTH, MLA_Q_RANK, MLA_HEADS * (MLA_NOPE_DIM + MLA_ROPE_DIM)), MLA_Q_RANK ** -0.5),
        'w_ukv': nrm(ks[14], (DEPTH, MLA_KV_RANK, MLA_HEADS * (MLA_NOPE_DIM + MLA_V_DIM)), MLA_KV_RANK ** -0.5),
        'rpb': nrm(ks[15], (DEPTH, NA_HEADS, 2 * WIN_H - 1, 2 * WIN_W - 1), 0.1),
        'w_o_na': nrm(ks[16], (DEPTH, NA_WIDTH, D), NA_WIDTH ** -0.5),
        'w_o_gqa': nrm(ks[17], (DEPTH, GQA_WIDTH, D), GQA_WIDTH ** -0.5),
        'w_o_mla': nrm(ks[18], (DEPTH, MLA_WIDTH, D), MLA_WIDTH ** -0.5),
        'w_out': nrm(ks[19], (DEPTH, D, D), D ** -0.5),
        'w_ffn_gu': nrm(ks[20], (N_DENSE, D, 2 * FFN_DIM), D ** -0.5),
        'w_ffn_dn': nrm(ks[21], (N_DENSE, FFN_DIM, D), FFN_DIM ** -0.5),
        'w_router': nrm(ks[22], (N_MOE, D, N_EXPERTS), D ** -0.5),
        'w_moe_gu': nrm(ks[23], (N_MOE, N_EXPERTS, D, 2 * EXPERT_DIM), D ** -0.5),
        'w_moe_dn': nrm(ks[24], (N_MOE, N_EXPERTS, EXPERT_DIM, D), EXPERT_DIM ** -0.5),
        'norm_final': gain(ks[25], (D,)),
    }


def reference(x, c, ctx, c_ctx, w_ada, b_ada, norm_mix, norm_ffn, w_in, q_norm_gqa, k_norm_gqa,
              q_lora_norm, kv_lora_norm, w_uq, w_ukv, rpb, w_o_na, w_o_gqa, w_o_mla, w_out,
              w_ffn_gu, w_ffn_dn, w_router, w_moe_gu, w_moe_dn, norm_final):
    S = x.shape[1]
    C = ctx.shape[1]
    n_rows = S // GRID_W
    pos = jnp.arange(S)
    rows = (pos // GRID_W).astype(jnp.float32)
    cols = (pos % GRID_W).astype(jnp.float32)
    silu_c = jax.nn.silu(c)
    silu_cc = jax.nn.silu(c_ctx)
    for i in range(DEPTH):
        last = i == DEPTH - 1
        mod = (silu_c @ w_ada[i] + b_ada[i])[:, None, :]
        mod_c = silu_cc @ w_ada[i] + b_ada[i]
        sh1, sc1, g1, sh2, sc2, g2 = jnp.split(mod, 6, axis=-1)
        csh1, csc1, cg1, csh2, csc2, cg2 = jnp.split(mod_c, 6, axis=-1)

        h = modulate(rms_norm(x, norm_mix[i]), sh1, sc1)
        hc = modulate(rms_norm(ctx, norm_mix[i]), csh1, csc1)
        p_kv, p_q, p_gate = split_last(h @ w_in[i], (KV_WIDTH, Q_WIDTH, GATE_WIDTH))
        p_c = hc @ (w_in[i][:, :KV_WIDTH] if last else w_in[i])
        k_na, v_na, k_g, v_g, k_m, v_m = mixer_kv(p_kv, k_norm_gqa[i], kv_lora_norm[i], w_ukv[i], rows, cols)
        k_na_c, v_na_c, k_g_c, v_g_c, k_m_c, v_m_c = mixer_kv(
            p_c[..., :KV_WIDTH], k_norm_gqa[i], kv_lora_norm[i], w_ukv[i], None, None)
        q_na, q_g, q_m = mixer_q(p_q, q_norm_gqa[i], q_lora_norm[i], w_uq[i], rows, cols)
        y_na = neighbourhood_attention(q_na, k_na, v_na, k_na_c, v_na_c, rpb[i], n_rows)
        y_gqa = blocked_attention(q_g, k_g, v_g, k_g_c, v_g_c, GQA_SCALE)
        y_mla = blocked_attention(q_m[:, :, :, None], k_m, v_m, k_m_c, v_m_c, MLA_SCALE)
        x = x + g1 * merge_branches(y_na, y_gqa, y_mla, p_gate, w_o_na[i], w_o_gqa[i], w_o_mla[i], w_out[i])

        if not last:
            _, p_q_c, p_gate_c = split_last(p_c, (KV_WIDTH, Q_WIDTH, GATE_WIDTH))
            q_na_c, q_g_c, q_m_c = mixer_q(p_q_c, q_norm_gqa[i], q_lora_norm[i], w_uq[i], None, None)
            B = ctx.shape[0]
            yc_na = attend(q_na_c[:, :, :, None], k_na_c, v_na_c, NA_SCALE).reshape(B, C, NA_WIDTH)
            yc_gqa = attend(q_g_c, k_g_c, v_g_c, GQA_SCALE).reshape(B, C, GQA_WIDTH)
            yc_mla = attend(q_m_c[:, :, :, None], k_m_c, v_m_c, MLA_SCALE).reshape(B, C, MLA_WIDTH)
            ctx = ctx + cg1 * merge_branches(yc_na, yc_gqa, yc_mla, p_gate_c,
                                             w_o_na[i], w_o_gqa[i], w_o_mla[i], w_out[i])

        h2 = modulate(rms_norm(x, norm_ffn[i]), sh2, sc2)
        if last:
            x = x + g2 * channel_mixer(h2, i, w_ffn_gu, w_ffn_dn, w_router, w_moe_gu, w_moe_dn)
        else:
            hc2 = modulate(rms_norm(ctx, norm_ffn[i]), csh2, csc2)
            f = channel_mixer(jnp.concatenate([hc2, h2], axis=1), i, w_ffn_gu, w_ffn_dn,
                              w_router, w_moe_gu, w_moe_dn)
            ctx = ctx + cg2 * f[:, :C]
            x = x + g2 * f[:, C:]
    return rms_norm(x, norm_final)
```

```python
import contextlib
import os
import numpy as np
import ml_dtypes
import concourse.bass as bass
import concourse.mybir as mybir
from concourse.bass_utils import run_bass_kernel_spmd

F32, BF16 = mybir.dt.float32, mybir.dt.bfloat16
AF = mybir.ActivationFunctionType
ALU = mybir.AluOpType
AX = mybir.AxisListType
BF = ml_dtypes.bfloat16


class Cfg:
    def __init__(s, D=1024, C=256, S=4096, L=4, F=2816, E=8):
        s.D, s.C, s.S, s.L, s.F, s.E = D, C, S, L, F, E
        s.T = C + S
        s.KD = D // 128
        s.R = S // 64
        s.KF = F // 128
        s.chunks = [(0, C)] + [(C + i * 512, 512) for i in range(S // 512)]
        s.NKT = s.T // 128
        s.CT = C // 128
        s.ND = (L + 1) // 2
        s.NM = L // 2
        s.NT1 = 19 + 24


class Buf:
    __slots__ = ("name", "w", "r", "dsem")

    def __init__(s, name):
        s.name = name
        s.w = {}
        s.r = {}
        s.dsem = None


class Tl:
    def __init__(s, t, name):
        s.t = t
        s.b = Buf(name)


class KB:
    NDMASEM = 90

    def __init__(self, nc):
        self.nc = nc
        self.eng = {"pe": nc.tensor, "act": nc.scalar, "dve": nc.vector, "pool": nc.gpsimd, "sp": nc.sync}
        self.sems = []
        self.last = []
        self.semidx = {}
        for e in ("pe", "act", "dve", "pool"):
            self.semidx[e] = self._newsem("s_" + e)
        self.cnt = {e: 0 for e in self.semidx}
        self.waited = {e: {} for e in self.eng}
        self.dsems = []
        self.nd = 0
        self.stack = contextlib.ExitStack()
        self.ps = []
        for i in range(8):
            t = self.stack.enter_context(nc.psum_tensor("ps%d" % i, [128, 512], F32))
            self.ps.append(Tl(t, "ps%d" % i))
        self.psi = 0
        self.ntile = 0

    def _newsem(self, name):
        s = self.nc.semaphore(name).__enter__()
        self.sems.append(s)
        self.last.append(0)
        return len(self.sems) - 1

    def tile(self, st, shape, dt, name=None):
        self.ntile += 1
        name = (name or "t") + "_%d" % self.ntile
        t = st.enter_context(self.nc.sbuf_tensor(name, list(shape), dt))
        return Tl(t, name)

    def psum(self, pool=None):
        pool = pool or (0, 8)
        lo, n = pool
        key = ("psi", lo, n)
        i = getattr(self, "_rr", {}).get(key, 0)
        if not hasattr(self, "_rr"):
            self._rr = {}
        self._rr[key] = (i + 1) % n
        return self.ps[lo + i]

    def _wait(self, e, si, v):
        wd = self.waited[e]
        if e == "pe" and si == self.semidx["pe"]:
            return
        if wd.get(si, 0) < v:
            self.eng[e].wait_ge(self.sems[si], v)
            wd[si] = v

    def _deps(self, e, reads, writes):
        need = {}
        for b in reads:
            for si, v in b.w.items():
                if need.get(si, 0) < v:
                    need[si] = v
        for b in writes:
            for si, v in b.w.items():
                if need.get(si, 0) < v:
                    need[si] = v
            for si, v in b.r.items():
                if need.get(si, 0) < v:
                    need[si] = v
        for si, v in need.items():
            self._wait(e, si, v)

    def _mark(self, tok, reads, writes):
        si, v = tok
        for b in writes:
            b.w = {si: v}
            b.r = {}
        for b in reads:
            if any(b is w for w in writes):
                continue
            b.r[si] = v

    def op(self, e, fn, reads=(), writes=()):
        reads = [x.b if isinstance(x, Tl) else x for x in reads]
        writes = [x.b if isinstance(x, Tl) else x for x in writes]
        self._deps(e, reads, writes)
        ins = fn(self.eng[e])
        self.cnt[e] += 1
        si = self.semidx[e]
        ins.then_inc(self.sems[si], 1)
        self.last[si] = self.cnt[e]
        self._mark((si, self.cnt[e]), reads, writes)

    def dma(self, q, out, in_, reads, writes, sb):
        reads = [x.b if isinstance(x, Tl) else x for x in reads]
        writes = [x.b if isinstance(x, Tl) else x for x in writes]
        sb = sb.b if isinstance(sb, Tl) else sb
        if sb.dsem is None:
            if len(self.dsems) < self.NDMASEM:
                self.dsems.append(self._newsem("d%d" % len(self.dsems)))
                sb.dsem = self.dsems[-1]
            else:
                sb.dsem = self.dsems[self.nd % self.NDMASEM]
            self.nd += 1
        si = sb.dsem
        self._deps(q, reads, writes)
        if self.last[si] > 0:
            self._wait(q, si, self.last[si])
        ins = self.eng[q].dma_start(out=out, in_=in_)
        self.last[si] += 16
        ins.then_inc(self.sems[si], 16)
        self._mark((si, self.last[si]), reads, writes)

    def idma(self, out, out_off, in_, in_off, reads, writes, sb):
        reads = [x.b if isinstance(x, Tl) else x for x in reads]
        writes = [x.b if isinstance(x, Tl) else x for x in writes]
        sb = sb.b if isinstance(sb, Tl) else sb
        if sb.dsem is None:
            if len(self.dsems) < self.NDMASEM:
                self.dsems.append(self._newsem("d%d" % len(self.dsems)))
                sb.dsem = self.dsems[-1]
            else:
                sb.dsem = self.dsems[self.nd % self.NDMASEM]
            self.nd += 1
        si = sb.dsem
        self._deps("pool", reads, writes)
        if self.last[si] > 0:
            self._wait("pool", si, self.last[si])
        ins = self.eng["pool"].indirect_dma_start(out=out, out_offset=out_off, in_=in_, in_offset=in_off)
        self.last[si] += 16
        ins.then_inc(self.sems[si], 16)
        self._mark((si, self.last[si]), reads, writes)

    def barrier(self):
        for e in self.eng:
            for si, v in enumerate(self.last):
                if v > 0:
                    self._wait(e, si, v)


def mm(out, lhsT, rhs, start=True, stop=True):
    return lambda e: e.matmul(out, lhsT, rhs, start=start, stop=stop)


def build(cfg, debug=False, stop=None):
    nc = bass.Bass("TRN2", target_bir_lowering=False)
    D, C, S, L, F, E, T, KD, KF = cfg.D, cfg.C, cfg.S, cfg.L, cfg.F, cfg.E, cfg.T, cfg.KD, cfg.KF
    NKT, CT, R = cfg.NKT, cfg.CT, cfg.R

    def din(name, shape, dt=F32):
        return nc.dram_tensor(name, list(shape), dt, kind="ExternalInput").ap()

    def dscr(name, shape, dt=BF16):
        return nc.dram_tensor(name, list(shape), dt, kind=("ExternalOutput" if debug else "Internal")).ap()

    xT_in = din("xT", [D, T])
    NVL = 2 * KD + 48 + 6 + 3
    NV = L * NVL + 3 * KD
    vecs = din("vecs", [128, NV])
    w_ada = din("w_ada", [L, 48, 128, KD * 128])
    w1 = din("w1", [L, cfg.NT1, 128, KD * 128])
    wv = din("wv", [L, 128, KD * 384])
    wuq = din("wuq", [L, 128, 2 * 384])
    wuqr = din("wuqr", [L, 128, 2 * 384])
    wukvk = din("wukvk", [L, 128, 256])
    wukvv = din("wukvv", [L, 128, 256])
    rpbT = din("rpbT", [L, 64, 3840])
    wo = din("wo", [L, 128, 8 * D])
    wout = din("wout", [L, 128, KD * D])
    wgu = din("wgu", [max(cfg.ND, 1), KF, 128, KD * 256])
    wdn = din("wdn", [max(cfg.ND, 1), KD, 128, KF * 128])
    NMx = max(cfg.NM, 1)
    wgum = din("wgum", [NMx * E * KF * 128, KD * 256])
    wdnm = din("wdnm", [NMx * E * F, D])
    wr = din("wr", [max(cfg.NM, 1), 128, KD * 8])
    NCB = 128 + 128 + 64 + 128 + 128
    cbf = din("cbf", [128, NCB], BF16)
    ropet = din("ropet", [128, 4, T], BF16)
    negm = din("negm", [64, 3840], BF16)
    BS = 512
    NTHR = 10
    NBMAX = (2 * T + E * (BS - 1)) // BS
    NCF = 128 + 128 + E * 128 + 1 + NTHR + NBMAX + KF
    cf32 = din("cf32", [128, NCF])
    outT = nc.dram_tensor("outT", [D, S], F32, kind="ExternalOutput").ap()

    XT = nc.dram_tensor("XTs", [D, T], F32, kind="Internal").ap()
    KnaT = dscr("KnaT", [256, T])
    QnaT = dscr("QnaT", [256, T])
    KgT = dscr("KgT", [128, T])
    QgT = dscr("QgT", [512, T])
    KmT = dscr("KmT", [4 * 96, T])
    QmT = dscr("QmT", [4 * 96, T])
    Vna = dscr("Vna", [T, 4 * 65])
    Vg = dscr("Vg", [T, 2 * 65])
    Vm = dscr("Vm", [T, 4 * 65])
    GT = dscr("GT", [3 * D, T])
    YT = dscr("YT", [D, T])
    XsD = nc.dram_tensor("Xs", [NBMAX * BS, D], BF16, kind="Internal").ap()
    YsD = nc.dram_tensor("Ys", [NBMAX * BS, D], F32, kind="Internal").ap()

    kb = KB(nc)
    dXT = [Buf("XT%d" % i) for i in range(len(cfg.chunks))]
    dQK = {n: Buf(n) for n in ("KnaT", "QnaT", "KgT", "QgT", "KmT", "QmT", "Vna", "Vg", "Vm", "GT", "YT")}

    gst = contextlib.ExitStack()
    cb = kb.tile(gst, [128, NCB], BF16, "cbf")
    cf = kb.tile(gst, [128, NCF], F32, "cf32")
    vc = kb.tile(gst, [128, NV], F32, "vecs")
    modv = kb.tile(gst, [128, L * 48 * 2], F32, "modv")
    dummy = Buf("dram_in")
    kb.dma("sp", cb.t[:, :], cbf[:, :], [dummy], [cb], cb)
    kb.dma("sp", cf.t[:, :], cf32[:, :], [dummy], [cf], cf)
    kb.dma("sp", vc.t[:, :], vecs[:, :], [dummy], [vc], vc)
    o = 0
    ones128 = cb.t[:, o:o + 128]; o += 128
    bd64 = cb.t[:, o:o + 128]; o += 128
    id64b = cb.t[:, o:o + 64]; o += 64
    ustrict = cb.t[:, o:o + 128]; o += 128
    identb = cb.t[:, o:o + 128]; o += 128
    id128f = cf.t[:, 0:128]
    sel64 = cf.t[0:65, 128:256]
    o2 = 128 + 128 + E * 128
    epsc = cf.t[:, o2:o2 + 1]
    thr_c = cf.t[:, o2 + 1:o2 + 1 + NTHR]
    jrow_c = cf.t[:, o2 + 1 + NTHR:o2 + 1 + NTHR + NBMAX]
    cidx_c = cf.t[:, o2 + 1 + NTHR + NBMAX:o2 + 1 + NTHR + NBMAX + KF]

    def sel_e(e):
        return cf.t[0:8, 256 + e * 128:256 + (e + 1) * 128]

    def vcol(l, j, n=1):
        return vc.t[:, l * NVL + j:l * NVL + j + n]
    V_NMIX, V_NFFN, V_BADA, V_QG, V_QGR, V_KG, V_KGR, V_QL, V_KVL = 0, KD, 2 * KD, 2 * KD + 48, 2 * KD + 49, 2 * KD + 50, 2 * KD + 51, 2 * KD + 52, 2 * KD + 54
    gofs = L * NVL
    v_nfinal = vc.t[:, gofs:gofs + KD]
    v_c = vc.t[:, gofs + KD:gofs + 3 * KD]

    def mod(l, which, j, s):
        i = ((l * 48) + which * KD + j) * 2 + s
        return modv.t[:, i:i + 1]

    with contextlib.ExitStack() as st:
        sc = kb.tile(st, [128, 2 * KD], F32, "silu_c")
        sc2 = kb.tile(st, [128, KD, 2], F32, "silu_c2")
        kb.op("act", lambda e: e.activation(out=sc.t[:, :], in_=v_c, func=AF.Silu), [vc], [sc])
        for s_ in range(2):
            kb.op("dve", lambda e, s_=s_: e.tensor_copy(out=sc2.t[:, :, s_], in_=sc.t[:, s_ * KD:(s_ + 1) * KD]), [sc], [sc2])
        wst = [kb.tile(st, [128, KD * 128], F32, "wada") for _ in range(3)]
        i = 0
        for l in range(L):
            for nt in range(48):
                w_ = wst[i % 3]; i += 1
                kb.dma("sp", w_.t[:, :], w_ada[l, nt], [dummy], [w_], w_)
                p = kb.psum()
                for k in range(KD):
                    kb.op("pe", mm(p.t[:, 0:2], w_.t[:, k * 128:(k + 1) * 128], sc2.t[:, k, :], k == 0, k == KD - 1), [w_, sc2], [p])
                base = ((l * 48) + nt) * 2
                kb.op("dve", lambda e, p=p, base=base, l=l, nt=nt: e.tensor_scalar(out=modv.t[:, base:base + 2], in0=p.t[:, 0:2], scalar1=vcol(l, V_BADA + nt), scalar2=None, op0=ALU.add), [p, vc], [modv])
        kb.barrier()
    if stop == "P0":
        return nc

    def rstd_from(ps_ssq, n, dim, out_t):
        kb.op("act", lambda e: e.activation(out=out_t.t[:, 0:n], in_=ps_ssq.t[:, 0:n], func=AF.Ln, bias=epsc, scale=1.0 / dim), [ps_ssq, cf], [out_t])
        kb.op("act", lambda e: e.activation(out=out_t.t[:, 0:n], in_=out_t.t[:, 0:n], func=AF.Exp, scale=-0.5), [], [out_t])

    def norm_chunk(st_tiles, xsrc, l, ci, which_norm, which_sh, which_sc, out_bf, out_col0, out_f32=None):
        xt, sq, tmp, rs, gs = st_tiles
        t0, n = cfg.chunks[ci]
        s_ = 1 if ci == 0 else 0
        kb.dma("sp", xt.t[:, :, 0:n], xsrc[:, t0:t0 + n].rearrange("(k p) t -> p k t", p=128), [dXT[ci]], [xt], xt)
        for k in range(KD):
            kb.op("dve", lambda e, k=k: e.scalar_tensor_tensor(out=gs.t[:, k:k + 1], in0=mod(l, which_sc, k, s_), scalar=1.0, in1=vcol(l, which_norm + k), op0=ALU.add, op1=ALU.mult), [modv, vc], [gs])
        p = kb.psum()
        for k in range(KD):
            kb.op("act", lambda e, k=k: e.activation(out=sq.t[:, k, 0:n], in_=xt.t[:, k, 0:n], func=AF.Square), [xt], [sq])
            kb.op("pe", mm(p.t[:, 0:n], ones128, sq.t[:, k, 0:n], k == 0, k == KD - 1), [sq, cb], [p])
        rstd_from(p, n, D, rs)
        for k in range(KD):
            kb.op("dve", lambda e, k=k: e.tensor_tensor(out=tmp.t[:, 0:n], in0=xt.t[:, k, 0:n], in1=rs.t[:, 0:n], op=ALU.mult), [xt, rs], [tmp])
            if out_f32 is not None:
                kb.op("act", lambda e, k=k: e.activation(out=out_f32.t[:, k, 0:n], in_=tmp.t[:, 0:n], func=AF.Identity, bias=mod(l, which_sh, k, s_), scale=gs.t[:, k:k + 1]), [tmp, gs, modv], [out_f32])
                kb.op("pool", lambda e, k=k: e.tensor_copy(out=out_bf.t[:, k, out_col0:out_col0 + n], in_=out_f32.t[:, k, 0:n]), [out_f32], [out_bf])
            else:
                kb.op("act", lambda e, k=k: e.activation(out=out_bf.t[:, k, out_col0:out_col0 + n], in_=tmp.t[:, 0:n], func=AF.Identity, bias=mod(l, which_sh, k, s_), scale=gs.t[:, k:k + 1]), [tmp, gs, modv], [out_bf])

    def norm_tiles(st, wmax=512):
        return (kb.tile(st, [128, KD, wmax], F32, "xt"), kb.tile(st, [128, KD, wmax], BF16, "sq"),
                kb.tile(st, [128, wmax], F32, "tmp"), kb.tile(st, [128, wmax], F32, "rs"), kb.tile(st, [128, KD], F32, "gs"))

    def load_w_bf(stg, dst, src_ap, width, eng):
        kb.dma("sp", stg.t[:, 0:width], src_ap, [dummy], [stg], stg)
        return stg

    rr = {"ev": 0, "cast": 0}

    def cast(out_ap, out_buf, in_ap, in_buf):
        rr["cast"] ^= 1
        if rr["cast"]:
            kb.op("dve", lambda e: e.tensor_copy(out=out_ap, in_=in_ap), [in_buf], [out_buf])
        else:
            kb.op("act", lambda e: e.activation(out=out_ap, in_=in_ap, func=AF.Copy), [in_buf], [out_buf])

    def evac(out_ap, out_buf, ps_t, ps_ap, func=None):
        if func is not None:
            kb.op("act", lambda e: e.activation(out=out_ap, in_=ps_ap, func=func), [ps_t], [out_buf])
            return
        rr["ev"] ^= 1
        if rr["ev"]:
            kb.op("act", lambda e: e.activation(out=out_ap, in_=ps_ap, func=AF.Copy), [ps_t], [out_buf])
        else:
            kb.op("dve", lambda e: e.tensor_copy(out=out_ap, in_=ps_ap), [ps_t], [out_buf])

    dXs = Buf("Xs")
    dYs = Buf("Ys")
    WbfG = nc.dram_tensor("WbfG", [E * KF * 128, KD * 256], BF16, kind="Internal").ap()
    WbfD = nc.dram_tensor("WbfD", [E * F, D], BF16, kind="Internal").ap()
    dWbf = Buf("Wbf")

    class Conv:
        def __init__(self):
            self.jobs = []
            self.nl = 0
            self.ncs = 0
            self.cst = None

        def add_layer(self, m_):
            assert self.ncs == len(self.jobs)
            self.jobs = []
            self.nl = 0
            self.ncs = 0
            for e_ in range(E):
                for f in range(KF):
                    r0 = ((m_ * E + e_) * KF + f) * 128
                    d0 = (e_ * KF + f) * 128
                    self.jobs.append((wgum[r0:r0 + 128, :], WbfG[d0:d0 + 128, :], False))
                for k2 in range(KF // 2):
                    r0 = (m_ * E + e_) * F + k2 * 256
                    d0 = e_ * F + k2 * 256
                    self.jobs.append((wdnm[r0:r0 + 256, :].rearrange("(a p) d -> p a d", p=128), WbfD[d0:d0 + 256, :].rearrange("(a p) d -> p a d", p=128), True))

        def active(self):
            return self.ncs < len(self.jobs)

        def attach(self, st):
            self.cst = [kb.tile(st, [128, 2048], F32, "cvs") for _ in range(3)]
            self.cbt = [kb.tile(st, [128, 2048], BF16, "cvb") for _ in range(3)]

        def _load(self):
            i = self.nl
            src, dst, three = self.jobs[i]
            sg = self.cst[i % 3]
            o = sg.t[:, :].rearrange("p (a d) -> p a d", a=2) if three else sg.t[:, :]
            kb.dma("sp", o, src, [dummy], [sg], sg)
            self.nl += 1

        def _cast_store(self):
            i = self.ncs
            src, dst, three = self.jobs[i]
            sg = self.cst[i % 3]; cb_ = self.cbt[i % 3]
            kb.op("pool", lambda e: e.tensor_copy(out=cb_.t[:, :], in_=sg.t[:, :]), [sg], [cb_])
            i_ = cb_.t[:, :].rearrange("p (a d) -> p a d", a=2) if three else cb_.t[:, :]
            kb.dma("pool", dst, i_, [cb_], [dWbf], cb_)
            self.ncs += 1

        def step(self):
            if self.cst is None:
                return
            if self.ncs < self.nl:
                self._cast_store()
            if self.nl < len(self.jobs):
                self._load()

        def drain(self, everything=False):
            if self.cst is None:
                return
            while True:
                if self.ncs < self.nl:
                    self._cast_store()
                elif everything and self.nl < len(self.jobs):
                    self._load()
                else:
                    break
            self.cst = None

    conv = Conv()

    def routed_moe(l, last):
        m_ = l // 2
        clist = list(range(len(cfg.chunks)))
        if last:
            clist = clist[1:]
        tok0 = cfg.chunks[clist[0]][0]
        TL = sum(cfg.chunks[ci][1] for ci in clist)
        NTT = TL // 128
        NB = (2 * TL + E * (BS - 1)) // BS
        SUB = BS // 128
        groups = [clist[i:i + 2] for i in range(0, len(clist), 2)]
        with contextlib.ExitStack() as st:
            selA = kb.tile(st, [128, NTT, 8], F32, "selA")
            sel1A = kb.tile(st, [128, NTT, 8], F32, "sel1A")
            gwA = kb.tile(st, [128, NTT, 8], F32, "gwA")
            posI = kb.tile(st, [128, NTT * 2], mybir.dt.int32, "posI")
            wAB = kb.tile(st, [128, NTT * 2], F32, "wAB")
            gidxI = kb.tile(st, [128, NB * KF], mybir.dt.int32, "gidxI")
            didxI = kb.tile(st, [128, NB * KF], mybir.dt.int32, "didxI")
            with contextlib.ExitStack() as st1:
                h2tm = kb.tile(st1, [128, NTT, D], BF16, "h2tm")
                wr_t = kb.tile(st1, [128, KD, 8], F32, "wr")
                kb.dma("sp", wr_t.t[:, :, :], wr[m_].rearrange("p (k e) -> p k e", e=8), [dummy], [wr_t], wr_t)
                sm = [kb.tile(st1, [128, 8], F32, "sm%d" % i) for i in range(6)]
                s1 = [kb.tile(st1, [128, 1], F32, "s1%d" % i) for i in range(5)]
                zt = kb.tile(st1, [128, SUB, D], BF16, "zeros")
                kb.op("pool", lambda e: e.memset(zt.t[:, :, :], 0.0), [], [zt])
                for j in range(NB):
                    kb.dma("sp", XsD[j * BS:(j + 1) * BS, :].rearrange("(s p) d -> p s d", p=128), zt.t[:, :, :], [zt], [dXs], zt)
                with contextlib.ExitStack() as st2:
                    h2 = kb.tile(st2, [128, KD, 512], BF16, "h2r")
                    h2f = kb.tile(st2, [128, KD, 512], F32, "h2f")
                    nt4 = norm_tiles(st2)
                    for ci in clist:
                        t0, n = cfg.chunks[ci]
                        norm_chunk(nt4, XT, l, ci, V_NFFN, 3, 4, h2, 0, out_f32=h2f)
                        for tl_ in range(n // 128):
                            tt = (t0 - tok0) // 128 + tl_
                            cs = slice(tl_ * 128, (tl_ + 1) * 128)
                            p = kb.psum()
                            for k in range(KD):
                                kb.op("pe", mm(p.t[:, 0:8], h2f.t[:, k, cs], wr_t.t[:, k, :], k == 0, k == KD - 1), [h2f, wr_t], [p])
                            Lg, m1e, L2, selm, ex, gw = sm
                            m1, m2, nm1, ss, rs1 = s1
                            kb.op("dve", lambda e: e.tensor_copy(out=Lg.t[:, :], in_=p.t[:, 0:8]), [p], [Lg])
                            kb.op("dve", lambda e: e.reduce_max(out=m1.t[:, :], in_=Lg.t[:, :], axis=AX.X), [Lg], [m1])
                            kb.op("dve", lambda e: e.tensor_scalar(out=sel1A.t[:, tt, :], in0=Lg.t[:, :], scalar1=m1.t[:, 0:1], scalar2=None, op0=ALU.is_equal), [Lg, m1], [sel1A])
                            kb.op("dve", lambda e: e.tensor_scalar(out=m1e.t[:, :], in0=sel1A.t[:, tt, :], scalar1=-1e30, scalar2=None, op0=ALU.mult), [sel1A], [m1e])
                            kb.op("dve", lambda e: e.tensor_tensor(out=L2.t[:, :], in0=Lg.t[:, :], in1=m1e.t[:, :], op=ALU.add), [Lg, m1e], [L2])
                            kb.op("dve", lambda e: e.reduce_max(out=m2.t[:, :], in_=L2.t[:, :], axis=AX.X), [L2], [m2])
                            kb.op("dve", lambda e: e.tensor_scalar(out=selA.t[:, tt, :], in0=Lg.t[:, :], scalar1=m2.t[:, 0:1], scalar2=None, op0=ALU.is_ge), [Lg, m2], [selA])
                            kb.op("dve", lambda e: e.tensor_scalar(out=nm1.t[:, :], in0=m1.t[:, :], scalar1=-1.0, scalar2=None, op0=ALU.mult), [m1], [nm1])
                            kb.op("act", lambda e: e.activation(out=ex.t[:, :], in_=Lg.t[:, :], func=AF.Exp, bias=nm1.t[:, 0:1], scale=1.0), [Lg, nm1], [ex])
                            kb.op("dve", lambda e: e.tensor_tensor(out=ex.t[:, :], in0=ex.t[:, :], in1=selA.t[:, tt, :], op=ALU.mult), [selA], [ex])
                            kb.op("dve", lambda e: e.reduce_sum(out=ss.t[:, :], in_=ex.t[:, :], axis=AX.X), [ex], [ss])
                            kb.op("dve", lambda e: e.reciprocal(out=rs1.t[:, :], in_=ss.t[:, :]), [ss], [rs1])
                            kb.op("dve", lambda e: e.tensor_scalar(out=gwA.t[:, tt, :], in0=ex.t[:, :], scalar1=rs1.t[:, 0:1], scalar2=None, op0=ALU.mult), [ex, rs1], [gwA])
                            for hf in range(2):
                                p2 = kb.psum()
                                for kk in range(KD // 2):
                                    k = hf * (KD // 2) + kk
                                    kb.op("pe", mm(p2.t[:, kk * 128:(kk + 1) * 128], h2.t[:, k, cs], identb), [h2, cb], [p2])
                                evac(h2tm.t[:, tt, hf * 512:(hf + 1) * 512], h2tm, p2, p2.t[:, 0:512])
                    kb.barrier()
                with contextlib.ExitStack() as st2:
                    selb = kb.tile(st2, [128, NTT, 8], BF16, "selb")
                    cnt = kb.tile(st2, [128, 8], F32, "cnt")
                    nblk = kb.tile(st2, [128, 8], F32, "nblk")
                    pend = kb.tile(st2, [128, 8], F32, "pend")
                    pstart = kb.tile(st2, [128, 8], F32, "pstart")
                    tmpT = kb.tile(st2, [128, NTHR], F32, "tmpT")
                    sT = kb.tile(st2, [128, 1], F32, "sT")
                    bexp = kb.tile(st2, [128, NB], F32, "bexp")
                    tmpB = kb.tile(st2, [128, NB], F32, "tmpB")
                    eoff = kb.tile(st2, [128, NB], F32, "eoff")
                    idxf = kb.tile(st2, [128, NB * KF], F32, "idxf")
                    posf = kb.tile(st2, [128, 8], F32, "posf")
                    sel2 = kb.tile(st2, [128, 8], F32, "sel2")
                    tm8 = kb.tile(st2, [128, 8], F32, "tm8")
                    posAB = kb.tile(st2, [128, NTT * 2], F32, "posAB")
                    kb.op("dve", lambda e: e.tensor_copy(out=selb.t[:, :, :], in_=selA.t[:, :, :]), [selA], [selb])
                    pc = kb.psum()
                    for tt in range(NTT):
                        kb.op("pe", mm(pc.t[:, 0:8], ones128, selb.t[:, tt, :], tt == 0, tt == NTT - 1), [selb, cb], [pc])
                    kb.op("dve", lambda e: e.tensor_copy(out=cnt.t[:, :], in_=pc.t[:, 0:8]), [pc], [cnt])
                    for e_ in range(E):
                        kb.op("dve", lambda e: e.tensor_scalar(out=tmpT.t[:, :], in0=thr_c, scalar1=cnt.t[:, e_:e_ + 1], scalar2=None, op0=ALU.is_ge), [cf, cnt], [tmpT])
                        kb.op("dve", lambda e: e.reduce_sum(out=sT.t[:, :], in_=tmpT.t[:, :], axis=AX.X), [tmpT], [sT])
                        kb.op("dve", lambda e: e.tensor_scalar(out=nblk.t[:, e_:e_ + 1], in0=sT.t[:, :], scalar1=-1.0, scalar2=float(NTHR), op0=ALU.mult, op1=ALU.add), [sT], [nblk])
                    kb.op("dve", lambda e: e.tensor_copy(out=pend.t[:, 0:1], in_=nblk.t[:, 0:1]), [nblk], [pend])
                    for e_ in range(1, E):
                        kb.op("dve", lambda e: e.tensor_tensor(out=pend.t[:, e_:e_ + 1], in0=pend.t[:, e_ - 1:e_], in1=nblk.t[:, e_:e_ + 1], op=ALU.add), [nblk], [pend])
                    kb.op("dve", lambda e: e.tensor_tensor(out=pstart.t[:, :], in0=pend.t[:, :], in1=nblk.t[:, :], op=ALU.subtract), [pend, nblk], [pstart])
                    kb.op("dve", lambda e: e.tensor_scalar(out=pstart.t[:, :], in0=pstart.t[:, :], scalar1=float(BS), scalar2=None, op0=ALU.mult), [], [pstart])
                    for e_ in range(E):
                        if e_ == 0:
                            kb.op("dve", lambda e: e.tensor_scalar(out=bexp.t[:, :], in0=jrow_c[:, 0:NB], scalar1=pend.t[:, 0:1], scalar2=None, op0=ALU.is_ge), [cf, pend], [bexp])
                        else:
                            kb.op("dve", lambda e: e.tensor_scalar(out=tmpB.t[:, :], in0=jrow_c[:, 0:NB], scalar1=pend.t[:, e_:e_ + 1], scalar2=None, op0=ALU.is_ge), [cf, pend], [tmpB])
                            kb.op("dve", lambda e: e.tensor_tensor(out=bexp.t[:, :], in0=bexp.t[:, :], in1=tmpB.t[:, :], op=ALU.add), [tmpB], [bexp])
                    kb.op("dve", lambda e: e.tensor_scalar(out=bexp.t[:, :], in0=bexp.t[:, :], scalar1=float(E - 1), scalar2=None, op0=ALU.min), [], [bexp])
                    for (mult_, base_, dstI) in ((float(KF * 128), 0.0, gidxI), (float(F), 0.0, didxI)):
                        kb.op("dve", lambda e: e.tensor_scalar(out=eoff.t[:, :], in0=bexp.t[:, :], scalar1=mult_, scalar2=base_, op0=ALU.mult, op1=ALU.add), [bexp], [eoff])
                        for j in range(NB):
                            kb.op("dve", lambda e: e.tensor_scalar(out=idxf.t[:, j * KF:(j + 1) * KF], in0=cidx_c, scalar1=eoff.t[:, j:j + 1], scalar2=None, op0=ALU.add), [cf, eoff], [idxf])
                        kb.op("dve", lambda e: e.tensor_copy(out=dstI.t[:, :], in_=idxf.t[:, :]), [idxf], [dstI])
                    for tt in range(NTT):
                        pp = kb.psum()
                        for t2_ in range(tt):
                            kb.op("pe", mm(pp.t[:, 0:8], ones128, selb.t[:, t2_, :], t2_ == 0, False), [selb, cb], [pp])
                        kb.op("pe", mm(pp.t[:, 0:8], ustrict, selb.t[:, tt, :], tt == 0, True), [selb, cb], [pp])
                        kb.op("dve", lambda e: e.tensor_tensor(out=posf.t[:, :], in0=pp.t[:, 0:8], in1=pstart.t[:, :], op=ALU.add), [pp, pstart], [posf])
                        kb.op("dve", lambda e: e.tensor_tensor(out=sel2.t[:, :], in0=selA.t[:, tt, :], in1=sel1A.t[:, tt, :], op=ALU.subtract), [selA, sel1A], [sel2])
                        for a_, (selX, selXb) in enumerate(((sel1A.t[:, tt, :], sel1A), (sel2.t[:, :], sel2))):
                            kb.op("dve", lambda e: e.tensor_tensor(out=tm8.t[:, :], in0=posf.t[:, :], in1=selX, op=ALU.mult), [posf, selXb], [tm8])
                            kb.op("dve", lambda e: e.reduce_sum(out=posAB.t[:, tt * 2 + a_:tt * 2 + a_ + 1], in_=tm8.t[:, :], axis=AX.X), [tm8], [posAB])
                            kb.op("dve", lambda e: e.tensor_tensor(out=tm8.t[:, :], in0=gwA.t[:, tt, :], in1=selX, op=ALU.mult), [gwA, selXb], [tm8])
                            kb.op("dve", lambda e: e.reduce_sum(out=wAB.t[:, tt * 2 + a_:tt * 2 + a_ + 1], in_=tm8.t[:, :], axis=AX.X), [tm8], [wAB])
                    kb.op("dve", lambda e: e.tensor_copy(out=posI.t[:, :], in_=posAB.t[:, :]), [posAB], [posI])
                    scs = [Buf("scs%d" % i_) for i_ in range(8)]
                    for tt in range(NTT):
                        for a_ in range(2):
                            kb.idma(XsD[:, :], bass.IndirectOffsetOnAxis(ap=posI.t[:, tt * 2 + a_:tt * 2 + a_ + 1], axis=0), h2tm.t[:, tt, :], None, [h2tm, posI, dXs], [Buf("snk")], scs[(tt * 2 + a_) % 8])
                    kb.barrier()
            with contextlib.ExitStack() as st1:
                Xg = [kb.tile(st1, [128, SUB, D], BF16, "Xg") for _ in range(2)]
                XTb = kb.tile(st1, [128, KD, BS], BF16, "XTb")
                actT = kb.tile(st1, [128, KF, BS], BF16, "actT")
                gbb = [kb.tile(st1, [128, KD * 256], BF16, "gbb") for _ in range(4)]
                dbb = [kb.tile(st1, [128, D], BF16, "dbb") for _ in range(4)]
                sl = [kb.tile(st1, [128, BS], BF16, "silu") for _ in range(3)]
                yst = [kb.tile(st1, [128, D], F32, "yst") for _ in range(2)]
                wc = 0
                for j in range(NB):
                    xg = Xg[j % 2]
                    kb.dma("sp", xg.t[:, :, :], XsD[j * BS:(j + 1) * BS, :].rearrange("(s p) d -> p s d", p=128), [dXs], [xg], xg)
                    for k in range(KD):
                        p = kb.psum()
                        for s_ in range(SUB):
                            kb.op("pe", mm(p.t[:, s_ * 128:(s_ + 1) * 128], xg.t[:, s_, k * 128:(k + 1) * 128], identb), [xg, cb], [p])
                        evac(XTb.t[:, k, :], XTb, p, p.t[:, 0:BS])
                    for f in range(KF):
                        gb = gbb[wc % 4]; wc += 1
                        kb.idma(gb.t[:, :], None, WbfG[:, :], bass.IndirectOffsetOnAxis(ap=gidxI.t[:, j * KF + f:j * KF + f + 1], axis=0), [gidxI, dWbf], [gb], gb)
                        pg = kb.psum(); pu = kb.psum()
                        for k in range(KD):
                            kb.op("pe", mm(pg.t[:, 0:BS], gb.t[:, k * 256:k * 256 + 128], XTb.t[:, k, :], k == 0, k == KD - 1), [gb, XTb], [pg])
                        for k in range(KD):
                            kb.op("pe", mm(pu.t[:, 0:BS], gb.t[:, k * 256 + 128:k * 256 + 256], XTb.t[:, k, :], k == 0, k == KD - 1), [gb, XTb], [pu])
                        sl_ = sl[f % 3]
                        kb.op("act", lambda e: e.activation(out=sl_.t[:, :], in_=pg.t[:, 0:BS], func=AF.Silu), [pg], [sl_])
                        kb.op("dve", lambda e: e.tensor_tensor(out=actT.t[:, f, :], in0=pu.t[:, 0:BS], in1=sl_.t[:, :], op=ALU.mult), [pu, sl_], [actT])
                    for kf in range(KF):
                        db = dbb[wc % 4]; wc += 1
                        kb.idma(db.t[:, :], None, WbfD[:, :], bass.IndirectOffsetOnAxis(ap=didxI.t[:, j * KF + kf:j * KF + kf + 1], axis=0), [didxI, dWbf], [db], db)
                        for s_ in range(SUB):
                            for hf in range(D // 512):
                                pb_ = kb.ps[(s_ * (D // 512) + hf) % 8]
                                kb.op("pe", mm(pb_.t[:, 0:512], actT.t[:, kf, s_ * 128:(s_ + 1) * 128], db.t[:, hf * 512:(hf + 1) * 512], kf == 0, kf == KF - 1), [actT, db], [pb_])
                    for s_ in range(SUB):
                        y_ = yst[s_ % 2]
                        for hf in range(D // 512):
                            pb_ = kb.ps[(s_ * (D // 512) + hf) % 8]
                            evac(y_.t[:, hf * 512:(hf + 1) * 512], y_, pb_, pb_.t[:, 0:512])
                        r0 = j * BS + s_ * 128
                        kb.dma("sp", YsD[r0:r0 + 128, :], y_.t[:, :], [y_], [dYs], y_)
                kb.barrier()
            with contextlib.ExitStack() as st1:
                yA = [kb.tile(st1, [128, D], F32, "yA") for _ in range(4)]
                yB = [kb.tile(st1, [128, D], F32, "yB") for _ in range(4)]
                u4 = [kb.tile(st1, [128, 4, D], F32, "u4") for _ in range(2)]
                xts = [kb.tile(st1, [128, 512], F32, "x5") for _ in range(3)]
                tr5 = [kb.tile(st1, [128, 512], F32, "tr5") for _ in range(2)]
                xc = 0
                for qi, ci in enumerate(clist):
                    t0, n = cfg.chunks[ci]
                    s_i = 1 if ci == 0 else 0
                    u_ = u4[qi % 2]
                    for tl_ in range(n // 128):
                        tt = (t0 - tok0) // 128 + tl_
                        a_ = yA[tt % 4]; b_ = yB[tt % 4]
                        kb.idma(a_.t[:, :], None, YsD[:, :], bass.IndirectOffsetOnAxis(ap=posI.t[:, tt * 2:tt * 2 + 1], axis=0), [posI, dYs], [a_], a_)
                        kb.idma(b_.t[:, :], None, YsD[:, :], bass.IndirectOffsetOnAxis(ap=posI.t[:, tt * 2 + 1:tt * 2 + 2], axis=0), [posI, dYs], [b_], b_)
                        kb.op("dve", lambda e: e.tensor_scalar(out=u_.t[:, tl_, :], in0=a_.t[:, :], scalar1=wAB.t[:, tt * 2:tt * 2 + 1], scalar2=None, op0=ALU.mult), [a_, wAB], [u_])
                        kb.op("dve", lambda e: e.scalar_tensor_tensor(out=u_.t[:, tl_, :], in0=b_.t[:, :], scalar=wAB.t[:, tt * 2 + 1:tt * 2 + 2], in1=u_.t[:, tl_, :], op0=ALU.mult, op1=ALU.add), [b_, wAB], [u_])
                    for k in range(KD):
                        p = kb.psum()
                        for tl_ in range(n // 128):
                            kb.op("pe", mm(p.t[:, tl_ * 128:(tl_ + 1) * 128], u_.t[:, tl_, k * 128:(k + 1) * 128], id128f), [u_, cf], [p])
                        tr_ = tr5[xc % 2]
                        x_ = xts[xc % 3]; xc += 1
                        js = slice(k * 128, (k + 1) * 128)
                        kb.dma("sp", x_.t[:, 0:n], XT[js, t0:t0 + n], [dummy], [x_], x_)
                        kb.op("act", lambda e: e.activation(out=tr_.t[:, 0:n], in_=p.t[:, 0:n], func=AF.Identity, scale=mod(l, 5, k, s_i)), [p, modv], [tr_])
                        kb.op("dve", lambda e: e.tensor_tensor(out=x_.t[:, 0:n], in0=x_.t[:, 0:n], in1=tr_.t[:, 0:n], op=ALU.add), [tr_], [x_])
                        kb.dma("pool", XT[js, t0:t0 + n], x_.t[:, 0:n], [x_], [Buf("snk")], x_)
                kb.barrier()

    for l in range(L):
        last = l == L - 1
        moe = l % 2 == 1
        xsrc = xT_in if l == 0 else XT
        nch = len(cfg.chunks)
        halves = [list(range(0, (nch + 1) // 2)), list(range((nch + 1) // 2, nch))]
        for hchunks in halves:
          if not hchunks:
              continue
          hb = cfg.chunks[hchunks[0]][0]
          W = sum(cfg.chunks[ci][1] for ci in hchunks)
          with contextlib.ExitStack() as st:
            hT = kb.tile(st, [128, KD, W], BF16, "hT")
            with contextlib.ExitStack() as st2:
                nts = [norm_tiles(st2) for _ in range(2)]
                for i_, ci in enumerate(hchunks):
                    t0, n = cfg.chunks[ci]
                    norm_chunk(nts[i_ % 2], xsrc, l, ci, V_NMIX, 0, 1, hT, t0 - hb)
                kb.barrier()
            rt = kb.tile(st, [128, 4, W], BF16, "ropet")
            if stop == "P1a":
                return nc
            kb.dma("sp", rt.t[:, :, :], ropet[:, :, hb:hb + W], [dummy], [rt], rt)
            ropeg_cos = rt.t[:, 0, :]; ropeg_sin = rt.t[:, 1, :]; ropem_cos = rt.t[:, 2, :]; ropem_sin = rt.t[:, 3, :]
            stgs = [kb.tile(st, [128, KD * 128], F32, "wstg") for _ in range(3)]
            wbs = [kb.tile(st, [128, KD, 128], BF16, "wb") for _ in range(4)]
            outs = [kb.tile(st, [128, 512], BF16, "o1") for _ in range(4)]
            sqb = [kb.tile(st, [128, 512], BF16, "sqb") for _ in range(2)]
            rsb = [kb.tile(st, [128, 512], F32, "rsb") for _ in range(2)]
            tf = [kb.tile(st, [128, 512], F32, "tf") for _ in range(4)]
            cnt = {"w": 0, "o": 0, "s": 0, "t": 0, "r": 0}

            def getw(nt):
                i = cnt["w"]; cnt["w"] += 1
                sg = stgs[i % 3]; wb_ = wbs[i % 4]
                kb.dma("sp", sg.t[:, :], w1[l, nt], [dummy], [sg], sg)
                cast(wb_.t[:, :, :], wb_, sg.t[:, :].rearrange("p (k c) -> p k c", k=KD), sg)
                return wb_

            def proj(wb_, ci):
                t0, n = cfg.chunks[ci]
                p = kb.psum()
                for k in range(KD):
                    kb.op("pe", mm(p.t[:, 0:n], wb_.t[:, k, :], hT.t[:, k, t0 - hb:t0 - hb + n], k == 0, k == KD - 1), [wb_, hT], [p])
                return p

            def nxt(lst, key):
                i = cnt[key]; cnt[key] += 1
                return lst[i % len(lst)]

            def store(o_, n, dst_ap, dbuf):
                kb.dma("pool", dst_ap, o_.t[:, 0:n], [o_], [dbuf], o_)

            def plain_group(tiles, dst, dbuf, func=None):
                for j, nt in enumerate(tiles):
                    wb_ = getw(nt)
                    for ci in hchunks:
                        t0, n = cfg.chunks[ci]
                        p = proj(wb_, ci)
                        o_ = nxt(outs, "o")
                        evac(o_.t[:, 0:n], o_, p, p.t[:, 0:n], func)
                        store(o_, n, dst[j * 128:(j + 1) * 128, t0:t0 + n], dbuf)

            def sq_rstd(plist, ones_ap, dim, n):
                rs_ = nxt(rsb, "r")
                pc = kb.psum()
                for i_, pa in enumerate(plist):
                    sq_ = nxt(sqb, "s")
                    kb.op("act", lambda e: e.activation(out=sq_.t[:, 0:n], in_=pa.t[:, 0:n], func=AF.Square), [pa], [sq_])
                    kb.op("pe", mm(pc.t[:, 0:n], ones_ap, sq_.t[:, 0:n], i_ == 0, i_ == len(plist) - 1), [sq_, cb], [pc])
                rstd_from(pc, n, dim, rs_)
                return rs_

            def rope_norm_group(tiles, rtiles, gcol, grcol, dst, dbuf):
                for j in range(len(tiles)):
                    wa = getw(tiles[j]); wr_ = getw(rtiles[j])
                    for ci in hchunks:
                        t0, n = cfg.chunks[ci]
                        c0 = t0 - hb
                        pa = proj(wa, ci); pb = proj(wr_, ci)
                        STEP = int(os.environ.get("STEP", "99"))
                        if STEP < 1:
                            continue
                        rs_ = sq_rstd([pa], bd64, 64, n)
                        t1 = nxt(tf, "t"); t2 = nxt(tf, "t")
                        if STEP < 2:
                            continue
                        kb.op("act", lambda e: e.activation(out=t1.t[:, 0:n], in_=pa.t[:, 0:n], func=AF.Identity, scale=vcol(l, gcol)), [pa, vc], [t1])
                        kb.op("act", lambda e: e.activation(out=t2.t[:, 0:n], in_=pb.t[:, 0:n], func=AF.Identity, scale=vcol(l, grcol)), [pb, vc], [t2])
                        kb.op("dve", lambda e: e.tensor_tensor(out=t1.t[:, 0:n], in0=t1.t[:, 0:n], in1=ropeg_cos[:, c0:c0 + n], op=ALU.mult), [rt], [t1])
                        kb.op("dve", lambda e: e.tensor_tensor(out=t2.t[:, 0:n], in0=t2.t[:, 0:n], in1=ropeg_sin[:, c0:c0 + n], op=ALU.mult), [rt], [t2])
                        if STEP < 3:
                            continue
                        kb.op("pool", lambda e: e.tensor_tensor(out=t1.t[:, 0:n], in0=t1.t[:, 0:n], in1=t2.t[:, 0:n], op=ALU.add), [t2], [t1])
                        o_ = nxt(outs, "o")
                        if STEP < 4:
                            continue
                        kb.op("dve", lambda e: e.tensor_tensor(out=o_.t[:, 0:n], in0=t1.t[:, 0:n], in1=rs_.t[:, 0:n], op=ALU.mult), [t1, rs_], [o_])
                        store(o_, n, dst[j * 128:(j + 1) * 128, t0:t0 + n], dbuf)

            plain_group([0, 1], KnaT, dQK["KnaT"])
            if stop == "P1b":
                kb.barrier(); return nc
            rope_norm_group([2], [3], V_KG, V_KGR, KgT, dQK["KgT"])
            if stop == "P1c":
                kb.barrier(); return nc
            plain_group([7, 8], QnaT, dQK["QnaT"])
            rope_norm_group([9, 10, 11, 12], [13, 14, 15, 16], V_QG, V_QGR, QgT, dQK["QgT"])
            plain_group(list(range(19, 19 + 24)), GT, dQK["GT"], func=AF.Sigmoid)
            if stop == "P1d":
                kb.barrier(); return nc
            ckvn = kb.tile(st, [128, W], BF16, "ckvn")
            cqn = kb.tile(st, [128, 2, W], BF16, "cqn")
            krope = kb.tile(st, [128, W], BF16, "krope")
            wa = getw(4)
            for ci in hchunks:
                t0, n = cfg.chunks[ci]
                c0 = t0 - hb
                pa = proj(wa, ci)
                rs_ = sq_rstd([pa], ones128, 128, n)
                t1 = nxt(tf, "t")
                kb.op("act", lambda e: e.activation(out=t1.t[:, 0:n], in_=pa.t[:, 0:n], func=AF.Identity, scale=vcol(l, V_KVL)), [pa, vc], [t1])
                kb.op("dve", lambda e: e.tensor_tensor(out=ckvn.t[:, c0:c0 + n], in0=t1.t[:, 0:n], in1=rs_.t[:, 0:n], op=ALU.mult), [t1, rs_], [ckvn])
            wa = getw(17); wb2 = getw(18)
            for ci in hchunks:
                t0, n = cfg.chunks[ci]
                c0 = t0 - hb
                pa = proj(wa, ci); pb = proj(wb2, ci)
                rs_ = sq_rstd([pa, pb], ones128, 256, n)
                t1 = nxt(tf, "t"); t2 = nxt(tf, "t")
                kb.op("act", lambda e: e.activation(out=t1.t[:, 0:n], in_=pa.t[:, 0:n], func=AF.Identity, scale=vcol(l, V_QL)), [pa, vc], [t1])
                kb.op("act", lambda e: e.activation(out=t2.t[:, 0:n], in_=pb.t[:, 0:n], func=AF.Identity, scale=vcol(l, V_QL + 1)), [pb, vc], [t2])
                kb.op("dve", lambda e: e.tensor_tensor(out=cqn.t[:, 0, c0:c0 + n], in0=t1.t[:, 0:n], in1=rs_.t[:, 0:n], op=ALU.mult), [t1, rs_], [cqn])
                kb.op("dve", lambda e: e.tensor_tensor(out=cqn.t[:, 1, c0:c0 + n], in0=t2.t[:, 0:n], in1=rs_.t[:, 0:n], op=ALU.mult), [t2, rs_], [cqn])
            wa = getw(5); wb2 = getw(6)
            for ci in hchunks:
                t0, n = cfg.chunks[ci]
                c0 = t0 - hb
                pa = proj(wa, ci); pb = proj(wb2, ci)
                t1 = nxt(tf, "t"); t2 = nxt(tf, "t")
                kb.op("dve", lambda e: e.tensor_tensor(out=t1.t[64:96, 0:n], in0=pa.t[64:96, 0:n], in1=ropem_cos[64:96, c0:c0 + n], op=ALU.mult), [pa, rt], [t1])
                kb.op("dve", lambda e: e.tensor_tensor(out=t2.t[64:96, 0:n], in0=pb.t[64:96, 0:n], in1=ropem_sin[64:96, c0:c0 + n], op=ALU.mult), [pb, rt], [t2])
                kb.op("dve", lambda e: e.tensor_tensor(out=krope.t[64:96, c0:c0 + n], in0=t1.t[64:96, 0:n], in1=t2.t[64:96, 0:n], op=ALU.add), [t1, t2], [krope])
            w2s = kb.tile(st, [128, 1536], F32, "w2s")
            wuq_b = kb.tile(st, [128, 2, 384], BF16, "wuq")
            wuqr_b = kb.tile(st, [128, 2, 384], BF16, "wuqr")
            wkk_b = kb.tile(st, [128, 256], BF16, "wkk")
            wkv_b = kb.tile(st, [128, 256], BF16, "wkv")
            wv_b = kb.tile(st, [128, KD, 384], BF16, "wvb")
            for src_, dst_, wd_, db_ in ((wuq[l], wuq_b.t[:, :, :].rearrange("p a b -> p (a b)"), 768, wuq_b), (wuqr[l], wuqr_b.t[:, :, :].rearrange("p a b -> p (a b)"), 768, wuqr_b),
                                        (wukvk[l], wkk_b.t[:, :], 256, wkk_b), (wukvv[l], wkv_b.t[:, :], 256, wkv_b),
                                        (wv[l][:, 0:1536], wv_b.t[:, 0:KD // 2, :].rearrange("p a b -> p (a b)"), 1536, wv_b),
                                        (wv[l][:, 1536:3072], wv_b.t[:, KD // 2:KD, :].rearrange("p a b -> p (a b)"), 1536, wv_b)):
                kb.dma("sp", w2s.t[:, 0:wd_], src_, [dummy], [w2s], w2s)
                kb.op("dve", lambda e: e.tensor_copy(out=dst_, in_=w2s.t[:, 0:wd_]), [w2s], [db_])
            kst = [kb.tile(st, [128, 512], BF16, "kst") for _ in range(3)]
            if stop == "P1e":
                kb.barrier(); return nc
            for ci in hchunks:
                t0, n = cfg.chunks[ci]
                c0 = t0 - hb
                for h in range(4):
                    p = kb.psum()
                    kb.op("pe", mm(p.t[0:64, 0:n], wkk_b.t[:, h * 64:(h + 1) * 64], ckvn.t[:, c0:c0 + n]), [wkk_b, ckvn], [p])
                    o_ = nxt(kst, "o")
                    evac(o_.t[0:64, 0:n], o_, p, p.t[0:64, 0:n])
                    kb.op("pool", lambda e: e.tensor_copy(out=o_.t[64:96, 0:n], in_=krope.t[64:96, c0:c0 + n]), [krope], [o_])
                    kb.dma("pool", KmT[h * 96:(h + 1) * 96, t0:t0 + n], o_.t[0:96, 0:n], [o_], [dQK["KmT"]], o_)
                    p = kb.psum(); pr = kb.psum()
                    for k in range(2):
                        kb.op("pe", mm(p.t[0:96, 0:n], wuq_b.t[:, k, h * 96:(h + 1) * 96], cqn.t[:, k, c0:c0 + n], k == 0, k == 1), [wuq_b, cqn], [p])
                    for k in range(2):
                        kb.op("pe", mm(pr.t[0:96, 0:n], wuqr_b.t[:, k, h * 96:(h + 1) * 96], cqn.t[:, k, c0:c0 + n], k == 0, k == 1), [wuqr_b, cqn], [pr])
                    o_ = nxt(kst, "o")
                    evac(o_.t[0:64, 0:n], o_, p, p.t[0:64, 0:n])
                    t1 = nxt(tf, "t"); t2 = nxt(tf, "t")
                    kb.op("dve", lambda e: e.tensor_tensor(out=t1.t[64:96, 0:n], in0=p.t[64:96, 0:n], in1=ropem_cos[64:96, c0:c0 + n], op=ALU.mult), [p, rt], [t1])
                    kb.op("dve", lambda e: e.tensor_tensor(out=t2.t[64:96, 0:n], in0=pr.t[64:96, 0:n], in1=ropem_sin[64:96, c0:c0 + n], op=ALU.mult), [pr, rt], [t2])
                    kb.op("dve", lambda e: e.tensor_tensor(out=o_.t[64:96, 0:n], in0=t1.t[64:96, 0:n], in1=t2.t[64:96, 0:n], op=ALU.add), [t1, t2], [o_])
                    kb.dma("pool", QmT[h * 96:(h + 1) * 96, t0:t0 + n], o_.t[0:96, 0:n], [o_], [dQK["QmT"]], o_)
            if stop == "P1f":
                kb.barrier(); return nc
            vst = [kb.tile(st, [128, 10, 65], BF16, "vst") for _ in range(3)]
            for v_ in vst:
                kb.op("pool", lambda e: e.memset(v_.t[:, :, :], 1.0), [], [v_])
            for tt in range(hb // 128, (hb + W) // 128):
                c0 = tt * 128 - hb
                p = kb.psum(); p2 = kb.psum()
                for k in range(KD):
                    kb.op("pe", mm(p.t[:, 0:384], hT.t[:, k, c0:c0 + 128], wv_b.t[:, k, :], k == 0, k == KD - 1), [hT, wv_b], [p])
                kb.op("pe", mm(p2.t[:, 0:256], ckvn.t[:, c0:c0 + 128], wkv_b.t[:, :]), [ckvn, wkv_b], [p2])
                v_ = nxt(vst, "o")
                kb.op("act", lambda e: e.activation(out=v_.t[:, 0:6, 0:64], in_=p.t[:, 0:384].rearrange("p (h d) -> p h d", h=6), func=AF.Copy), [p], [v_])
                kb.op("dve", lambda e: e.tensor_copy(out=v_.t[:, 6:10, 0:64], in_=p2.t[:, 0:256].rearrange("p (h d) -> p h d", h=4)), [p2], [v_])
                rs_ = slice(tt * 128, (tt + 1) * 128)
                kb.dma("pool", Vna[rs_, :].rearrange("t (h d) -> t h d", h=4), v_.t[:, 0:4, :], [v_], [dQK["Vna"]], v_)
                kb.dma("pool", Vg[rs_, :].rearrange("t (h d) -> t h d", h=2), v_.t[:, 4:6, :], [v_], [dQK["Vg"]], v_)
                kb.dma("pool", Vm[rs_, :].rearrange("t (h d) -> t h d", h=4), v_.t[:, 6:10, :], [v_], [dQK["Vm"]], v_)
            kb.barrier()

        if stop == "P1":
            return nc
        PS_S = (0, 4); PS_O = (4, 2); PS_B = (6, 2)

        def attn_phase(name, dk, nh, nkv, Ksrc, Qsrc, Vsrc, scale, yrow0, na=False):
            with contextlib.ExitStack() as st:
                pk = 128 if dk == 64 else dk
                Kt = kb.tile(st, [pk, nkv, T], BF16, "K" + name)
                Vt = kb.tile(st, [128, NKT, nkv, 65], BF16, "V" + name)
                if pk != dk:
                    kb.op("pool", lambda e: e.memset(Kt.t[dk:pk, :, :], 0.0), [], [Kt])
                kb.dma("sp", Kt.t[0:dk, :, :], Ksrc.rearrange("(h d) t -> d h t", d=dk), [dQK["K%sT" % name]], [Kt], Kt)
                kb.dma("sp", Vt.t[:, :, :, :], Vsrc.rearrange("(k p) (h d) -> p k h d", p=128, d=65), [dQK["V" + name]], [Vt], Vt)
                if na:
                    Vo = kb.tile(st, [128, NKT - CT - 1, nkv, 65], BF16, "Vo")
                    kb.dma("sp", Vo.t[:, :, :, :], Vsrc[C + 64:T - 64, :].rearrange("(k p) (h d) -> p k h d", p=128, d=65), [dQK["V" + name]], [Vo], Vo)
                    rst = kb.tile(st, [64, 3840], F32, "rpbst")
                    Tc = kb.tile(st, [128, 4, 15, 64], BF16, "Tcat")
                    kb.op("pool", lambda e: e.memset(Tc.t[64:128, :, :, :], 0.0), [], [Tc])
                    kb.dma("sp", rst.t[:, :], rpbT[l], [dummy], [rst], rst)
                    ngm = kb.tile(st, [64, 3840], BF16, "negm")
                    kb.dma("sp", ngm.t[:, :], negm[:, :], [dummy], [ngm], ngm)
                    kb.op("dve", lambda e: e.scalar_tensor_tensor(out=Tc.t[0:64, :, :, :].rearrange("p a b c -> p (a b c)"), in0=rst.t[:, :], scalar=8.0, in1=ngm.t[:, :], op0=ALU.mult, op1=ALU.add), [rst, ngm], [Tc])
                Qs = [kb.tile(st, [pk, nh, 512], BF16, "Q" + name) for _ in range(2)]
                if pk != dk:
                    for q_ in Qs:
                        kb.op("pool", lambda e, q_=q_: e.memset(q_.t[dk:pk, :, :], 0.0), [], [q_])
                Ps = [kb.tile(st, [128, 512], BF16, "P" + name) for _ in range(4)]
                Rt = kb.tile(st, [65, 512], F32, "Rt")
                bcs = [kb.tile(st, [64, 512], F32, "bc") for _ in range(2)]
                ys = [kb.tile(st, [64, 512], BF16, "ys") for _ in range(3)]
                kb.op("pool", lambda e: e.memset(Rt.t[:, :], 0.0), [], [Rt])
                if conv.active():
                    conv.attach(st)
                c_ = {"p": 0, "y": 0, "b": 0}
                pnorm = {"f": None}
                clist = list(range(len(cfg.chunks)))
                if last:
                    clist = clist[1:]
                for qi, ci in enumerate(clist):
                    t0, n = cfg.chunks[ci]
                    Q = Qs[qi % 2]
                    kb.dma("sp", Q.t[0:dk, :, 0:n], Qsrc[:, t0:t0 + n].rearrange("(h d) t -> d h t", d=dk), [dQK["Q%sT" % name]], [Q], Q)
                    for h in range(nh):
                        kvh = h * nkv // nh
                        po = kb.psum(PS_O)
                        if ci == 0 or not na:
                            kts = list(range(CT)) if ci == 0 else list(range(NKT))
                            LA = 2
                            pend = []
                            for i_, kt in enumerate(kts):
                                ps_ = kb.psum(PS_S)
                                kb.op("pe", mm(ps_.t[:, 0:n], Kt.t[:, kvh, kt * 128:(kt + 1) * 128], Q.t[:, h, 0:n]), [Kt, Q], [ps_])
                                pend.append((ps_, kt))
                                if i_ == min(LA, len(kts) - 1) and pnorm["f"] is not None:
                                    pnorm["f"](); pnorm["f"] = None
                                if len(pend) > LA or i_ == len(kts) - 1:
                                    while pend and (len(pend) > LA or i_ == len(kts) - 1):
                                        ps2, kt2 = pend.pop(0)
                                        P = Ps[c_["p"] % 4]; c_["p"] += 1
                                        kb.op("act", lambda e, P=P, ps2=ps2: e.activation(out=P.t[:, 0:n], in_=ps2.t[:, 0:n], func=AF.Exp, scale=scale), [ps2], [P])
                                        kb.op("pe", mm(po.t[0:65, 0:n], Vt.t[:, kt2, kvh, :], P.t[:, 0:n], kt2 == kts[0], kt2 == kts[-1]), [Vt, P], [po])
                        else:
                            r0 = (t0 - C) // 64
                            pendu = []

                            def finish_unit(u):
                                ps_u, groups_u, qs_u = u
                                ng = len(groups_u)
                                P = Ps[c_["p"] % 4]; c_["p"] += 1
                                kb.op("act", lambda e: e.activation(out=P.t[:, 0:ng * 64], in_=ps_u.t[:, 0:ng * 64], func=AF.Exp, scale=scale), [ps_u], [P])
                                for gi, (tk, vt, vb, dr) in enumerate(groups_u):
                                    kb.op("pe", mm(po.t[0:65, qs_u], vt, P.t[:, gi * 64:(gi + 1) * 64], gi == 0, gi == ng - 1), [vb, P], [po])
                            for rl in range(n // 64):
                                r = r0 + rl
                                row0 = min(max(r - 4, 0), R - 8)
                                qs = slice(rl * 64, (rl + 1) * 64)
                                ps_ = kb.psum(PS_S)
                                groups = []
                                for g in range(4):
                                    tk = C + (row0 + 2 * g) * 64
                                    if row0 % 2 == 0:
                                        vt = Vt.t[:, tk // 128, kvh, :]
                                        vb = Vt
                                    else:
                                        vt = Vo.t[:, (tk - C - 64) // 128, kvh, :]
                                        vb = Vo
                                    groups.append((tk, vt, vb, row0 + 2 * g - r))
                                for g in range(CT):
                                    groups.append((g * 128, Vt.t[:, g, kvh, :], Vt, None))
                                for gi, (tk, vt, vb, dr) in enumerate(groups):
                                    osl = slice(gi * 64, (gi + 1) * 64)
                                    kb.op("pe", mm(ps_.t[:, osl], Kt.t[:, kvh, tk:tk + 128], Q.t[:, h, qs], True, dr is None), [Kt, Q], [ps_])
                                    if dr is not None:
                                        kb.op("pe", mm(ps_.t[:, osl], Tc.t[:, h, dr + 7:dr + 9, :].rearrange("p a b -> p (a b)"), id64b, False, True), [Tc, cb], [ps_])
                                pendu.append((ps_, groups, qs))
                                if rl == 0 and pnorm["f"] is not None:
                                    pnorm["f"](); pnorm["f"] = None
                                if len(pendu) > 2:
                                    finish_unit(pendu.pop(0))
                            while pendu:
                                finish_unit(pendu.pop(0))
                        kb.op("dve", lambda e, po=po: e.reciprocal(out=Rt.t[64:65, 0:n], in_=po.t[64:65, 0:n]), [po], [Rt])
                        pb_ = kb.psum(PS_B)
                        kb.op("pe", mm(pb_.t[:, 0:n], sel64, Rt.t[0:65, 0:n]), [cf, Rt], [pb_])

                        def rest(po=po, pb_=pb_, h=h, t0=t0, n=n):
                            bc = bcs[c_["b"] % 2]; c_["b"] += 1
                            kb.op("dve", lambda e: e.tensor_copy(out=bc.t[:, 0:n], in_=pb_.t[0:64, 0:n]), [pb_], [bc])
                            y_ = ys[c_["y"] % 3]; c_["y"] += 1
                            kb.op("dve", lambda e: e.tensor_tensor(out=y_.t[:, 0:n], in0=po.t[0:64, 0:n], in1=bc.t[:, 0:n], op=ALU.mult), [po, bc], [y_])
                            kb.dma("pool", YT[yrow0 + h * 64:yrow0 + (h + 1) * 64, t0:t0 + n], y_.t[:, 0:n], [y_], [dQK["YT"]], y_)
                            conv.step()
                        pnorm["f"] = rest
                if pnorm["f"] is not None:
                    pnorm["f"](); pnorm["f"] = None
                conv.drain(everything=(moe and name == "m"))
                kb.barrier()

        if l + 1 < L and (l + 1) % 2 == 1:
            conv.add_layer((l + 1) // 2)
        elif l == 0 and moe:
            conv.add_layer(0)
        attn_phase("na", 64, 4, 4, KnaT, QnaT, Vna, 0.125, 0, na=True)
        if stop == "P2a":
            return nc
        attn_phase("g", 64, 8, 2, KgT, QgT, Vg, 0.125, 256)
        attn_phase("m", 96, 4, 4, KmT, QmT, Vm, 96 ** -0.5, 768)

        if stop == "P2":
            return nc
        with contextlib.ExitStack() as st:
            stg = [kb.tile(st, [128, 2048], F32, "w3s") for _ in range(2)]
            wo_b = kb.tile(st, [128, 8, D], BF16, "wo")
            wout_b = kb.tile(st, [128, KD, D], BF16, "wout")
            i = 0
            for src_, dst_ in ((wo[l], wo_b), (wout[l], wout_b)):
                for k in range(0, 8, 2):
                    sg = stg[i % 2]; i += 1
                    kb.dma("sp", sg.t[:, :], src_[:, k * D:(k + 2) * D], [dummy], [sg], sg)
                    cast(dst_.t[:, k:k + 2, :], dst_, sg.t[:, :].rearrange("p (a b) -> p a b", a=2), sg)
            Ys = [kb.tile(st, [128, 8, 512], BF16, "Y") for _ in range(2)]
            Gs = [kb.tile(st, [128, 24, 512], BF16, "G") for _ in range(2)]
            Xs = [kb.tile(st, [128, KD, 512], F32, "X") for _ in range(2)]
            mT = [kb.tile(st, [128, KD, 512], BF16, "m") for _ in range(2)]
            tfs = [kb.tile(st, [128, 512], F32, "t3") for _ in range(4)]
            tc = 0
            clist = list(range(len(cfg.chunks)))
            if last:
                clist = clist[1:]
            for qi, ci in enumerate(clist):
                t0, n = cfg.chunks[ci]
                s_ = 1 if ci == 0 else 0
                Y = Ys[qi % 2]; G = Gs[qi % 2]; X = Xs[qi % 2]; m_ = mT[qi % 2]
                kb.dma("sp", Y.t[:, :, 0:n], YT[:, t0:t0 + n].rearrange("(k p) t -> p k t", p=128), [dQK["YT"]], [Y], Y)
                kb.dma("sp", G.t[:, :, 0:n], GT[:, t0:t0 + n].rearrange("(k p) t -> p k t", p=128), [dQK["GT"]], [G], G)
                kb.dma("sp", X.t[:, :, 0:n], xsrc[:, t0:t0 + n].rearrange("(k p) t -> p k t", p=128), [dXT[ci]], [X], X)
                for j in range(KD):
                    js = slice(j * 128, (j + 1) * 128)
                    pa = kb.psum(); pb = kb.psum(); pc = kb.psum()
                    for k in range(2):
                        kb.op("pe", mm(pa.t[:, 0:n], wo_b.t[:, k, js], Y.t[:, k, 0:n], k == 0, k == 1), [wo_b, Y], [pa])
                    for k in range(4):
                        kb.op("pe", mm(pb.t[:, 0:n], wo_b.t[:, 2 + k, js], Y.t[:, 2 + k, 0:n], k == 0, k == 3), [wo_b, Y], [pb])
                    for k in range(2):
                        kb.op("pe", mm(pc.t[:, 0:n], wo_b.t[:, 6 + k, js], Y.t[:, 6 + k, 0:n], k == 0, k == 1), [wo_b, Y], [pc])
                    t1 = tfs[tc % 4]; t2 = tfs[(tc + 1) % 4]; tc += 2
                    kb.op("dve", lambda e, t1=t1, pa=pa, G=G, j=j: e.tensor_tensor(out=t1.t[:, 0:n], in0=pa.t[:, 0:n], in1=G.t[:, j, 0:n], op=ALU.mult), [pa, G], [t1])
                    kb.op("dve", lambda e, t2=t2, pb=pb, G=G, j=j: e.tensor_tensor(out=t2.t[:, 0:n], in0=pb.t[:, 0:n], in1=G.t[:, 8 + j, 0:n], op=ALU.mult), [pb, G], [t2])
                    kb.op("pool", lambda e, t1=t1, t2=t2: e.tensor_tensor(out=t1.t[:, 0:n], in0=t1.t[:, 0:n], in1=t2.t[:, 0:n], op=ALU.add), [t2], [t1])
                    kb.op("dve", lambda e, t2=t2, pc=pc, G=G, j=j: e.tensor_tensor(out=t2.t[:, 0:n], in0=pc.t[:, 0:n], in1=G.t[:, 16 + j, 0:n], op=ALU.mult), [pc, G], [t2])
                    kb.op("pool", lambda e, t1=t1, t2=t2, m_=m_, j=j: e.tensor_tensor(out=m_.t[:, j, 0:n], in0=t1.t[:, 0:n], in1=t2.t[:, 0:n], op=ALU.add), [t1, t2], [m_])
                for j in range(KD):
                    js = slice(j * 128, (j + 1) * 128)
                    p = kb.psum()
                    for k in range(KD):
                        kb.op("pe", mm(p.t[:, 0:n], wout_b.t[:, k, js], m_.t[:, k, 0:n], k == 0, k == KD - 1), [wout_b, m_], [p])
                    tr_ = tfs[tc % 4]; tc += 1
                    kb.op("act", lambda e, p=p, j=j, s_=s_, tr_=tr_: e.activation(out=tr_.t[:, 0:n], in_=p.t[:, 0:n], func=AF.Identity, scale=mod(l, 2, j, s_)), [p, modv], [tr_])
                    kb.op("dve", lambda e, X=X, j=j, tr_=tr_: e.tensor_tensor(out=X.t[:, j, 0:n], in0=X.t[:, j, 0:n], in1=tr_.t[:, 0:n], op=ALU.add), [tr_], [X])
                kb.dma("pool", XT[:, t0:t0 + n].rearrange("(k p) t -> p k t", p=128), X.t[:, :, 0:n], [X], [dXT[ci]], X)
            kb.barrier()

        if stop == "P3":
            return nc
        if moe and not os.environ.get("DENSE_MOE"):
            routed_moe(l, last)
            continue
        clist = list(range(len(cfg.chunks)))
        if last:
            clist = clist[1:]
        groups = [clist[i:i + 2] for i in range(0, len(clist), 2)]
        with contextlib.ExitStack() as st:
            h2 = kb.tile(st, [128, KD, 1024], BF16, "h2")
            if moe:
                acc = kb.tile(st, [128, KD, 1024], F32, "acc")
                gwbc = kb.tile(st, [128, E, 1024], BF16, "gwbc")
                wr_t = kb.tile(st, [128, KD, 8], F32, "wr")
                gwT = kb.tile(st, [8, 1024], F32, "gwT")
                sm = [kb.tile(st, [128, 8], F32, "sm%d" % i) for i in range(6)]
                s1 = [kb.tile(st, [128, 1], F32, "s1%d" % i) for i in range(5)]
                kb.dma("sp", wr_t.t[:, :, :], wr[l // 2].rearrange("p (k e) -> p k e", e=8), [dummy], [wr_t], wr_t)
            wc = 0
            xc = 0
            for grp in groups:
                cols = []
                c0 = 0
                with contextlib.ExitStack() as st2:
                    nt4 = norm_tiles(st2)
                    h2f = kb.tile(st2, [128, KD, 512], F32, "h2f") if moe else None
                    for ci in grp:
                        t0, n = cfg.chunks[ci]
                        cols.append((ci, t0, n, c0))
                        norm_chunk(nt4, XT, l, ci, V_NFFN, 3, 4, h2, c0, out_f32=h2f)
                        if moe:
                            for tt in range(n // 128):
                                p = kb.psum()
                                for k in range(KD):
                                    kb.op("pe", mm(p.t[:, 0:8], h2f.t[:, k, tt * 128:(tt + 1) * 128], wr_t.t[:, k, :], k == 0, k == KD - 1), [h2f, wr_t], [p])
                                Lg, m1e, L2, selm, ex, gw = sm
                                m1, m2, nm1, ss, rs1 = s1
                                kb.op("dve", lambda e: e.tensor_copy(out=Lg.t[:, :], in_=p.t[:, 0:8]), [p], [Lg])
                                kb.op("dve", lambda e: e.reduce_max(out=m1.t[:, :], in_=Lg.t[:, :], axis=AX.X), [Lg], [m1])
                                kb.op("dve", lambda e: e.tensor_scalar(out=m1e.t[:, :], in0=Lg.t[:, :], scalar1=m1.t[:, 0:1], scalar2=-1e30, op0=ALU.is_equal, op1=ALU.mult), [Lg, m1], [m1e])
                                kb.op("dve", lambda e: e.tensor_tensor(out=L2.t[:, :], in0=Lg.t[:, :], in1=m1e.t[:, :], op=ALU.add), [Lg, m1e], [L2])
                                kb.op("dve", lambda e: e.reduce_max(out=m2.t[:, :], in_=L2.t[:, :], axis=AX.X), [L2], [m2])
                                kb.op("dve", lambda e: e.tensor_scalar(out=selm.t[:, :], in0=Lg.t[:, :], scalar1=m2.t[:, 0:1], scalar2=None, op0=ALU.is_ge), [Lg, m2], [selm])
                                kb.op("dve", lambda e: e.tensor_scalar(out=nm1.t[:, :], in0=m1.t[:, :], scalar1=-1.0, scalar2=None, op0=ALU.mult), [m1], [nm1])
                                kb.op("act", lambda e: e.activation(out=ex.t[:, :], in_=Lg.t[:, :], func=AF.Exp, bias=nm1.t[:, 0:1], scale=1.0), [Lg, nm1], [ex])
                                kb.op("dve", lambda e: e.tensor_tensor(out=ex.t[:, :], in0=ex.t[:, :], in1=selm.t[:, :], op=ALU.mult), [selm], [ex])
                                kb.op("dve", lambda e: e.reduce_sum(out=ss.t[:, :], in_=ex.t[:, :], axis=AX.X), [ex], [ss])
                                kb.op("dve", lambda e: e.reciprocal(out=rs1.t[:, :], in_=ss.t[:, :]), [ss], [rs1])
                                kb.op("dve", lambda e: e.tensor_scalar(out=gw.t[:, :], in0=ex.t[:, :], scalar1=rs1.t[:, 0:1], scalar2=None, op0=ALU.mult), [ex, rs1], [gw])
                                p2 = kb.psum()
                                kb.op("pe", mm(p2.t[0:8, 0:128], gw.t[:, :], id128f), [gw, cf], [p2])
                                kb.op("dve", lambda e: e.tensor_copy(out=gwT.t[:, c0 + tt * 128:c0 + (tt + 1) * 128], in_=p2.t[0:8, 0:128]), [p2], [gwT])
                            for e_ in range(E):
                                p = kb.psum()
                                kb.op("pe", mm(p.t[:, 0:n], sel_e(e_), gwT.t[:, c0:c0 + n]), [cf, gwT], [p])
                                evac(gwbc.t[:, e_, c0:c0 + n], gwbc, p, p.t[:, 0:n])
                        c0 += n
                    kb.barrier()
                with contextlib.ExitStack() as st2:
                    act = kb.tile(st2, [128, KF, 1024], BF16, "act")
                    gus = [kb.tile(st2, [128, KD * 256], F32, "gus") for _ in range(3)]
                    gub = [kb.tile(st2, [128, KD, 256], BF16, "gub") for _ in range(3)]
                    dns = [kb.tile(st2, [128, KF * 128], F32, "dns") for _ in range(3)]
                    dnb = [kb.tile(st2, [128, KF, 128], BF16, "dnb") for _ in range(3)]
                    xts = [kb.tile(st2, [128, 512], F32, "x4") for _ in range(3)]
                    sl = [kb.tile(st2, [128, 512], BF16, "silu") for _ in range(3)]
                    t5 = [kb.tile(st2, [128, 512], BF16, "t5") for _ in range(2)]
                    for e_ in range(E if moe else 1):
                        gsrc = wgu[l // 2]
                        dsrc = wdn[l // 2]
                        for f in range(KF):
                            sg = gus[wc % 3]; gb = gub[wc % 3]; wc += 1
                            kb.dma("sp", sg.t[:, :], gsrc[f], [dummy], [sg], sg)
                            cast(gb.t[:, :, :], gb, sg.t[:, :].rearrange("p (k c) -> p k c", k=KD), sg)
                            for (ci, t0, n, c0) in cols:
                                pg = kb.psum(); pu = kb.psum()
                                for k in range(KD):
                                    kb.op("pe", mm(pg.t[:, 0:n], gb.t[:, k, 0:128], h2.t[:, k, c0:c0 + n], k == 0, k == KD - 1), [gb, h2], [pg])
                                for k in range(KD):
                                    kb.op("pe", mm(pu.t[:, 0:n], gb.t[:, k, 128:256], h2.t[:, k, c0:c0 + n], k == 0, k == KD - 1), [gb, h2], [pu])
                                s_ = sl[xc % 3]; xc += 1
                                kb.op("act", lambda e: e.activation(out=s_.t[:, 0:n], in_=pg.t[:, 0:n], func=AF.Silu), [pg], [s_])
                                if moe:
                                    t_ = t5[xc % 2]
                                    kb.op("dve", lambda e: e.tensor_tensor(out=t_.t[:, 0:n], in0=pu.t[:, 0:n], in1=s_.t[:, 0:n], op=ALU.mult), [pu, s_], [t_])
                                    kb.op("pool", lambda e: e.tensor_tensor(out=act.t[:, f, c0:c0 + n], in0=t_.t[:, 0:n], in1=gwbc.t[:, e_, c0:c0 + n], op=ALU.mult), [t_, gwbc], [act])
                                else:
                                    kb.op("dve", lambda e: e.tensor_tensor(out=act.t[:, f, c0:c0 + n], in0=pu.t[:, 0:n], in1=s_.t[:, 0:n], op=ALU.mult), [pu, s_], [act])
                        for j in range(KD):
                            sg = dns[wc % 3]; db = dnb[wc % 3]; wc += 1
                            kb.dma("sp", sg.t[:, :], dsrc[j], [dummy], [sg], sg)
                            cast(db.t[:, :, :], db, sg.t[:, :].rearrange("p (k c) -> p k c", k=KF), sg)
                            for (ci, t0, n, c0) in cols:
                                s_i = 1 if ci == 0 else 0
                                p = kb.psum()
                                for k in range(KF):
                                    kb.op("pe", mm(p.t[:, 0:n], db.t[:, k, :], act.t[:, k, c0:c0 + n], k == 0, k == KF - 1), [db, act], [p])
                                fin = (not moe) or e_ == E - 1
                                if moe and e_ == 0:
                                    evac(acc.t[:, j, c0:c0 + n], acc, p, p.t[:, 0:n])
                                elif moe:
                                    kb.op("dve", lambda e: e.tensor_tensor(out=acc.t[:, j, c0:c0 + n], in0=p.t[:, 0:n], in1=acc.t[:, j, c0:c0 + n], op=ALU.add), [p], [acc])
                                if fin:
                                    x_ = xts[xc % 3]; xc += 1
                                    js = slice(j * 128, (j + 1) * 128)
                                    kb.dma("sp", x_.t[:, 0:n], XT[js, t0:t0 + n], [dummy], [x_], x_)
                                    if moe:
                                        kb.op("dve", lambda e: e.scalar_tensor_tensor(out=x_.t[:, 0:n], in0=acc.t[:, j, c0:c0 + n], scalar=mod(l, 5, j, s_i), in1=x_.t[:, 0:n], op0=ALU.mult, op1=ALU.add), [acc, modv], [x_])
                                    else:
                                        tq_ = t5[xc % 2]
                                        tq32 = xts[(xc + 1) % 3]
                                        kb.op("act", lambda e: e.activation(out=tq32.t[:, 0:n], in_=p.t[:, 0:n], func=AF.Identity, scale=mod(l, 5, j, s_i)), [p, modv], [tq32])
                                        kb.op("dve", lambda e: e.tensor_tensor(out=x_.t[:, 0:n], in0=x_.t[:, 0:n], in1=tq32.t[:, 0:n], op=ALU.add), [tq32], [x_])
                                    kb.dma("pool", XT[js, t0:t0 + n], x_.t[:, 0:n], [x_], [Buf("snk")], x_)
                    kb.barrier()

    with contextlib.ExitStack() as st:
        xt = [kb.tile(st, [128, KD, 512], F32, "xf") for _ in range(2)]
        sq = [kb.tile(st, [128, KD, 512], BF16, "sqf") for _ in range(2)]
        rs = [kb.tile(st, [128, 512], F32, "rsf") for _ in range(2)]
        for qi, ci in enumerate(range(1, len(cfg.chunks))):
            t0, n = cfg.chunks[ci]
            x_ = xt[qi % 2]; s_ = sq[qi % 2]; r_ = rs[qi % 2]
            kb.dma("sp", x_.t[:, :, 0:n], XT[:, t0:t0 + n].rearrange("(k p) t -> p k t", p=128), [dXT[ci]], [x_], x_)
            p = kb.psum()
            for k in range(KD):
                kb.op("act", lambda e, k=k, x_=x_, s_=s_: e.activation(out=s_.t[:, k, 0:n], in_=x_.t[:, k, 0:n], func=AF.Square), [x_], [s_])
                kb.op("pe", mm(p.t[:, 0:n], ones128, s_.t[:, k, 0:n], k == 0, k == KD - 1), [s_, cb], [p])
            rstd_from(p, n, D, r_)
            for k in range(KD):
                kb.op("dve", lambda e, k=k, x_=x_, r_=r_: e.scalar_tensor_tensor(out=x_.t[:, k, 0:n], in0=x_.t[:, k, 0:n], scalar=v_nfinal[:, k:k + 1], in1=r_.t[:, 0:n], op0=ALU.mult, op1=ALU.mult), [r_, vc], [x_])
            kb.dma("pool", outT[:, t0 - C:t0 - C + n].rearrange("(k p) t -> p k t", p=128), x_.t[:, :, 0:n], [x_], [dXT[ci]], x_)
        kb.barrier()
    return nc


def _tile_cols(w, KD):
    D = w.shape[0]
    return np.ascontiguousarray(w.reshape(KD, 128, w.shape[1]).transpose(1, 0, 2).reshape(128, -1))


def host_prep(cfg, inp):
    D, C, S, L, F, E, T, KD, KF = cfg.D, cfg.C, cfg.S, cfg.L, cfg.F, cfg.E, cfg.T, cfg.KD, cfg.KF
    f32 = np.float32
    sh = {}
    NVL = 2 * KD + 48 + 6 + 3

    def fm(v):
        return np.asarray(v, f32).reshape(-1, 128).T

    def rot64(g):
        return np.concatenate([g[32:], g[:32]])
    w_in = np.asarray(inp["w_in"], f32)
    KVW = 928
    o_kna, o_vna, o_kg, o_vg, o_ckv, o_kr = 0, 256, 512, 640, 768, 896
    o_qna, o_qg, o_cq, o_gate = KVW, KVW + 256, KVW + 768, KVW + 1024

    def rotcols(base, nheads, d):
        idx = []
        for h in range(nheads):
            idx += list(range(base + h * d + d // 2, base + (h + 1) * d)) + list(range(base + h * d, base + h * d + d // 2))
        return idx
    w1 = np.zeros((L, cfg.NT1, 128, KD * 128), f32)
    wvv = np.zeros((L, 128, KD * 384), f32)
    for l in range(L):
        W = w_in[l]
        tiles = []
        tiles += [W[:, o_kna:o_kna + 128], W[:, o_kna + 128:o_kna + 256]]
        tiles += [W[:, o_kg:o_kg + 128], W[:, rotcols(o_kg, 2, 64)]]
        tiles += [W[:, o_ckv:o_ckv + 128]]
        t5 = np.zeros((D, 128), f32); t5[:, 64:96] = W[:, o_kr:o_kr + 32]
        t6 = np.zeros((D, 128), f32); t6[:, 64:96] = W[:, rotcols(o_kr, 1, 32)]
        tiles += [t5, t6]
        tiles += [W[:, o_qna:o_qna + 128], W[:, o_qna + 128:o_qna + 256]]
        tiles += [W[:, o_qg + j * 128:o_qg + (j + 1) * 128] for j in range(4)]
        rc = rotcols(o_qg, 8, 64)
        tiles += [W[:, rc[j * 128:(j + 1) * 128]] for j in range(4)]
        tiles += [W[:, o_cq:o_cq + 128], W[:, o_cq + 128:o_cq + 256]]
        tiles += [W[:, o_gate + j * 128:o_gate + (j + 1) * 128] for j in range(24)]
        for i, t in enumerate(tiles):
            w1[l, i] = _tile_cols(t, KD)
        Wv = np.concatenate([W[:, o_vna:o_vna + 256], W[:, o_vg:o_vg + 128]], axis=1)
        wvv[l] = _tile_cols(Wv, KD)
    sh["w1"] = w1
    sh["wv"] = wvv
    w_ada = np.asarray(inp["w_ada"], f32)
    sh["w_ada"] = np.ascontiguousarray(w_ada.reshape(L, KD, 128, 48, 128).transpose(0, 3, 2, 1, 4).reshape(L, 48, 128, KD * 128))
    w_uq = np.asarray(inp["w_uq"], f32)
    sh["wuq"] = np.ascontiguousarray(w_uq.reshape(L, 2, 128, 384).transpose(0, 2, 1, 3).reshape(L, 128, 768))
    wuqr = np.zeros_like(w_uq)
    for h in range(4):
        b = h * 96 + 64
        wuqr[:, :, b:b + 16] = w_uq[:, :, b + 16:b + 32]
        wuqr[:, :, b + 16:b + 32] = w_uq[:, :, b:b + 16]
    sh["wuqr"] = np.ascontiguousarray(wuqr.reshape(L, 2, 128, 384).transpose(0, 2, 1, 3).reshape(L, 128, 768))
    w_ukv = np.asarray(inp["w_ukv"], f32).reshape(L, 128, 4, 128)
    sh["wukvk"] = np.ascontiguousarray(w_ukv[:, :, :, :64].reshape(L, 128, 256))
    sh["wukvv"] = np.ascontiguousarray(w_ukv[:, :, :, 64:].reshape(L, 128, 256))
    rpb = np.asarray(inp["rpb"], f32)
    jq = np.arange(64)[:, None]; jk = np.arange(64)[None, :]
    dc = np.clip(jk - jq, -15, 15) + 15
    g = rpb[:, :, :, dc]
    sh["rpbT"] = np.ascontiguousarray(g.transpose(0, 3, 1, 2, 4).reshape(L, 64, 3840))
    wo_all = np.concatenate([np.asarray(inp["w_o_na"], f32), np.asarray(inp["w_o_gqa"], f32), np.asarray(inp["w_o_mla"], f32)], axis=1)
    sh["wo"] = np.ascontiguousarray(wo_all.reshape(L, 8, 128, D).transpose(0, 2, 1, 3).reshape(L, 128, 8 * D))
    sh["wout"] = np.ascontiguousarray(np.asarray(inp["w_out"], f32).reshape(L, KD, 128, D).transpose(0, 2, 1, 3).reshape(L, 128, KD * D))

    def gu_layout(w):
        lead = w.shape[:-2]
        gte = w[..., :F].reshape(*lead, KD, 128, KF, 128)
        up = w[..., F:].reshape(*lead, KD, 128, KF, 128)
        cat = np.stack([gte, up], axis=-2)
        nl = len(lead)
        perm = list(range(nl)) + [nl + 2, nl + 1, nl + 0, nl + 3, nl + 4]
        return np.ascontiguousarray(cat.transpose(perm).reshape(*lead, KF, 128, KD * 256))

    def dn_layout(w):
        lead = w.shape[:-2]
        nl = len(lead)
        a = w.reshape(*lead, KF, 128, KD, 128)
        perm = list(range(nl)) + [nl + 2, nl + 1, nl + 0, nl + 3]
        return np.ascontiguousarray(a.transpose(perm).reshape(*lead, KD, 128, KF * 128))
    sh["wgu"] = gu_layout(np.asarray(inp["w_ffn_gu"], f32))
    sh["wdn"] = dn_layout(np.asarray(inp["w_ffn_dn"], f32))
    if cfg.NM > 0:
        sh["wgum"] = gu_layout(np.asarray(inp["w_moe_gu"], f32)).reshape(cfg.NM * E * KF * 128, KD * 256)
        sh["wdnm"] = np.ascontiguousarray(np.asarray(inp["w_moe_dn"], f32).reshape(cfg.NM * E * F, D))
        sh["wr"] = np.ascontiguousarray(np.asarray(inp["w_router"], f32).reshape(cfg.NM, KD, 128, 8).transpose(0, 2, 1, 3).reshape(cfg.NM, 128, KD * 8))
    else:
        sh["wgum"] = np.zeros((E * KF * 128, KD * 256), f32)
        sh["wdnm"] = np.zeros((E * F, D), f32)
        sh["wr"] = np.zeros((1, 128, KD * 8), f32)
    pos = np.arange(S)
    rows = (pos // 64).astype(f32); cols = (pos % 64).astype(f32)

    def tables(half):
        nf = half // 2
        inv = np.power(10000.0, -np.arange(nf, dtype=f32) / nf).astype(f32)
        ang = np.concatenate([rows[:, None] * inv, cols[:, None] * inv], axis=-1)
        cos = np.concatenate([np.ones((C, half), f32), np.cos(ang)], 0).T
        sin = np.concatenate([np.zeros((C, half), f32), np.sin(ang)], 0).T
        return cos, sin
    cg, sg = tables(32)
    cosg = np.concatenate([cg, cg, cg, cg], 0)
    sing = np.concatenate([-sg, sg, -sg, sg], 0)
    cm, sm_ = tables(16)
    cosm = np.zeros((128, T), f32); sinm = np.zeros((128, T), f32)
    cosm[64:96] = np.concatenate([cm, cm], 0)
    sinm[64:96] = np.concatenate([-sm_, sm_], 0)
    col0 = np.clip(np.arange(64) - 8, 0, 48)
    inwin = (jk >= col0[:, None]) & (jk < col0[:, None] + 16)
    neg = np.where(inwin, 0.0, -30000.0).astype(f32)
    negm = np.tile(neg[:, None, :], (1, 60, 1)).reshape(64, 3840)
    ones = np.ones((128, 128), f32)
    bd = np.zeros((128, 128), f32); bd[:64, :64] = 1; bd[64:, 64:] = 1
    idb = np.zeros((128, 64), f32); idb[:64] = np.eye(64)
    ustr = np.triu(np.ones((128, 128), f32), 1)
    sh["cbf"] = np.ascontiguousarray(np.concatenate([ones, bd, idb, ustr, np.eye(128, dtype=f32)], 1).astype(BF))
    sh["ropet"] = np.ascontiguousarray(np.stack([cosg, sing, cosm, sinm], 1).astype(BF))
    sh["negm"] = np.ascontiguousarray(negm.astype(BF))
    sel64 = np.zeros((128, 128), f32); sel64[64, :64] = 1
    sele = np.zeros((128, E, 128), f32)
    for e in range(E):
        sele[e, e, :] = 1
    sh["cf32"] = np.ascontiguousarray(np.concatenate([np.eye(128, dtype=f32), sel64, sele.reshape(128, E * 128), np.full((128, 1), 1e-6, f32),
        np.tile((np.arange(10, dtype=f32) * 512)[None, :], (128, 1)), np.tile(np.arange((2 * T + E * 511) // 512, dtype=f32)[None, :], (128, 1)),
        (np.arange(KF, dtype=f32)[None, :] * 128 + np.arange(128, dtype=f32)[:, None])], 1))
    NV = L * NVL + 3 * KD
    per = []
    xin = np.asarray(inp["x"], f32); ctx = np.asarray(inp["ctx"], f32); c = np.asarray(inp["c"], f32)
    B = xin.shape[0]
    vbase = np.zeros((128, NV), f32)
    for l in range(L):
        o = l * NVL
        vbase[:, o:o + KD] = fm(inp["norm_mix"][l])
        vbase[:, o + KD:o + 2 * KD] = fm(inp["norm_ffn"][l])
        vbase[:, o + 2 * KD:o + 2 * KD + 48] = fm(inp["b_ada"][l])
        qg = np.asarray(inp["q_norm_gqa"][l], f32); kg = np.asarray(inp["k_norm_gqa"][l], f32)
        vbase[:, o + 2 * KD + 48] = np.concatenate([qg, qg])
        vbase[:, o + 2 * KD + 49] = np.concatenate([rot64(qg), rot64(qg)])
        vbase[:, o + 2 * KD + 50] = np.concatenate([kg, kg])
        vbase[:, o + 2 * KD + 51] = np.concatenate([rot64(kg), rot64(kg)])
        vbase[:, o + 2 * KD + 52:o + 2 * KD + 54] = fm(inp["q_lora_norm"][l])
        vbase[:, o + 2 * KD + 54] = np.asarray(inp["kv_lora_norm"][l], f32)
    go = L * NVL
    vbase[:, go:go + KD] = fm(inp["norm_final"])
    vbase[:, go + 2 * KD:go + 3 * KD] = fm(inp["c_ctx"])
    for b in range(B):
        m = dict(sh)
        v = vbase.copy()
        v[:, go + KD:go + 2 * KD] = fm(c[b])
        m["vecs"] = v
        m["xT"] = np.ascontiguousarray(np.concatenate([ctx[b], xin[b]], 0).T)
        per.append(m)
    return per


_CACHE = {}


def kernel(**inputs):
    cfg = Cfg()
    if "nc" not in _CACHE:
        _CACHE["nc"] = build(cfg)
    nc = _CACHE["nc"]
    in_maps = host_prep(cfg, inputs)
    res = run_bass_kernel_spmd(nc, in_maps, core_ids=list(range(len(in_maps))))
    out = np.stack([np.ascontiguousarray(r["outT"].T) for r in res.results], 0)
    return out.astype(np.float32)
```

```python
import contextlib
import os
import numpy as np
import ml_dtypes
import concourse.bass as bass
import concourse.mybir as mybir
from concourse.bass_utils import run_bass_kernel_spmd

F32, BF16 = mybir.dt.float32, mybir.dt.bfloat16
AF = mybir.ActivationFunctionType
ALU = mybir.AluOpType
AX = mybir.AxisListType
BF = ml_dtypes.bfloat16


class Cfg:
    def __init__(s, D=1024, C=256, S=4096, L=4, F=2816, E=8):
        s.D, s.C, s.S, s.L, s.F, s.E = D, C, S, L, F, E
        s.T = C + S
        s.KD = D // 128
        s.R = S // 64
        s.KF = F // 128
        s.chunks = [(0, C)] + [(C + i * 512, 512) for i in range(S // 512)]
        s.NKT = s.T // 128
        s.CT = C // 128
        s.ND = (L + 1) // 2
        s.NM = L // 2
        s.NT1 = 19 + 24


class Buf:
    __slots__ = ("name", "w", "r", "dsem", "dram")

    def __init__(s, name, dram=False):
        s.name = name
        s.w = {}
        s.r = {}
        s.dsem = None
        s.dram = dram


class Tl:
    def __init__(s, t, name):
        s.t = t
        s.b = Buf(name)


class KB:
    NDMASEM = 90

    def __init__(self, nc):
        self.nc = nc
        self.eng = {"pe": nc.tensor, "act": nc.scalar, "dve": nc.vector, "pool": nc.gpsimd, "sp": nc.sync}
        self.sems = []
        self.last = []
        self.semidx = {}
        for e in ("pe", "act", "dve", "pool"):
            self.semidx[e] = self._newsem("s_" + e)
        self.cnt = {e: 0 for e in self.semidx}
        self.waited = {e: {} for e in self.eng}
        self.dsems = []
        self.nd = 0
        self.stack = contextlib.ExitStack()
        self.ps = []
        for i in range(8):
            t = self.stack.enter_context(nc.psum_tensor("ps%d" % i, [128, 512], F32))
            self.ps.append(Tl(t, "ps%d" % i))
        self.psi = 0
        self.ntile = 0

    def _newsem(self, name):
        s = self.nc.semaphore(name).__enter__()
        self.sems.append(s)
        self.last.append(0)
        return len(self.sems) - 1

    def tile(self, st, shape, dt, name=None):
        self.ntile += 1
        name = (name or "t") + "_%d" % self.ntile
        t = st.enter_context(self.nc.sbuf_tensor(name, list(shape), dt))
        return Tl(t, name)

    def psum(self, pool=None):
        pool = pool or (0, 8)
        lo, n = pool
        key = ("psi", lo, n)
        i = getattr(self, "_rr", {}).get(key, 0)
        if not hasattr(self, "_rr"):
            self._rr = {}
        self._rr[key] = (i + 1) % n
        return self.ps[lo + i]

    def _wait(self, e, si, v):
        wd = self.waited[e]
        if e == "pe" and si == self.semidx["pe"]:
            return
        if wd.get(si, 0) < v:
            self.eng[e].wait_ge(self.sems[si], v)
            wd[si] = v

    def _deps(self, e, reads, writes):
        need = {}
        for b in reads:
            for si, v in b.w.items():
                if need.get(si, 0) < v:
                    need[si] = v
        for b in writes:
            if not b.dram:
                for si, v in b.w.items():
                    if need.get(si, 0) < v:
                        need[si] = v
            for si, v in b.r.items():
                if need.get(si, 0) < v:
                    need[si] = v
        for si, v in need.items():
            self._wait(e, si, v)

    def _mark(self, tok, reads, writes):
        si, v = tok
        for b in writes:
            if b.dram:
                if b.w.get(si, 0) < v:
                    b.w[si] = v
            else:
                b.w = {si: v}
            b.r = {}
        for b in reads:
            if any(b is w for w in writes):
                continue
            b.r[si] = v

    def op(self, e, fn, reads=(), writes=()):
        reads = [x.b if isinstance(x, Tl) else x for x in reads]
        writes = [x.b if isinstance(x, Tl) else x for x in writes]
        self._deps(e, reads, writes)
        ins = fn(self.eng[e])
        self.cnt[e] += 1
        si = self.semidx[e]
        ins.then_inc(self.sems[si], 1)
        self.last[si] = self.cnt[e]
        self._mark((si, self.cnt[e]), reads, writes)

    def dma(self, q, out, in_, reads, writes, sb):
        reads = [x.b if isinstance(x, Tl) else x for x in reads]
        writes = [x.b if isinstance(x, Tl) else x for x in writes]
        sb = sb.b if isinstance(sb, Tl) else sb
        if sb.dsem is None:
            if len(self.dsems) < self.NDMASEM:
                self.dsems.append(self._newsem("d%d" % len(self.dsems)))
                sb.dsem = self.dsems[-1]
            else:
                sb.dsem = self.dsems[self.nd % self.NDMASEM]
            self.nd += 1
        si = sb.dsem
        self._deps(q, reads, writes)
        if self.last[si] > 0:
            self._wait(q, si, self.last[si])
        ins = self.eng[q].dma_start(out=out, in_=in_)
        self.last[si] += 16
        ins.then_inc(self.sems[si], 16)
        self._mark((si, self.last[si]), reads, writes)

    def idma(self, out, out_off, in_, in_off, reads, writes, sb):
        reads = [x.b if isinstance(x, Tl) else x for x in reads]
        writes = [x.b if isinstance(x, Tl) else x for x in writes]
        sb = sb.b if isinstance(sb, Tl) else sb
        if sb.dsem is None:
            if len(self.dsems) < self.NDMASEM:
                self.dsems.append(self._newsem("d%d" % len(self.dsems)))
                sb.dsem = self.dsems[-1]
            else:
                sb.dsem = self.dsems[self.nd % self.NDMASEM]
            self.nd += 1
        si = sb.dsem
        self._deps("pool", reads, writes)
        if self.last[si] > 0:
            self._wait("pool", si, self.last[si])
        ins = self.eng["pool"].indirect_dma_start(out=out, out_offset=out_off, in_=in_, in_offset=in_off)
        self.last[si] += 16
        ins.then_inc(self.sems[si], 16)
        self._mark((si, self.last[si]), reads, writes)

    def barrier(self):
        for e in self.eng:
            for si, v in enumerate(self.last):
                if v > 0:
                    self._wait(e, si, v)


def mm(out, lhsT, rhs, start=True, stop=True):
    return lambda e: e.matmul(out, lhsT, rhs, start=start, stop=stop)


def build(cfg, debug=False, stop=None):
    nc = bass.Bass("TRN2", target_bir_lowering=False)
    D, C, S, L, F, E, T, KD, KF = cfg.D, cfg.C, cfg.S, cfg.L, cfg.F, cfg.E, cfg.T, cfg.KD, cfg.KF
    NKT, CT, R = cfg.NKT, cfg.CT, cfg.R

    def din(name, shape, dt=F32):
        return nc.dram_tensor(name, list(shape), dt, kind="ExternalInput").ap()

    def dscr(name, shape, dt=BF16):
        return nc.dram_tensor(name, list(shape), dt, kind=("ExternalOutput" if debug else "Internal")).ap()

    xT_in = din("xT", [D, T])
    NVL = 2 * KD + 48 + 6 + 3
    NV = L * NVL + 3 * KD
    vecs = din("vecs", [128, NV])
    w_ada = din("w_ada", [L, 48, 128, KD * 128])
    w1 = din("w1", [L, cfg.NT1, 128, KD * 128])
    wv = din("wv", [L, 128, KD * 384])
    wuq = din("wuq", [L, 128, 2 * 384])
    wuqr = din("wuqr", [L, 128, 2 * 384])
    wukvk = din("wukvk", [L, 128, 256])
    wukvv = din("wukvv", [L, 128, 256])
    rpbT = din("rpbT", [L, 64, 3840])
    wo = din("wo", [L, 128, 8 * D])
    wout = din("wout", [L, 128, KD * D])
    wgu = din("wgu", [max(cfg.ND, 1), KF, 128, KD * 256])
    wdn = din("wdn", [max(cfg.ND, 1), KD, 128, KF * 128])
    NMx = max(cfg.NM, 1)
    wgum = din("wgum", [NMx * E * KF * 128, KD * 256])
    wdnm = din("wdnm", [NMx * E * F, D])
    wr = din("wr", [max(cfg.NM, 1), 128, KD * 8])
    NCB = 128 + 128 + 64 + 128 + 128
    cbf = din("cbf", [128, NCB], BF16)
    ropet = din("ropet", [128, 4, T], BF16)
    negm = din("negm", [64, 3840], BF16)
    BS = 512
    NTHR = 10
    NBMAX = (2 * T + E * (BS - 1)) // BS
    NCF = 128 + 128 + E * 128 + 1 + NTHR + NBMAX + KF
    cf32 = din("cf32", [128, NCF])
    outT = nc.dram_tensor("outT", [D, S], F32, kind="ExternalOutput").ap()

    XT = nc.dram_tensor("XTs", [D, T], F32, kind="Internal").ap()
    KnaT = dscr("KnaT", [256, T])
    QnaT = dscr("QnaT", [256, T])
    KgT = dscr("KgT", [128, T])
    QgT = dscr("QgT", [512, T])
    KmT = dscr("KmT", [4 * 96, T])
    QmT = dscr("QmT", [4 * 96, T])
    Vna = dscr("Vna", [T, 4 * 65])
    Vg = dscr("Vg", [T, 2 * 65])
    Vm = dscr("Vm", [T, 4 * 65])
    GT = dscr("GT", [3 * D, T])
    YT = dscr("YT", [D, T])
    XsD = nc.dram_tensor("Xs", [NBMAX * BS, D], BF16, kind="Internal").ap()
    YsD = nc.dram_tensor("Ys", [NBMAX * BS, D], F32, kind="Internal").ap()

    kb = KB(nc)
    dXT = [Buf("XT%d" % i, dram=True) for i in range(len(cfg.chunks))]
    dQK = {n: Buf(n, dram=True) for n in ("KnaT", "QnaT", "KgT", "QgT", "KmT", "QmT", "Vna", "Vg", "Vm", "GT", "YT")}

    gst = contextlib.ExitStack()
    cb = kb.tile(gst, [128, NCB], BF16, "cbf")
    cf = kb.tile(gst, [128, NCF], F32, "cf32")
    vc = kb.tile(gst, [128, NV], F32, "vecs")
    modv = kb.tile(gst, [128, L * 48 * 2], F32, "modv")
    dummy = Buf("dram_in")
    kb.dma("sp", cb.t[:, :], cbf[:, :], [dummy], [cb], cb)
    kb.dma("sp", cf.t[:, :], cf32[:, :], [dummy], [cf], cf)
    kb.dma("sp", vc.t[:, :], vecs[:, :], [dummy], [vc], vc)
    o = 0
    ones128 = cb.t[:, o:o + 128]; o += 128
    bd64 = cb.t[:, o:o + 128]; o += 128
    id64b = cb.t[:, o:o + 64]; o += 64
    ustrict = cb.t[:, o:o + 128]; o += 128
    identb = cb.t[:, o:o + 128]; o += 128
    id128f = cf.t[:, 0:128]
    sel64 = cf.t[0:65, 128:256]
    o2 = 128 + 128 + E * 128
    epsc = cf.t[:, o2:o2 + 1]
    thr_c = cf.t[:, o2 + 1:o2 + 1 + NTHR]
    jrow_c = cf.t[:, o2 + 1 + NTHR:o2 + 1 + NTHR + NBMAX]
    cidx_c = cf.t[:, o2 + 1 + NTHR + NBMAX:o2 + 1 + NTHR + NBMAX + KF]

    def sel_e(e):
        return cf.t[0:8, 256 + e * 128:256 + (e + 1) * 128]

    def vcol(l, j, n=1):
        return vc.t[:, l * NVL + j:l * NVL + j + n]
    V_NMIX, V_NFFN, V_BADA, V_QG, V_QGR, V_KG, V_KGR, V_QL, V_KVL = 0, KD, 2 * KD, 2 * KD + 48, 2 * KD + 49, 2 * KD + 50, 2 * KD + 51, 2 * KD + 52, 2 * KD + 54
    gofs = L * NVL
    v_nfinal = vc.t[:, gofs:gofs + KD]
    v_c = vc.t[:, gofs + KD:gofs + 3 * KD]

    def mod(l, which, j, s):
        i = ((l * 48) + which * KD + j) * 2 + s
        return modv.t[:, i:i + 1]

    with contextlib.ExitStack() as st:
        sc = kb.tile(st, [128, 2 * KD], F32, "silu_c")
        sc2 = kb.tile(st, [128, KD, 2], F32, "silu_c2")
        kb.op("act", lambda e: e.activation(out=sc.t[:, :], in_=v_c, func=AF.Silu), [vc], [sc])
        for s_ in range(2):
            kb.op("dve", lambda e, s_=s_: e.tensor_copy(out=sc2.t[:, :, s_], in_=sc.t[:, s_ * KD:(s_ + 1) * KD]), [sc], [sc2])
        wst = [kb.tile(st, [128, KD * 128], F32, "wada") for _ in range(3)]
        i = 0
        for l in range(L):
            for nt in range(48):
                w_ = wst[i % 3]; i += 1
                kb.dma("sp", w_.t[:, :], w_ada[l, nt], [dummy], [w_], w_)
                p = kb.psum()
                for k in range(KD):
                    kb.op("pe", mm(p.t[:, 0:2], w_.t[:, k * 128:(k + 1) * 128], sc2.t[:, k, :], k == 0, k == KD - 1), [w_, sc2], [p])
                base = ((l * 48) + nt) * 2
                kb.op("dve", lambda e, p=p, base=base, l=l, nt=nt: e.tensor_scalar(out=modv.t[:, base:base + 2], in0=p.t[:, 0:2], scalar1=vcol(l, V_BADA + nt), scalar2=None, op0=ALU.add), [p, vc], [modv])
        kb.barrier()
    if stop == "P0":
        return nc

    def rstd_from(ps_ssq, n, dim, out_t):
        kb.op("act", lambda e: e.activation(out=out_t.t[:, 0:n], in_=ps_ssq.t[:, 0:n], func=AF.Ln, bias=epsc, scale=1.0 / dim), [ps_ssq, cf], [out_t])
        kb.op("act", lambda e: e.activation(out=out_t.t[:, 0:n], in_=out_t.t[:, 0:n], func=AF.Exp, scale=-0.5), [], [out_t])

    def norm_chunk(st_tiles, xsrc, l, ci, which_norm, which_sh, which_sc, out_bf, out_col0, out_f32=None):
        xt, sq, tmp, rs, gs = st_tiles
        t0, n = cfg.chunks[ci]
        s_ = 1 if ci == 0 else 0
        kb.dma("sp", xt.t[:, :, 0:n], xsrc[:, t0:t0 + n].rearrange("(k p) t -> p k t", p=128), [dXT[ci]], [xt], xt)
        for k in range(KD):
            kb.op("dve", lambda e, k=k: e.scalar_tensor_tensor(out=gs.t[:, k:k + 1], in0=mod(l, which_sc, k, s_), scalar=1.0, in1=vcol(l, which_norm + k), op0=ALU.add, op1=ALU.mult), [modv, vc], [gs])
        p = kb.psum()
        for k in range(KD):
            kb.op("act", lambda e, k=k: e.activation(out=sq.t[:, k, 0:n], in_=xt.t[:, k, 0:n], func=AF.Square), [xt], [sq])
            kb.op("pe", mm(p.t[:, 0:n], ones128, sq.t[:, k, 0:n], k == 0, k == KD - 1), [sq, cb], [p])
        rstd_from(p, n, D, rs)
        for k in range(KD):
            kb.op("dve", lambda e, k=k: e.tensor_tensor(out=tmp.t[:, 0:n], in0=xt.t[:, k, 0:n], in1=rs.t[:, 0:n], op=ALU.mult), [xt, rs], [tmp])
            if out_f32 is not None:
                kb.op("act", lambda e, k=k: e.activation(out=out_f32.t[:, k, 0:n], in_=tmp.t[:, 0:n], func=AF.Identity, bias=mod(l, which_sh, k, s_), scale=gs.t[:, k:k + 1]), [tmp, gs, modv], [out_f32])
                kb.op("pool", lambda e, k=k: e.tensor_copy(out=out_bf.t[:, k, out_col0:out_col0 + n], in_=out_f32.t[:, k, 0:n]), [out_f32], [out_bf])
            else:
                kb.op("act", lambda e, k=k: e.activation(out=out_bf.t[:, k, out_col0:out_col0 + n], in_=tmp.t[:, 0:n], func=AF.Identity, bias=mod(l, which_sh, k, s_), scale=gs.t[:, k:k + 1]), [tmp, gs, modv], [out_bf])

    def norm_tiles(st, wmax=512):
        return (kb.tile(st, [128, KD, wmax], F32, "xt"), kb.tile(st, [128, KD, wmax], BF16, "sq"),
                kb.tile(st, [128, wmax], F32, "tmp"), kb.tile(st, [128, wmax], F32, "rs"), kb.tile(st, [128, KD], F32, "gs"))

    def load_w_bf(stg, dst, src_ap, width, eng):
        kb.dma("sp", stg.t[:, 0:width], src_ap, [dummy], [stg], stg)
        return stg

    rr = {"ev": 0, "cast": 0}

    def cast(out_ap, out_buf, in_ap, in_buf):
        rr["cast"] ^= 1
        if rr["cast"]:
            kb.op("dve", lambda e: e.tensor_copy(out=out_ap, in_=in_ap), [in_buf], [out_buf])
        else:
            kb.op("act", lambda e: e.activation(out=out_ap, in_=in_ap, func=AF.Copy), [in_buf], [out_buf])

    def evac(out_ap, out_buf, ps_t, ps_ap, func=None):
        if func is not None:
            kb.op("act", lambda e: e.activation(out=out_ap, in_=ps_ap, func=func), [ps_t], [out_buf])
            return
        rr["ev"] ^= 1
        if rr["ev"]:
            kb.op("act", lambda e: e.activation(out=out_ap, in_=ps_ap, func=AF.Copy), [ps_t], [out_buf])
        else:
            kb.op("dve", lambda e: e.tensor_copy(out=out_ap, in_=ps_ap), [ps_t], [out_buf])

    dXs = Buf("Xs", dram=True)
    dYs = Buf("Ys", dram=True)
    WbfG = nc.dram_tensor("WbfG", [E * KF * 128, KD * 256], BF16, kind="Internal").ap()
    WbfD = nc.dram_tensor("WbfD", [E * F, D], BF16, kind="Internal").ap()
    dWbf = Buf("Wbf", dram=True)

    class Conv:
        def __init__(self):
            self.jobs = []
            self.nl = 0
            self.ncs = 0
            self.cst = None

        def add_layer(self, m_):
            assert self.ncs == len(self.jobs)
            self.jobs = []
            self.nl = 0
            self.ncs = 0
            for e_ in range(E):
                for f in range(KF):
                    r0 = ((m_ * E + e_) * KF + f) * 128
                    d0 = (e_ * KF + f) * 128
                    self.jobs.append((wgum[r0:r0 + 128, :], WbfG[d0:d0 + 128, :], False))
                for k2 in range(KF // 2):
                    r0 = (m_ * E + e_) * F + k2 * 256
                    d0 = e_ * F + k2 * 256
                    self.jobs.append((wdnm[r0:r0 + 256, :].rearrange("(a p) d -> p a d", p=128), WbfD[d0:d0 + 256, :].rearrange("(a p) d -> p a d", p=128), True))

        def active(self):
            return self.ncs < len(self.jobs)

        def attach(self, st):
            self.cst = [kb.tile(st, [128, 2048], F32, "cvs") for _ in range(3)]
            self.cbt = [kb.tile(st, [128, 2048], BF16, "cvb") for _ in range(3)]

        def _load(self):
            i = self.nl
            src, dst, three = self.jobs[i]
            sg = self.cst[i % 3]
            o = sg.t[:, :].rearrange("p (a d) -> p a d", a=2) if three else sg.t[:, :]
            kb.dma("sp", o, src, [dummy], [sg], sg)
            self.nl += 1

        def _cast_store(self):
            i = self.ncs
            src, dst, three = self.jobs[i]
            sg = self.cst[i % 3]; cb_ = self.cbt[i % 3]
            kb.op("pool", lambda e: e.tensor_copy(out=cb_.t[:, :], in_=sg.t[:, :]), [sg], [cb_])
            i_ = cb_.t[:, :].rearrange("p (a d) -> p a d", a=2) if three else cb_.t[:, :]
            kb.dma("pool", dst, i_, [cb_], [dWbf], cb_)
            self.ncs += 1

        def step(self):
            if self.cst is None:
                return
            if self.ncs < self.nl:
                self._cast_store()
            if self.nl < len(self.jobs):
                self._load()

        def drain(self, everything=False):
            if self.cst is None:
                return
            while True:
                if self.ncs < self.nl:
                    self._cast_store()
                elif everything and self.nl < len(self.jobs):
                    self._load()
                else:
                    break
            self.cst = None

    conv = Conv()

    def routed_moe(l, last):
        m_ = l // 2
        clist = list(range(len(cfg.chunks)))
        if last:
            clist = clist[1:]
        tok0 = cfg.chunks[clist[0]][0]
        TL = sum(cfg.chunks[ci][1] for ci in clist)
        NTT = TL // 128
        NB = (2 * TL + E * (BS - 1)) // BS
        SUB = BS // 128
        groups = [clist[i:i + 2] for i in range(0, len(clist), 2)]
        with contextlib.ExitStack() as st:
            selA = kb.tile(st, [128, NTT, 8], F32, "selA")
            sel1A = kb.tile(st, [128, NTT, 8], F32, "sel1A")
            gwA = kb.tile(st, [128, NTT, 8], F32, "gwA")
            posI = kb.tile(st, [128, NTT * 2], mybir.dt.int32, "posI")
            wAB = kb.tile(st, [128, NTT * 2], F32, "wAB")
            gidxI = kb.tile(st, [128, NB * KF], mybir.dt.int32, "gidxI")
            didxI = kb.tile(st, [128, NB * KF], mybir.dt.int32, "didxI")
            with contextlib.ExitStack() as st1:
                h2tm = kb.tile(st1, [128, NTT, D], BF16, "h2tm")
                wr_t = kb.tile(st1, [128, KD, 8], F32, "wr")
                kb.dma("sp", wr_t.t[:, :, :], wr[m_].rearrange("p (k e) -> p k e", e=8), [dummy], [wr_t], wr_t)
                sm = [kb.tile(st1, [128, 8], F32, "sm%d" % i) for i in range(6)]
                s1 = [kb.tile(st1, [128, 1], F32, "s1%d" % i) for i in range(5)]
                zt = kb.tile(st1, [128, SUB, D], BF16, "zeros")
                kb.op("pool", lambda e: e.memset(zt.t[:, :, :], 0.0), [], [zt])
                for j in range(NB):
                    kb.dma("sp", XsD[j * BS:(j + 1) * BS, :].rearrange("(s p) d -> p s d", p=128), zt.t[:, :, :], [zt], [dXs], zt)
                with contextlib.ExitStack() as st2:
                    h2 = kb.tile(st2, [128, KD, 512], BF16, "h2r")
                    h2f = kb.tile(st2, [128, KD, 512], F32, "h2f")
                    nt4 = norm_tiles(st2)
                    for ci in clist:
                        t0, n = cfg.chunks[ci]
                        norm_chunk(nt4, XT, l, ci, V_NFFN, 3, 4, h2, 0, out_f32=h2f)
                        for tl_ in range(n // 128):
                            tt = (t0 - tok0) // 128 + tl_
                            cs = slice(tl_ * 128, (tl_ + 1) * 128)
                            p = kb.psum()
                            for k in range(KD):
                                kb.op("pe", mm(p.t[:, 0:8], h2f.t[:, k, cs], wr_t.t[:, k, :], k == 0, k == KD - 1), [h2f, wr_t], [p])
                            Lg, m1e, L2, selm, ex, gw = sm
                            m1, m2, nm1, ss, rs1 = s1
                            kb.op("dve", lambda e: e.tensor_copy(out=Lg.t[:, :], in_=p.t[:, 0:8]), [p], [Lg])
                            kb.op("dve", lambda e: e.reduce_max(out=m1.t[:, :], in_=Lg.t[:, :], axis=AX.X), [Lg], [m1])
                            kb.op("dve", lambda e: e.tensor_scalar(out=sel1A.t[:, tt, :], in0=Lg.t[:, :], scalar1=m1.t[:, 0:1], scalar2=None, op0=ALU.is_equal), [Lg, m1], [sel1A])
                            kb.op("dve", lambda e: e.tensor_scalar(out=m1e.t[:, :], in0=sel1A.t[:, tt, :], scalar1=-1e30, scalar2=None, op0=ALU.mult), [sel1A], [m1e])
                            kb.op("dve", lambda e: e.tensor_tensor(out=L2.t[:, :], in0=Lg.t[:, :], in1=m1e.t[:, :], op=ALU.add), [Lg, m1e], [L2])
                            kb.op("dve", lambda e: e.reduce_max(out=m2.t[:, :], in_=L2.t[:, :], axis=AX.X), [L2], [m2])
                            kb.op("dve", lambda e: e.tensor_scalar(out=selA.t[:, tt, :], in0=Lg.t[:, :], scalar1=m2.t[:, 0:1], scalar2=None, op0=ALU.is_ge), [Lg, m2], [selA])
                            kb.op("dve", lambda e: e.tensor_scalar(out=nm1.t[:, :], in0=m1.t[:, :], scalar1=-1.0, scalar2=None, op0=ALU.mult), [m1], [nm1])
                            kb.op("act", lambda e: e.activation(out=ex.t[:, :], in_=Lg.t[:, :], func=AF.Exp, bias=nm1.t[:, 0:1], scale=1.0), [Lg, nm1], [ex])
                            kb.op("dve", lambda e: e.tensor_tensor(out=ex.t[:, :], in0=ex.t[:, :], in1=selA.t[:, tt, :], op=ALU.mult), [selA], [ex])
                            kb.op("dve", lambda e: e.reduce_sum(out=ss.t[:, :], in_=ex.t[:, :], axis=AX.X), [ex], [ss])
                            kb.op("dve", lambda e: e.reciprocal(out=rs1.t[:, :], in_=ss.t[:, :]), [ss], [rs1])
                            kb.op("dve", lambda e: e.tensor_scalar(out=gwA.t[:, tt, :], in0=ex.t[:, :], scalar1=rs1.t[:, 0:1], scalar2=None, op0=ALU.mult), [ex, rs1], [gwA])
                            for hf in range(2):
                                p2 = kb.psum()
                                for kk in range(KD // 2):
                                    k = hf * (KD // 2) + kk
                                    kb.op("pe", mm(p2.t[:, kk * 128:(kk + 1) * 128], h2.t[:, k, cs], identb), [h2, cb], [p2])
                                evac(h2tm.t[:, tt, hf * 512:(hf + 1) * 512], h2tm, p2, p2.t[:, 0:512])
                    kb.barrier()
                with contextlib.ExitStack() as st2:
                    selb = kb.tile(st2, [128, NTT, 8], BF16, "selb")
                    cnt = kb.tile(st2, [128, 8], F32, "cnt")
                    nblk = kb.tile(st2, [128, 8], F32, "nblk")
                    pend = kb.tile(st2, [128, 8], F32, "pend")
                    pstart = kb.tile(st2, [128, 8], F32, "pstart")
                    tmpT = kb.tile(st2, [128, NTHR], F32, "tmpT")
                    sT = kb.tile(st2, [128, 1], F32, "sT")
                    bexp = kb.tile(st2, [128, NB], F32, "bexp")
                    tmpB = kb.tile(st2, [128, NB], F32, "tmpB")
                    eoff = kb.tile(st2, [128, NB], F32, "eoff")
                    idxf = kb.tile(st2, [128, NB * KF], F32, "idxf")
                    posf = kb.tile(st2, [128, 8], F32, "posf")
                    sel2 = kb.tile(st2, [128, 8], F32, "sel2")
                    tm8 = kb.tile(st2, [128, 8], F32, "tm8")
                    posAB = kb.tile(st2, [128, NTT * 2], F32, "posAB")
                    kb.op("dve", lambda e: e.tensor_copy(out=selb.t[:, :, :], in_=selA.t[:, :, :]), [selA], [selb])
                    pc = kb.psum()
                    for tt in range(NTT):
                        kb.op("pe", mm(pc.t[:, 0:8], ones128, selb.t[:, tt, :], tt == 0, tt == NTT - 1), [selb, cb], [pc])
                    kb.op("dve", lambda e: e.tensor_copy(out=cnt.t[:, :], in_=pc.t[:, 0:8]), [pc], [cnt])
                    for e_ in range(E):
                        kb.op("dve", lambda e: e.tensor_scalar(out=tmpT.t[:, :], in0=thr_c, scalar1=cnt.t[:, e_:e_ + 1], scalar2=None, op0=ALU.is_ge), [cf, cnt], [tmpT])
                        kb.op("dve", lambda e: e.reduce_sum(out=sT.t[:, :], in_=tmpT.t[:, :], axis=AX.X), [tmpT], [sT])
                        kb.op("dve", lambda e: e.tensor_scalar(out=nblk.t[:, e_:e_ + 1], in0=sT.t[:, :], scalar1=-1.0, scalar2=float(NTHR), op0=ALU.mult, op1=ALU.add), [sT], [nblk])
                    kb.op("dve", lambda e: e.tensor_copy(out=pend.t[:, 0:1], in_=nblk.t[:, 0:1]), [nblk], [pend])
                    for e_ in range(1, E):
                        kb.op("dve", lambda e: e.tensor_tensor(out=pend.t[:, e_:e_ + 1], in0=pend.t[:, e_ - 1:e_], in1=nblk.t[:, e_:e_ + 1], op=ALU.add), [nblk], [pend])
                    kb.op("dve", lambda e: e.tensor_tensor(out=pstart.t[:, :], in0=pend.t[:, :], in1=nblk.t[:, :], op=ALU.subtract), [pend, nblk], [pstart])
                    kb.op("dve", lambda e: e.tensor_scalar(out=pstart.t[:, :], in0=pstart.t[:, :], scalar1=float(BS), scalar2=None, op0=ALU.mult), [], [pstart])
                    for e_ in range(E):
                        if e_ == 0:
                            kb.op("dve", lambda e: e.tensor_scalar(out=bexp.t[:, :], in0=jrow_c[:, 0:NB], scalar1=pend.t[:, 0:1], scalar2=None, op0=ALU.is_ge), [cf, pend], [bexp])
                        else:
                            kb.op("dve", lambda e: e.tensor_scalar(out=tmpB.t[:, :], in0=jrow_c[:, 0:NB], scalar1=pend.t[:, e_:e_ + 1], scalar2=None, op0=ALU.is_ge), [cf, pend], [tmpB])
                            kb.op("dve", lambda e: e.tensor_tensor(out=bexp.t[:, :], in0=bexp.t[:, :], in1=tmpB.t[:, :], op=ALU.add), [tmpB], [bexp])
                    kb.op("dve", lambda e: e.tensor_scalar(out=bexp.t[:, :], in0=bexp.t[:, :], scalar1=float(E - 1), scalar2=None, op0=ALU.min), [], [bexp])
                    for (mult_, base_, dstI) in ((float(KF * 128), 0.0, gidxI), (float(F), 0.0, didxI)):
                        kb.op("dve", lambda e: e.tensor_scalar(out=eoff.t[:, :], in0=bexp.t[:, :], scalar1=mult_, scalar2=base_, op0=ALU.mult, op1=ALU.add), [bexp], [eoff])
                        for j in range(NB):
                            kb.op("dve", lambda e: e.tensor_scalar(out=idxf.t[:, j * KF:(j + 1) * KF], in0=cidx_c, scalar1=eoff.t[:, j:j + 1], scalar2=None, op0=ALU.add), [cf, eoff], [idxf])
                        kb.op("dve", lambda e: e.tensor_copy(out=dstI.t[:, :], in_=idxf.t[:, :]), [idxf], [dstI])
                    for tt in range(NTT):
                        pp = kb.psum()
                        for t2_ in range(tt):
                            kb.op("pe", mm(pp.t[:, 0:8], ones128, selb.t[:, t2_, :], t2_ == 0, False), [selb, cb], [pp])
                        kb.op("pe", mm(pp.t[:, 0:8], ustrict, selb.t[:, tt, :], tt == 0, True), [selb, cb], [pp])
                        kb.op("dve", lambda e: e.tensor_tensor(out=posf.t[:, :], in0=pp.t[:, 0:8], in1=pstart.t[:, :], op=ALU.add), [pp, pstart], [posf])
                        kb.op("dve", lambda e: e.tensor_tensor(out=sel2.t[:, :], in0=selA.t[:, tt, :], in1=sel1A.t[:, tt, :], op=ALU.subtract), [selA, sel1A], [sel2])
                        for a_, (selX, selXb) in enumerate(((sel1A.t[:, tt, :], sel1A), (sel2.t[:, :], sel2))):
                            kb.op("dve", lambda e: e.tensor_tensor(out=tm8.t[:, :], in0=posf.t[:, :], in1=selX, op=ALU.mult), [posf, selXb], [tm8])
                            kb.op("dve", lambda e: e.reduce_sum(out=posAB.t[:, tt * 2 + a_:tt * 2 + a_ + 1], in_=tm8.t[:, :], axis=AX.X), [tm8], [posAB])
                            kb.op("dve", lambda e: e.tensor_tensor(out=tm8.t[:, :], in0=gwA.t[:, tt, :], in1=selX, op=ALU.mult), [gwA, selXb], [tm8])
                            kb.op("dve", lambda e: e.reduce_sum(out=wAB.t[:, tt * 2 + a_:tt * 2 + a_ + 1], in_=tm8.t[:, :], axis=AX.X), [tm8], [wAB])
                    kb.op("dve", lambda e: e.tensor_copy(out=posI.t[:, :], in_=posAB.t[:, :]), [posAB], [posI])
                    scs = [Buf("scs%d" % i_) for i_ in range(8)]
                    for tt in range(NTT):
                        for a_ in range(2):
                            kb.idma(XsD[:, :], bass.IndirectOffsetOnAxis(ap=posI.t[:, tt * 2 + a_:tt * 2 + a_ + 1], axis=0), h2tm.t[:, tt, :], None, [h2tm, posI, dXs], [Buf("snk")], scs[(tt * 2 + a_) % 8])
                    kb.barrier()
            with contextlib.ExitStack() as st1:
                Xg = [kb.tile(st1, [128, SUB, D], BF16, "Xg") for _ in range(2)]
                XTb = kb.tile(st1, [128, KD, BS], BF16, "XTb")
                actT = kb.tile(st1, [128, KF, BS], BF16, "actT")
                gbb = [kb.tile(st1, [128, KD * 256], BF16, "gbb") for _ in range(4)]
                dbb = [kb.tile(st1, [128, D], BF16, "dbb") for _ in range(4)]
                sl = [kb.tile(st1, [128, BS], BF16, "silu") for _ in range(3)]
                yst = [kb.tile(st1, [128, D], F32, "yst") for _ in range(2)]
                wc = 0
                for j in range(NB):
                    xg = Xg[j % 2]
                    kb.dma("sp", xg.t[:, :, :], XsD[j * BS:(j + 1) * BS, :].rearrange("(s p) d -> p s d", p=128), [dXs], [xg], xg)
                    for k in range(KD):
                        p = kb.psum()
                        for s_ in range(SUB):
                            kb.op("pe", mm(p.t[:, s_ * 128:(s_ + 1) * 128], xg.t[:, s_, k * 128:(k + 1) * 128], identb), [xg, cb], [p])
                        evac(XTb.t[:, k, :], XTb, p, p.t[:, 0:BS])
                    for f in range(KF):
                        gb = gbb[wc % 4]; wc += 1
                        kb.idma(gb.t[:, :], None, WbfG[:, :], bass.IndirectOffsetOnAxis(ap=gidxI.t[:, j * KF + f:j * KF + f + 1], axis=0), [gidxI, dWbf], [gb], gb)
                        pg = kb.psum(); pu = kb.psum()
                        for k in range(KD):
                            kb.op("pe", mm(pg.t[:, 0:BS], gb.t[:, k * 256:k * 256 + 128], XTb.t[:, k, :], k == 0, k == KD - 1), [gb, XTb], [pg])
                        for k in range(KD):
                            kb.op("pe", mm(pu.t[:, 0:BS], gb.t[:, k * 256 + 128:k * 256 + 256], XTb.t[:, k, :], k == 0, k == KD - 1), [gb, XTb], [pu])
                        sl_ = sl[f % 3]
                        kb.op("act", lambda e: e.activation(out=sl_.t[:, :], in_=pg.t[:, 0:BS], func=AF.Silu), [pg], [sl_])
                        kb.op("dve", lambda e: e.tensor_tensor(out=actT.t[:, f, :], in0=pu.t[:, 0:BS], in1=sl_.t[:, :], op=ALU.mult), [pu, sl_], [actT])
                    for kf in range(KF):
                        db = dbb[wc % 4]; wc += 1
                        kb.idma(db.t[:, :], None, WbfD[:, :], bass.IndirectOffsetOnAxis(ap=didxI.t[:, j * KF + kf:j * KF + kf + 1], axis=0), [didxI, dWbf], [db], db)
                        for s_ in range(SUB):
                            for hf in range(D // 512):
                                pb_ = kb.ps[(s_ * (D // 512) + hf) % 8]
                                kb.op("pe", mm(pb_.t[:, 0:512], actT.t[:, kf, s_ * 128:(s_ + 1) * 128], db.t[:, hf * 512:(hf + 1) * 512], kf == 0, kf == KF - 1), [actT, db], [pb_])
                    for s_ in range(SUB):
                        y_ = yst[s_ % 2]
                        for hf in range(D // 512):
                            pb_ = kb.ps[(s_ * (D // 512) + hf) % 8]
                            evac(y_.t[:, hf * 512:(hf + 1) * 512], y_, pb_, pb_.t[:, 0:512])
                        r0 = j * BS + s_ * 128
                        kb.dma("sp", YsD[r0:r0 + 128, :], y_.t[:, :], [y_], [dYs], y_)
                kb.barrier()
            with contextlib.ExitStack() as st1:
                yA = [kb.tile(st1, [128, D], F32, "yA") for _ in range(4)]
                yB = [kb.tile(st1, [128, D], F32, "yB") for _ in range(4)]
                u4 = [kb.tile(st1, [128, 4, D], F32, "u4") for _ in range(2)]
                xts = [kb.tile(st1, [128, 512], F32, "x5") for _ in range(3)]
                tr5 = [kb.tile(st1, [128, 512], F32, "tr5") for _ in range(2)]
                xc = 0
                for qi, ci in enumerate(clist):
                    t0, n = cfg.chunks[ci]
                    s_i = 1 if ci == 0 else 0
                    u_ = u4[qi % 2]
                    for tl_ in range(n // 128):
                        tt = (t0 - tok0) // 128 + tl_
                        a_ = yA[tt % 4]; b_ = yB[tt % 4]
                        kb.idma(a_.t[:, :], None, YsD[:, :], bass.IndirectOffsetOnAxis(ap=posI.t[:, tt * 2:tt * 2 + 1], axis=0), [posI, dYs], [a_], a_)
                        kb.idma(b_.t[:, :], None, YsD[:, :], bass.IndirectOffsetOnAxis(ap=posI.t[:, tt * 2 + 1:tt * 2 + 2], axis=0), [posI, dYs], [b_], b_)
                        kb.op("dve", lambda e: e.tensor_scalar(out=u_.t[:, tl_, :], in0=a_.t[:, :], scalar1=wAB.t[:, tt * 2:tt * 2 + 1], scalar2=None, op0=ALU.mult), [a_, wAB], [u_])
                        kb.op("dve", lambda e: e.scalar_tensor_tensor(out=u_.t[:, tl_, :], in0=b_.t[:, :], scalar=wAB.t[:, tt * 2 + 1:tt * 2 + 2], in1=u_.t[:, tl_, :], op0=ALU.mult, op1=ALU.add), [b_, wAB], [u_])
                    for k in range(KD):
                        p = kb.psum()
                        for tl_ in range(n // 128):
                            kb.op("pe", mm(p.t[:, tl_ * 128:(tl_ + 1) * 128], u_.t[:, tl_, k * 128:(k + 1) * 128], id128f), [u_, cf], [p])
                        tr_ = tr5[xc % 2]
                        x_ = xts[xc % 3]; xc += 1
                        js = slice(k * 128, (k + 1) * 128)
                        kb.dma("sp", x_.t[:, 0:n], XT[js, t0:t0 + n], [dummy], [x_], x_)
                        kb.op("act", lambda e: e.activation(out=tr_.t[:, 0:n], in_=p.t[:, 0:n], func=AF.Identity, scale=mod(l, 5, k, s_i)), [p, modv], [tr_])
                        kb.op("dve", lambda e: e.tensor_tensor(out=x_.t[:, 0:n], in0=x_.t[:, 0:n], in1=tr_.t[:, 0:n], op=ALU.add), [tr_], [x_])
                        kb.dma("pool", XT[js, t0:t0 + n], x_.t[:, 0:n], [x_], [Buf("snk")], x_)
                kb.barrier()

    for l in range(L):
        last = l == L - 1
        moe = l % 2 == 1
        xsrc = xT_in if l == 0 else XT
        nch = len(cfg.chunks)
        halves = [list(range(0, (nch + 1) // 2)), list(range((nch + 1) // 2, nch))]
        for hchunks in halves:
          if not hchunks:
              continue
          hb = cfg.chunks[hchunks[0]][0]
          W = sum(cfg.chunks[ci][1] for ci in hchunks)
          with contextlib.ExitStack() as st:
            hT = kb.tile(st, [128, KD, W], BF16, "hT")
            with contextlib.ExitStack() as st2:
                nts = [norm_tiles(st2) for _ in range(2)]
                for i_, ci in enumerate(hchunks):
                    t0, n = cfg.chunks[ci]
                    norm_chunk(nts[i_ % 2], xsrc, l, ci, V_NMIX, 0, 1, hT, t0 - hb)
                kb.barrier()
            rt = kb.tile(st, [128, 4, W], BF16, "ropet")
            if stop == "P1a":
                return nc
            kb.dma("sp", rt.t[:, :, :], ropet[:, :, hb:hb + W], [dummy], [rt], rt)
            ropeg_cos = rt.t[:, 0, :]; ropeg_sin = rt.t[:, 1, :]; ropem_cos = rt.t[:, 2, :]; ropem_sin = rt.t[:, 3, :]
            stgs = [kb.tile(st, [128, KD * 128], F32, "wstg") for _ in range(3)]
            wbs = [kb.tile(st, [128, KD, 128], BF16, "wb") for _ in range(4)]
            outs = [kb.tile(st, [128, 512], BF16, "o1") for _ in range(4)]
            sqb = [kb.tile(st, [128, 512], BF16, "sqb") for _ in range(2)]
            rsb = [kb.tile(st, [128, 512], F32, "rsb") for _ in range(2)]
            tf = [kb.tile(st, [128, 512], F32, "tf") for _ in range(4)]
            cnt = {"w": 0, "o": 0, "s": 0, "t": 0, "r": 0}

            wseq = [0, 1, 2, 3, 7, 8, 9, 13, 10, 14, 11, 15, 12, 16] + list(range(19, 19 + 24)) + [4, 17, 18, 5, 6]
            wstate = {"issued": 0, "ready": {}}

            def issue_next():
                i = wstate["issued"]
                if i >= len(wseq):
                    return
                sg = stgs[i % 3]; wb_ = wbs[i % 4]
                kb.dma("sp", sg.t[:, :], w1[l, wseq[i]], [dummy], [sg], sg)
                cast(wb_.t[:, :, :], wb_, sg.t[:, :].rearrange("p (k c) -> p k c", k=KD), sg)
                wstate["ready"][i] = wb_
                wstate["issued"] += 1

            def getw(nt):
                i = cnt["w"]; cnt["w"] += 1
                assert wseq[i] == nt
                while wstate["issued"] <= i + 1 and wstate["issued"] < len(wseq):
                    issue_next()
                return wstate["ready"].pop(i)

            def proj(wb_, ci):
                t0, n = cfg.chunks[ci]
                p = kb.psum()
                for k in range(KD):
                    kb.op("pe", mm(p.t[:, 0:n], wb_.t[:, k, :], hT.t[:, k, t0 - hb:t0 - hb + n], k == 0, k == KD - 1), [wb_, hT], [p])
                return p

            def nxt(lst, key):
                i = cnt[key]; cnt[key] += 1
                return lst[i % len(lst)]

            def store(o_, n, dst_ap, dbuf):
                kb.dma("pool", dst_ap, o_.t[:, 0:n], [o_], [dbuf], o_)

            def plain_group(tiles, dst, dbuf, func=None):
                for j, nt in enumerate(tiles):
                    wb_ = getw(nt)
                    for ci in hchunks:
                        t0, n = cfg.chunks[ci]
                        p = proj(wb_, ci)
                        o_ = nxt(outs, "o")
                        evac(o_.t[:, 0:n], o_, p, p.t[:, 0:n], func)
                        store(o_, n, dst[j * 128:(j + 1) * 128, t0:t0 + n], dbuf)

            def sq_rstd(plist, ones_ap, dim, n):
                rs_ = nxt(rsb, "r")
                pc = kb.psum()
                for i_, pa in enumerate(plist):
                    sq_ = nxt(sqb, "s")
                    kb.op("act", lambda e: e.activation(out=sq_.t[:, 0:n], in_=pa.t[:, 0:n], func=AF.Square), [pa], [sq_])
                    kb.op("pe", mm(pc.t[:, 0:n], ones_ap, sq_.t[:, 0:n], i_ == 0, i_ == len(plist) - 1), [sq_, cb], [pc])
                rstd_from(pc, n, dim, rs_)
                return rs_

            def rope_norm_group(tiles, rtiles, gcol, grcol, dst, dbuf):
                for j in range(len(tiles)):
                    wa = getw(tiles[j]); wr_ = getw(rtiles[j])
                    for ci in hchunks:
                        t0, n = cfg.chunks[ci]
                        c0 = t0 - hb
                        pa = proj(wa, ci); pb = proj(wr_, ci)
                        STEP = int(os.environ.get("STEP", "99"))
                        if STEP < 1:
                            continue
                        rs_ = sq_rstd([pa], bd64, 64, n)
                        t1 = nxt(tf, "t"); t2 = nxt(tf, "t")
                        if STEP < 2:
                            continue
                        kb.op("act", lambda e: e.activation(out=t1.t[:, 0:n], in_=pa.t[:, 0:n], func=AF.Identity, scale=vcol(l, gcol)), [pa, vc], [t1])
                        kb.op("act", lambda e: e.activation(out=t2.t[:, 0:n], in_=pb.t[:, 0:n], func=AF.Identity, scale=vcol(l, grcol)), [pb, vc], [t2])
                        kb.op("dve", lambda e: e.tensor_tensor(out=t1.t[:, 0:n], in0=t1.t[:, 0:n], in1=ropeg_cos[:, c0:c0 + n], op=ALU.mult), [rt], [t1])
                        kb.op("dve", lambda e: e.tensor_tensor(out=t2.t[:, 0:n], in0=t2.t[:, 0:n], in1=ropeg_sin[:, c0:c0 + n], op=ALU.mult), [rt], [t2])
                        if STEP < 3:
                            continue
                        kb.op("pool", lambda e: e.tensor_tensor(out=t1.t[:, 0:n], in0=t1.t[:, 0:n], in1=t2.t[:, 0:n], op=ALU.add), [t2], [t1])
                        o_ = nxt(outs, "o")
                        if STEP < 4:
                            continue
                        kb.op("dve", lambda e: e.tensor_tensor(out=o_.t[:, 0:n], in0=t1.t[:, 0:n], in1=rs_.t[:, 0:n], op=ALU.mult), [t1, rs_], [o_])
                        store(o_, n, dst[j * 128:(j + 1) * 128, t0:t0 + n], dbuf)

            plain_group([0, 1], KnaT, dQK["KnaT"])
            if stop == "P1b":
                kb.barrier(); return nc
            rope_norm_group([2], [3], V_KG, V_KGR, KgT, dQK["KgT"])
            if stop == "P1c":
                kb.barrier(); return nc
            plain_group([7, 8], QnaT, dQK["QnaT"])
            rope_norm_group([9, 10, 11, 12], [13, 14, 15, 16], V_QG, V_QGR, QgT, dQK["QgT"])
            plain_group(list(range(19, 19 + 24)), GT, dQK["GT"], func=AF.Sigmoid)
            if stop == "P1d":
                kb.barrier(); return nc
            ckvn = kb.tile(st, [128, W], BF16, "ckvn")
            cqn = kb.tile(st, [128, 2, W], BF16, "cqn")
            krope = kb.tile(st, [128, W], BF16, "krope")
            wa = getw(4)
            for ci in hchunks:
                t0, n = cfg.chunks[ci]
                c0 = t0 - hb
                pa = proj(wa, ci)
                rs_ = sq_rstd([pa], ones128, 128, n)
                t1 = nxt(tf, "t")
                kb.op("act", lambda e: e.activation(out=t1.t[:, 0:n], in_=pa.t[:, 0:n], func=AF.Identity, scale=vcol(l, V_KVL)), [pa, vc], [t1])
                kb.op("dve", lambda e: e.tensor_tensor(out=ckvn.t[:, c0:c0 + n], in0=t1.t[:, 0:n], in1=rs_.t[:, 0:n], op=ALU.mult), [t1, rs_], [ckvn])
            wa = getw(17); wb2 = getw(18)
            for ci in hchunks:
                t0, n = cfg.chunks[ci]
                c0 = t0 - hb
                pa = proj(wa, ci); pb = proj(wb2, ci)
                rs_ = sq_rstd([pa, pb], ones128, 256, n)
                t1 = nxt(tf, "t"); t2 = nxt(tf, "t")
                kb.op("act", lambda e: e.activation(out=t1.t[:, 0:n], in_=pa.t[:, 0:n], func=AF.Identity, scale=vcol(l, V_QL)), [pa, vc], [t1])
                kb.op("act", lambda e: e.activation(out=t2.t[:, 0:n], in_=pb.t[:, 0:n], func=AF.Identity, scale=vcol(l, V_QL + 1)), [pb, vc], [t2])
                kb.op("dve", lambda e: e.tensor_tensor(out=cqn.t[:, 0, c0:c0 + n], in0=t1.t[:, 0:n], in1=rs_.t[:, 0:n], op=ALU.mult), [t1, rs_], [cqn])
                kb.op("dve", lambda e: e.tensor_tensor(out=cqn.t[:, 1, c0:c0 + n], in0=t2.t[:, 0:n], in1=rs_.t[:, 0:n], op=ALU.mult), [t2, rs_], [cqn])
            wa = getw(5); wb2 = getw(6)
            for ci in hchunks:
                t0, n = cfg.chunks[ci]
                c0 = t0 - hb
                pa = proj(wa, ci); pb = proj(wb2, ci)
                t1 = nxt(tf, "t"); t2 = nxt(tf, "t")
                kb.op("dve", lambda e: e.tensor_tensor(out=t1.t[64:96, 0:n], in0=pa.t[64:96, 0:n], in1=ropem_cos[64:96, c0:c0 + n], op=ALU.mult), [pa, rt], [t1])
                kb.op("dve", lambda e: e.tensor_tensor(out=t2.t[64:96, 0:n], in0=pb.t[64:96, 0:n], in1=ropem_sin[64:96, c0:c0 + n], op=ALU.mult), [pb, rt], [t2])
                kb.op("dve", lambda e: e.tensor_tensor(out=krope.t[64:96, c0:c0 + n], in0=t1.t[64:96, 0:n], in1=t2.t[64:96, 0:n], op=ALU.add), [t1, t2], [krope])
            w2s = kb.tile(st, [128, 1536], F32, "w2s")
            wuq_b = kb.tile(st, [128, 2, 384], BF16, "wuq")
            wuqr_b = kb.tile(st, [128, 2, 384], BF16, "wuqr")
            wkk_b = kb.tile(st, [128, 256], BF16, "wkk")
            wkv_b = kb.tile(st, [128, 256], BF16, "wkv")
            wv_b = kb.tile(st, [128, KD, 384], BF16, "wvb")
            for src_, dst_, wd_, db_ in ((wuq[l], wuq_b.t[:, :, :].rearrange("p a b -> p (a b)"), 768, wuq_b), (wuqr[l], wuqr_b.t[:, :, :].rearrange("p a b -> p (a b)"), 768, wuqr_b),
                                        (wukvk[l], wkk_b.t[:, :], 256, wkk_b), (wukvv[l], wkv_b.t[:, :], 256, wkv_b),
                                        (wv[l][:, 0:1536], wv_b.t[:, 0:KD // 2, :].rearrange("p a b -> p (a b)"), 1536, wv_b),
                                        (wv[l][:, 1536:3072], wv_b.t[:, KD // 2:KD, :].rearrange("p a b -> p (a b)"), 1536, wv_b)):
                kb.dma("sp", w2s.t[:, 0:wd_], src_, [dummy], [w2s], w2s)
                kb.op("dve", lambda e: e.tensor_copy(out=dst_, in_=w2s.t[:, 0:wd_]), [w2s], [db_])
            kst = [kb.tile(st, [128, 512], BF16, "kst") for _ in range(3)]
            if stop == "P1e":
                kb.barrier(); return nc
            for ci in hchunks:
                t0, n = cfg.chunks[ci]
                c0 = t0 - hb
                for h in range(4):
                    p = kb.psum()
                    kb.op("pe", mm(p.t[0:64, 0:n], wkk_b.t[:, h * 64:(h + 1) * 64], ckvn.t[:, c0:c0 + n]), [wkk_b, ckvn], [p])
                    o_ = nxt(kst, "o")
                    evac(o_.t[0:64, 0:n], o_, p, p.t[0:64, 0:n])
                    kb.op("pool", lambda e: e.tensor_copy(out=o_.t[64:96, 0:n], in_=krope.t[64:96, c0:c0 + n]), [krope], [o_])
                    kb.dma("pool", KmT[h * 96:(h + 1) * 96, t0:t0 + n], o_.t[0:96, 0:n], [o_], [dQK["KmT"]], o_)
                    p = kb.psum(); pr = kb.psum()
                    for k in range(2):
                        kb.op("pe", mm(p.t[0:96, 0:n], wuq_b.t[:, k, h * 96:(h + 1) * 96], cqn.t[:, k, c0:c0 + n], k == 0, k == 1), [wuq_b, cqn], [p])
                    for k in range(2):
                        kb.op("pe", mm(pr.t[0:96, 0:n], wuqr_b.t[:, k, h * 96:(h + 1) * 96], cqn.t[:, k, c0:c0 + n], k == 0, k == 1), [wuqr_b, cqn], [pr])
                    o_ = nxt(kst, "o")
                    evac(o_.t[0:64, 0:n], o_, p, p.t[0:64, 0:n])
                    t1 = nxt(tf, "t"); t2 = nxt(tf, "t")
                    kb.op("dve", lambda e: e.tensor_tensor(out=t1.t[64:96, 0:n], in0=p.t[64:96, 0:n], in1=ropem_cos[64:96, c0:c0 + n], op=ALU.mult), [p, rt], [t1])
                    kb.op("dve", lambda e: e.tensor_tensor(out=t2.t[64:96, 0:n], in0=pr.t[64:96, 0:n], in1=ropem_sin[64:96, c0:c0 + n], op=ALU.mult), [pr, rt], [t2])
                    kb.op("dve", lambda e: e.tensor_tensor(out=o_.t[64:96, 0:n], in0=t1.t[64:96, 0:n], in1=t2.t[64:96, 0:n], op=ALU.add), [t1, t2], [o_])
                    kb.dma("pool", QmT[h * 96:(h + 1) * 96, t0:t0 + n], o_.t[0:96, 0:n], [o_], [dQK["QmT"]], o_)
            if stop == "P1f":
                kb.barrier(); return nc
            vst = [kb.tile(st, [128, 10, 65], BF16, "vst") for _ in range(3)]
            for v_ in vst:
                kb.op("pool", lambda e: e.memset(v_.t[:, :, :], 1.0), [], [v_])
            for tt in range(hb // 128, (hb + W) // 128):
                c0 = tt * 128 - hb
                p = kb.psum(); p2 = kb.psum()
                for k in range(KD):
                    kb.op("pe", mm(p.t[:, 0:384], hT.t[:, k, c0:c0 + 128], wv_b.t[:, k, :], k == 0, k == KD - 1), [hT, wv_b], [p])
                kb.op("pe", mm(p2.t[:, 0:256], ckvn.t[:, c0:c0 + 128], wkv_b.t[:, :]), [ckvn, wkv_b], [p2])
                v_ = nxt(vst, "o")
                kb.op("act", lambda e: e.activation(out=v_.t[:, 0:6, 0:64], in_=p.t[:, 0:384].rearrange("p (h d) -> p h d", h=6), func=AF.Copy), [p], [v_])
                kb.op("dve", lambda e: e.tensor_copy(out=v_.t[:, 6:10, 0:64], in_=p2.t[:, 0:256].rearrange("p (h d) -> p h d", h=4)), [p2], [v_])
                rs_ = slice(tt * 128, (tt + 1) * 128)
                kb.dma("pool", Vna[rs_, :].rearrange("t (h d) -> t h d", h=4), v_.t[:, 0:4, :], [v_], [dQK["Vna"]], v_)
                kb.dma("pool", Vg[rs_, :].rearrange("t (h d) -> t h d", h=2), v_.t[:, 4:6, :], [v_], [dQK["Vg"]], v_)
                kb.dma("pool", Vm[rs_, :].rearrange("t (h d) -> t h d", h=4), v_.t[:, 6:10, :], [v_], [dQK["Vm"]], v_)
            kb.barrier()

        if stop == "P1":
            return nc
        PS_S = (0, 4); PS_O = (4, 2); PS_B = (6, 2)

        def attn_phase(name, dk, nh, nkv, Ksrc, Qsrc, Vsrc, scale, yrow0, na=False):
            with contextlib.ExitStack() as st:
                pk = 128 if dk == 64 else dk
                Kt = kb.tile(st, [pk, nkv, T], BF16, "K" + name)
                Vt = kb.tile(st, [128, NKT, nkv, 65], BF16, "V" + name)
                if pk != dk:
                    kb.op("pool", lambda e: e.memset(Kt.t[dk:pk, :, :], 0.0), [], [Kt])
                kb.dma("sp", Kt.t[0:dk, :, :], Ksrc.rearrange("(h d) t -> d h t", d=dk), [dQK["K%sT" % name]], [Kt], Kt)
                kb.dma("sp", Vt.t[:, :, :, :], Vsrc.rearrange("(k p) (h d) -> p k h d", p=128, d=65), [dQK["V" + name]], [Vt], Vt)
                if na:
                    Vo = kb.tile(st, [128, NKT - CT - 1, nkv, 65], BF16, "Vo")
                    kb.dma("sp", Vo.t[:, :, :, :], Vsrc[C + 64:T - 64, :].rearrange("(k p) (h d) -> p k h d", p=128, d=65), [dQK["V" + name]], [Vo], Vo)
                    rst = kb.tile(st, [64, 3840], F32, "rpbst")
                    Tc = kb.tile(st, [128, 4, 15, 64], BF16, "Tcat")
                    kb.op("pool", lambda e: e.memset(Tc.t[64:128, :, :, :], 0.0), [], [Tc])
                    kb.dma("sp", rst.t[:, :], rpbT[l], [dummy], [rst], rst)
                    ngm = kb.tile(st, [64, 3840], BF16, "negm")
                    kb.dma("sp", ngm.t[:, :], negm[:, :], [dummy], [ngm], ngm)
                    kb.op("dve", lambda e: e.scalar_tensor_tensor(out=Tc.t[0:64, :, :, :].rearrange("p a b c -> p (a b c)"), in0=rst.t[:, :], scalar=8.0, in1=ngm.t[:, :], op0=ALU.mult, op1=ALU.add), [rst, ngm], [Tc])
                Qs = [kb.tile(st, [pk, nh, 512], BF16, "Q" + name) for _ in range(2)]
                if pk != dk:
                    for q_ in Qs:
                        kb.op("pool", lambda e, q_=q_: e.memset(q_.t[dk:pk, :, :], 0.0), [], [q_])
                Ps = [kb.tile(st, [128, 512], BF16, "P" + name) for _ in range(4)]
                Rt = kb.tile(st, [65, 512], F32, "Rt")
                bcs = [kb.tile(st, [64, 512], F32, "bc") for _ in range(2)]
                ys = [kb.tile(st, [64, 512], BF16, "ys") for _ in range(3)]
                kb.op("pool", lambda e: e.memset(Rt.t[:, :], 0.0), [], [Rt])
                if conv.active():
                    conv.attach(st)
                c_ = {"p": 0, "y": 0, "b": 0}
                pnorm = {"f": None}
                clist = list(range(len(cfg.chunks)))
                if last:
                    clist = clist[1:]
                for qi, ci in enumerate(clist):
                    t0, n = cfg.chunks[ci]
                    Q = Qs[qi % 2]
                    kb.dma("sp", Q.t[0:dk, :, 0:n], Qsrc[:, t0:t0 + n].rearrange("(h d) t -> d h t", d=dk), [dQK["Q%sT" % name]], [Q], Q)
                    for h in range(nh):
                        kvh = h * nkv // nh
                        po = kb.psum(PS_O)
                        if ci == 0 or not na:
                            kts = list(range(CT)) if ci == 0 else list(range(NKT))
                            LA = 2
                            pend = []
                            for i_, kt in enumerate(kts):
                                ps_ = kb.psum(PS_S)
                                kb.op("pe", mm(ps_.t[:, 0:n], Kt.t[:, kvh, kt * 128:(kt + 1) * 128], Q.t[:, h, 0:n]), [Kt, Q], [ps_])
                                pend.append((ps_, kt))
                                if i_ == min(LA, len(kts) - 1) and pnorm["f"] is not None:
                                    pnorm["f"](); pnorm["f"] = None
                                if len(pend) > LA or i_ == len(kts) - 1:
                                    while pend and (len(pend) > LA or i_ == len(kts) - 1):
                                        ps2, kt2 = pend.pop(0)
                                        P = Ps[c_["p"] % 4]; c_["p"] += 1
                                        kb.op("act", lambda e, P=P, ps2=ps2: e.activation(out=P.t[:, 0:n], in_=ps2.t[:, 0:n], func=AF.Exp, scale=scale), [ps2], [P])
                                        kb.op("pe", mm(po.t[0:65, 0:n], Vt.t[:, kt2, kvh, :], P.t[:, 0:n], kt2 == kts[0], kt2 == kts[-1]), [Vt, P], [po])
                        else:
                            r0 = (t0 - C) // 64
                            pendu = []

                            def finish_unit(u):
                                ps_u, groups_u, qs_u = u
                                ng = len(groups_u)
                                P = Ps[c_["p"] % 4]; c_["p"] += 1
                                kb.op("act", lambda e: e.activation(out=P.t[:, 0:ng * 64], in_=ps_u.t[:, 0:ng * 64], func=AF.Exp, scale=scale), [ps_u], [P])
                                for gi, (tk, vt, vb, dr) in enumerate(groups_u):
                                    kb.op("pe", mm(po.t[0:65, qs_u], vt, P.t[:, gi * 64:(gi + 1) * 64], gi == 0, gi == ng - 1), [vb, P], [po])
                            for rl in range(n // 64):
                                r = r0 + rl
                                row0 = min(max(r - 4, 0), R - 8)
                                qs = slice(rl * 64, (rl + 1) * 64)
                                ps_ = kb.psum(PS_S)
                                groups = []
                                for g in range(4):
                                    tk = C + (row0 + 2 * g) * 64
                                    if row0 % 2 == 0:
                                        vt = Vt.t[:, tk // 128, kvh, :]
                                        vb = Vt
                                    else:
                                        vt = Vo.t[:, (tk - C - 64) // 128, kvh, :]
                                        vb = Vo
                                    groups.append((tk, vt, vb, row0 + 2 * g - r))
                                for g in range(CT):
                                    groups.append((g * 128, Vt.t[:, g, kvh, :], Vt, None))
                                for gi, (tk, vt, vb, dr) in enumerate(groups):
                                    osl = slice(gi * 64, (gi + 1) * 64)
                                    kb.op("pe", mm(ps_.t[:, osl], Kt.t[:, kvh, tk:tk + 128], Q.t[:, h, qs], True, dr is None), [Kt, Q], [ps_])
                                    if dr is not None:
                                        kb.op("pe", mm(ps_.t[:, osl], Tc.t[:, h, dr + 7:dr + 9, :].rearrange("p a b -> p (a b)"), id64b, False, True), [Tc, cb], [ps_])
                                pendu.append((ps_, groups, qs))
                                if rl == 0 and pnorm["f"] is not None:
                                    pnorm["f"](); pnorm["f"] = None
                                if len(pendu) > 2:
                                    finish_unit(pendu.pop(0))
                            while pendu:
                                finish_unit(pendu.pop(0))
                        kb.op("dve", lambda e, po=po: e.reciprocal(out=Rt.t[64:65, 0:n], in_=po.t[64:65, 0:n]), [po], [Rt])
                        pb_ = kb.psum(PS_B)
                        kb.op("pe", mm(pb_.t[:, 0:n], sel64, Rt.t[0:65, 0:n]), [cf, Rt], [pb_])

                        def rest(po=po, pb_=pb_, h=h, t0=t0, n=n):
                            bc = bcs[c_["b"] % 2]; c_["b"] += 1
                            kb.op("dve", lambda e: e.tensor_copy(out=bc.t[:, 0:n], in_=pb_.t[0:64, 0:n]), [pb_], [bc])
                            y_ = ys[c_["y"] % 3]; c_["y"] += 1
                            kb.op("dve", lambda e: e.tensor_tensor(out=y_.t[:, 0:n], in0=po.t[0:64, 0:n], in1=bc.t[:, 0:n], op=ALU.mult), [po, bc], [y_])
                            kb.dma("pool", YT[yrow0 + h * 64:yrow0 + (h + 1) * 64, t0:t0 + n], y_.t[:, 0:n], [y_], [dQK["YT"]], y_)
                            conv.step()
                        pnorm["f"] = rest
                if pnorm["f"] is not None:
                    pnorm["f"](); pnorm["f"] = None
                conv.drain(everything=(moe and name == "m"))
                kb.barrier()

        if l + 1 < L and (l + 1) % 2 == 1:
            conv.add_layer((l + 1) // 2)
        elif l == 0 and moe:
            conv.add_layer(0)
        attn_phase("na", 64, 4, 4, KnaT, QnaT, Vna, 0.125, 0, na=True)
        if stop == "P2a":
            return nc
        attn_phase("g", 64, 8, 2, KgT, QgT, Vg, 0.125, 256)
        attn_phase("m", 96, 4, 4, KmT, QmT, Vm, 96 ** -0.5, 768)

        if stop == "P2":
            return nc
        with contextlib.ExitStack() as st:
            stg = [kb.tile(st, [128, 2048], F32, "w3s") for _ in range(2)]
            wo_b = kb.tile(st, [128, 8, D], BF16, "wo")
            wout_b = kb.tile(st, [128, KD, D], BF16, "wout")
            i = 0
            for src_, dst_ in ((wo[l], wo_b), (wout[l], wout_b)):
                for k in range(0, 8, 2):
                    sg = stg[i % 2]; i += 1
                    kb.dma("sp", sg.t[:, :], src_[:, k * D:(k + 2) * D], [dummy], [sg], sg)
                    cast(dst_.t[:, k:k + 2, :], dst_, sg.t[:, :].rearrange("p (a b) -> p a b", a=2), sg)
            Ys = [kb.tile(st, [128, 8, 512], BF16, "Y") for _ in range(2)]
            Gs = [kb.tile(st, [128, 24, 512], BF16, "G") for _ in range(2)]
            Xs = [kb.tile(st, [128, KD, 512], F32, "X") for _ in range(2)]
            mT = [kb.tile(st, [128, KD, 512], BF16, "m") for _ in range(2)]
            tfs = [kb.tile(st, [128, 512], F32, "t3") for _ in range(4)]
            tc = 0
            clist = list(range(len(cfg.chunks)))
            if last:
                clist = clist[1:]
            for qi, ci in enumerate(clist):
                t0, n = cfg.chunks[ci]
                s_ = 1 if ci == 0 else 0
                Y = Ys[qi % 2]; G = Gs[qi % 2]; X = Xs[qi % 2]; m_ = mT[qi % 2]
                kb.dma("sp", Y.t[:, :, 0:n], YT[:, t0:t0 + n].rearrange("(k p) t -> p k t", p=128), [dQK["YT"]], [Y], Y)
                kb.dma("sp", G.t[:, :, 0:n], GT[:, t0:t0 + n].rearrange("(k p) t -> p k t", p=128), [dQK["GT"]], [G], G)
                kb.dma("sp", X.t[:, :, 0:n], xsrc[:, t0:t0 + n].rearrange("(k p) t -> p k t", p=128), [dXT[ci]], [X], X)
                for j in range(KD):
                    js = slice(j * 128, (j + 1) * 128)
                    pa = kb.psum(); pb = kb.psum(); pc = kb.psum()
                    for k in range(2):
                        kb.op("pe", mm(pa.t[:, 0:n], wo_b.t[:, k, js], Y.t[:, k, 0:n], k == 0, k == 1), [wo_b, Y], [pa])
                    for k in range(4):
                        kb.op("pe", mm(pb.t[:, 0:n], wo_b.t[:, 2 + k, js], Y.t[:, 2 + k, 0:n], k == 0, k == 3), [wo_b, Y], [pb])
                    for k in range(2):
                        kb.op("pe", mm(pc.t[:, 0:n], wo_b.t[:, 6 + k, js], Y.t[:, 6 + k, 0:n], k == 0, k == 1), [wo_b, Y], [pc])
                    t1 = tfs[tc % 4]; t2 = tfs[(tc + 1) % 4]; tc += 2
                    kb.op("dve", lambda e, t1=t1, pa=pa, G=G, j=j: e.tensor_tensor(out=t1.t[:, 0:n], in0=pa.t[:, 0:n], in1=G.t[:, j, 0:n], op=ALU.mult), [pa, G], [t1])
                    kb.op("dve", lambda e, t2=t2, pb=pb, G=G, j=j: e.tensor_tensor(out=t2.t[:, 0:n], in0=pb.t[:, 0:n], in1=G.t[:, 8 + j, 0:n], op=ALU.mult), [pb, G], [t2])
                    kb.op("pool", lambda e, t1=t1, t2=t2: e.tensor_tensor(out=t1.t[:, 0:n], in0=t1.t[:, 0:n], in1=t2.t[:, 0:n], op=ALU.add), [t2], [t1])
                    kb.op("dve", lambda e, t2=t2, pc=pc, G=G, j=j: e.tensor_tensor(out=t2.t[:, 0:n], in0=pc.t[:, 0:n], in1=G.t[:, 16 + j, 0:n], op=ALU.mult), [pc, G], [t2])
                    kb.op("pool", lambda e, t1=t1, t2=t2, m_=m_, j=j: e.tensor_tensor(out=m_.t[:, j, 0:n], in0=t1.t[:, 0:n], in1=t2.t[:, 0:n], op=ALU.add), [t1, t2], [m_])
                for j in range(KD):
                    js = slice(j * 128, (j + 1) * 128)
                    p = kb.psum()
                    for k in range(KD):
                        kb.op("pe", mm(p.t[:, 0:n], wout_b.t[:, k, js], m_.t[:, k, 0:n], k == 0, k == KD - 1), [wout_b, m_], [p])
                    tr_ = tfs[tc % 4]; tc += 1
                    kb.op("act", lambda e, p=p, j=j, s_=s_, tr_=tr_: e.activation(out=tr_.t[:, 0:n], in_=p.t[:, 0:n], func=AF.Identity, scale=mod(l, 2, j, s_)), [p, modv], [tr_])
                    kb.op("dve", lambda e, X=X, j=j, tr_=tr_: e.tensor_tensor(out=X.t[:, j, 0:n], in0=X.t[:, j, 0:n], in1=tr_.t[:, 0:n], op=ALU.add), [tr_], [X])
                kb.dma("pool", XT[:, t0:t0 + n].rearrange("(k p) t -> p k t", p=128), X.t[:, :, 0:n], [X], [dXT[ci]], X)
            kb.barrier()

        if stop == "P3":
            return nc
        if moe and not os.environ.get("DENSE_MOE"):
            routed_moe(l, last)
            continue
        clist = list(range(len(cfg.chunks)))
        if last:
            clist = clist[1:]
        groups = [clist[i:i + 2] for i in range(0, len(clist), 2)]
        with contextlib.ExitStack() as st:
            h2 = kb.tile(st, [128, KD, 1024], BF16, "h2")
            if moe:
                acc = kb.tile(st, [128, KD, 1024], F32, "acc")
                gwbc = kb.tile(st, [128, E, 1024], BF16, "gwbc")
                wr_t = kb.tile(st, [128, KD, 8], F32, "wr")
                gwT = kb.tile(st, [8, 1024], F32, "gwT")
                sm = [kb.tile(st, [128, 8], F32, "sm%d" % i) for i in range(6)]
                s1 = [kb.tile(st, [128, 1], F32, "s1%d" % i) for i in range(5)]
                kb.dma("sp", wr_t.t[:, :, :], wr[l // 2].rearrange("p (k e) -> p k e", e=8), [dummy], [wr_t], wr_t)
            wc = 0
            wcs = [0]
            xc = 0
            for grp in groups:
                cols = []
                c0 = 0
                with contextlib.ExitStack() as st2:
                    nt4 = norm_tiles(st2)
                    h2f = kb.tile(st2, [128, KD, 512], F32, "h2f") if moe else None
                    for ci in grp:
                        t0, n = cfg.chunks[ci]
                        cols.append((ci, t0, n, c0))
                        norm_chunk(nt4, XT, l, ci, V_NFFN, 3, 4, h2, c0, out_f32=h2f)
                        if moe:
                            for tt in range(n // 128):
                                p = kb.psum()
                                for k in range(KD):
                                    kb.op("pe", mm(p.t[:, 0:8], h2f.t[:, k, tt * 128:(tt + 1) * 128], wr_t.t[:, k, :], k == 0, k == KD - 1), [h2f, wr_t], [p])
                                Lg, m1e, L2, selm, ex, gw = sm
                                m1, m2, nm1, ss, rs1 = s1
                                kb.op("dve", lambda e: e.tensor_copy(out=Lg.t[:, :], in_=p.t[:, 0:8]), [p], [Lg])
                                kb.op("dve", lambda e: e.reduce_max(out=m1.t[:, :], in_=Lg.t[:, :], axis=AX.X), [Lg], [m1])
                                kb.op("dve", lambda e: e.tensor_scalar(out=m1e.t[:, :], in0=Lg.t[:, :], scalar1=m1.t[:, 0:1], scalar2=-1e30, op0=ALU.is_equal, op1=ALU.mult), [Lg, m1], [m1e])
                                kb.op("dve", lambda e: e.tensor_tensor(out=L2.t[:, :], in0=Lg.t[:, :], in1=m1e.t[:, :], op=ALU.add), [Lg, m1e], [L2])
                                kb.op("dve", lambda e: e.reduce_max(out=m2.t[:, :], in_=L2.t[:, :], axis=AX.X), [L2], [m2])
                                kb.op("dve", lambda e: e.tensor_scalar(out=selm.t[:, :], in0=Lg.t[:, :], scalar1=m2.t[:, 0:1], scalar2=None, op0=ALU.is_ge), [Lg, m2], [selm])
                                kb.op("dve", lambda e: e.tensor_scalar(out=nm1.t[:, :], in0=m1.t[:, :], scalar1=-1.0, scalar2=None, op0=ALU.mult), [m1], [nm1])
                                kb.op("act", lambda e: e.activation(out=ex.t[:, :], in_=Lg.t[:, :], func=AF.Exp, bias=nm1.t[:, 0:1], scale=1.0), [Lg, nm1], [ex])
                                kb.op("dve", lambda e: e.tensor_tensor(out=ex.t[:, :], in0=ex.t[:, :], in1=selm.t[:, :], op=ALU.mult), [selm], [ex])
                                kb.op("dve", lambda e: e.reduce_sum(out=ss.t[:, :], in_=ex.t[:, :], axis=AX.X), [ex], [ss])
                                kb.op("dve", lambda e: e.reciprocal(out=rs1.t[:, :], in_=ss.t[:, :]), [ss], [rs1])
                                kb.op("dve", lambda e: e.tensor_scalar(out=gw.t[:, :], in0=ex.t[:, :], scalar1=rs1.t[:, 0:1], scalar2=None, op0=ALU.mult), [ex, rs1], [gw])
                                p2 = kb.psum()
                                kb.op("pe", mm(p2.t[0:8, 0:128], gw.t[:, :], id128f), [gw, cf], [p2])
                                kb.op("dve", lambda e: e.tensor_copy(out=gwT.t[:, c0 + tt * 128:c0 + (tt + 1) * 128], in_=p2.t[0:8, 0:128]), [p2], [gwT])
                            for e_ in range(E):
                                p = kb.psum()
                                kb.op("pe", mm(p.t[:, 0:n], sel_e(e_), gwT.t[:, c0:c0 + n]), [cf, gwT], [p])
                                evac(gwbc.t[:, e_, c0:c0 + n], gwbc, p, p.t[:, 0:n])
                        c0 += n
                    kb.barrier()
                with contextlib.ExitStack() as st2:
                    act = kb.tile(st2, [128, KF, 1024], BF16, "act")
                    gus = [kb.tile(st2, [128, KD * 256], F32, "gus") for _ in range(3)]
                    gub = [kb.tile(st2, [128, KD, 256], BF16, "gub") for _ in range(3)]
                    dns = [kb.tile(st2, [128, KF * 128], F32, "dns") for _ in range(3)]
                    dnb = [kb.tile(st2, [128, KF, 128], BF16, "dnb") for _ in range(3)]
                    xts = [kb.tile(st2, [128, 512], F32, "x4") for _ in range(3)]
                    sl = [kb.tile(st2, [128, 512], BF16, "silu") for _ in range(3)]
                    t5 = [kb.tile(st2, [128, 512], BF16, "t5") for _ in range(2)]
                    for e_ in range(E if moe else 1):
                        gsrc = wgu[l // 2]
                        dsrc = wdn[l // 2]
                        def issue_gu(f):
                            nonlocal_wc = wcs[0]; wcs[0] += 1
                            sg = gus[nonlocal_wc % 3]; gb = gub[nonlocal_wc % 3]
                            kb.dma("sp", sg.t[:, :], gsrc[f], [dummy], [sg], sg)
                            cast(gb.t[:, :, :], gb, sg.t[:, :].rearrange("p (k c) -> p k c", k=KD), sg)
                            return gb

                        def issue_dn(j):
                            nonlocal_wc = wcs[0]; wcs[0] += 1
                            sg = dns[nonlocal_wc % 3]; db = dnb[nonlocal_wc % 3]
                            kb.dma("sp", sg.t[:, :], dsrc[j], [dummy], [sg], sg)
                            cast(db.t[:, :, :], db, sg.t[:, :].rearrange("p (k c) -> p k c", k=KF), sg)
                            return db
                        gb_next = issue_gu(0)
                        for f in range(KF):
                            gb = gb_next
                            gb_next = issue_gu(f + 1) if f + 1 < KF else None
                            if f + 1 == KF:
                                db_next = issue_dn(0)
                            for (ci, t0, n, c0) in cols:
                                pg = kb.psum(); pu = kb.psum()
                                for k in range(KD):
                                    kb.op("pe", mm(pg.t[:, 0:n], gb.t[:, k, 0:128], h2.t[:, k, c0:c0 + n], k == 0, k == KD - 1), [gb, h2], [pg])
                                for k in range(KD):
                                    kb.op("pe", mm(pu.t[:, 0:n], gb.t[:, k, 128:256], h2.t[:, k, c0:c0 + n], k == 0, k == KD - 1), [gb, h2], [pu])
                                s_ = sl[xc % 3]; xc += 1
                                kb.op("act", lambda e: e.activation(out=s_.t[:, 0:n], in_=pg.t[:, 0:n], func=AF.Silu), [pg], [s_])
                                if moe:
                                    t_ = t5[xc % 2]
                                    kb.op("dve", lambda e: e.tensor_tensor(out=t_.t[:, 0:n], in0=pu.t[:, 0:n], in1=s_.t[:, 0:n], op=ALU.mult), [pu, s_], [t_])
                                    kb.op("pool", lambda e: e.tensor_tensor(out=act.t[:, f, c0:c0 + n], in0=t_.t[:, 0:n], in1=gwbc.t[:, e_, c0:c0 + n], op=ALU.mult), [t_, gwbc], [act])
                                else:
                                    kb.op("dve", lambda e: e.tensor_tensor(out=act.t[:, f, c0:c0 + n], in0=pu.t[:, 0:n], in1=s_.t[:, 0:n], op=ALU.mult), [pu, s_], [act])
                        for j in range(KD):
                            db = db_next
                            db_next = issue_dn(j + 1) if j + 1 < KD else None
                            for (ci, t0, n, c0) in cols:
                                s_i = 1 if ci == 0 else 0
                                p = kb.psum()
                                for k in range(KF):
                                    kb.op("pe", mm(p.t[:, 0:n], db.t[:, k, :], act.t[:, k, c0:c0 + n], k == 0, k == KF - 1), [db, act], [p])
                                fin = (not moe) or e_ == E - 1
                                if moe and e_ == 0:
                                    evac(acc.t[:, j, c0:c0 + n], acc, p, p.t[:, 0:n])
                                elif moe:
                                    kb.op("dve", lambda e: e.tensor_tensor(out=acc.t[:, j, c0:c0 + n], in0=p.t[:, 0:n], in1=acc.t[:, j, c0:c0 + n], op=ALU.add), [p], [acc])
                                if fin:
                                    x_ = xts[xc % 3]; xc += 1
                                    js = slice(j * 128, (j + 1) * 128)
                                    kb.dma("sp", x_.t[:, 0:n], XT[js, t0:t0 + n], [dummy], [x_], x_)
                                    if moe:
                                        kb.op("dve", lambda e: e.scalar_tensor_tensor(out=x_.t[:, 0:n], in0=acc.t[:, j, c0:c0 + n], scalar=mod(l, 5, j, s_i), in1=x_.t[:, 0:n], op0=ALU.mult, op1=ALU.add), [acc, modv], [x_])
                                    else:
                                        tq_ = t5[xc % 2]
                                        tq32 = xts[(xc + 1) % 3]
                                        kb.op("act", lambda e: e.activation(out=tq32.t[:, 0:n], in_=p.t[:, 0:n], func=AF.Identity, scale=mod(l, 5, j, s_i)), [p, modv], [tq32])
                                        kb.op("dve", lambda e: e.tensor_tensor(out=x_.t[:, 0:n], in0=x_.t[:, 0:n], in1=tq32.t[:, 0:n], op=ALU.add), [tq32], [x_])
                                    kb.dma("pool", XT[js, t0:t0 + n], x_.t[:, 0:n], [x_], [Buf("snk")], x_)
                    kb.barrier()

    with contextlib.ExitStack() as st:
        xt = [kb.tile(st, [128, KD, 512], F32, "xf") for _ in range(2)]
        sq = [kb.tile(st, [128, KD, 512], BF16, "sqf") for _ in range(2)]
        rs = [kb.tile(st, [128, 512], F32, "rsf") for _ in range(2)]
        for qi, ci in enumerate(range(1, len(cfg.chunks))):
            t0, n = cfg.chunks[ci]
            x_ = xt[qi % 2]; s_ = sq[qi % 2]; r_ = rs[qi % 2]
            kb.dma("sp", x_.t[:, :, 0:n], XT[:, t0:t0 + n].rearrange("(k p) t -> p k t", p=128), [dXT[ci]], [x_], x_)
            p = kb.psum()
            for k in range(KD):
                kb.op("act", lambda e, k=k, x_=x_, s_=s_: e.activation(out=s_.t[:, k, 0:n], in_=x_.t[:, k, 0:n], func=AF.Square), [x_], [s_])
                kb.op("pe", mm(p.t[:, 0:n], ones128, s_.t[:, k, 0:n], k == 0, k == KD - 1), [s_, cb], [p])
            rstd_from(p, n, D, r_)
            for k in range(KD):
                kb.op("dve", lambda e, k=k, x_=x_, r_=r_: e.scalar_tensor_tensor(out=x_.t[:, k, 0:n], in0=x_.t[:, k, 0:n], scalar=v_nfinal[:, k:k + 1], in1=r_.t[:, 0:n], op0=ALU.mult, op1=ALU.mult), [r_, vc], [x_])
            kb.dma("pool", outT[:, t0 - C:t0 - C + n].rearrange("(k p) t -> p k t", p=128), x_.t[:, :, 0:n], [x_], [dXT[ci]], x_)
        kb.barrier()
    return nc


def _tile_cols(w, KD):
    D = w.shape[0]
    return np.ascontiguousarray(w.reshape(KD, 128, w.shape[1]).transpose(1, 0, 2).reshape(128, -1))


def host_prep(cfg, inp):
    D, C, S, L, F, E, T, KD, KF = cfg.D, cfg.C, cfg.S, cfg.L, cfg.F, cfg.E, cfg.T, cfg.KD, cfg.KF
    f32 = np.float32
    sh = {}
    NVL = 2 * KD + 48 + 6 + 3

    def fm(v):
        return np.asarray(v, f32).reshape(-1, 128).T

    def rot64(g):
        return np.concatenate([g[32:], g[:32]])
    w_in = np.asarray(inp["w_in"], f32)
    KVW = 928
    o_kna, o_vna, o_kg, o_vg, o_ckv, o_kr = 0, 256, 512, 640, 768, 896
    o_qna, o_qg, o_cq, o_gate = KVW, KVW + 256, KVW + 768, KVW + 1024

    def rotcols(base, nheads, d):
        idx = []
        for h in range(nheads):
            idx += list(range(base + h * d + d // 2, base + (h + 1) * d)) + list(range(base + h * d, base + h * d + d // 2))
        return idx
    w1 = np.zeros((L, cfg.NT1, 128, KD * 128), f32)
    wvv = np.zeros((L, 128, KD * 384), f32)
    for l in range(L):
        W = w_in[l]
        tiles = []
        tiles += [W[:, o_kna:o_kna + 128], W[:, o_kna + 128:o_kna + 256]]
        tiles += [W[:, o_kg:o_kg + 128], W[:, rotcols(o_kg, 2, 64)]]
        tiles += [W[:, o_ckv:o_ckv + 128]]
        t5 = np.zeros((D, 128), f32); t5[:, 64:96] = W[:, o_kr:o_kr + 32]
        t6 = np.zeros((D, 128), f32); t6[:, 64:96] = W[:, rotcols(o_kr, 1, 32)]
        tiles += [t5, t6]
        tiles += [W[:, o_qna:o_qna + 128], W[:, o_qna + 128:o_qna + 256]]
        tiles += [W[:, o_qg + j * 128:o_qg + (j + 1) * 128] for j in range(4)]
        rc = rotcols(o_qg, 8, 64)
        tiles += [W[:, rc[j * 128:(j + 1) * 128]] for j in range(4)]
        tiles += [W[:, o_cq:o_cq + 128], W[:, o_cq + 128:o_cq + 256]]
        tiles += [W[:, o_gate + j * 128:o_gate + (j + 1) * 128] for j in range(24)]
        for i, t in enumerate(tiles):
            w1[l, i] = _tile_cols(t, KD)
        Wv = np.concatenate([W[:, o_vna:o_vna + 256], W[:, o_vg:o_vg + 128]], axis=1)
        wvv[l] = _tile_cols(Wv, KD)
    sh["w1"] = w1
    sh["wv"] = wvv
    w_ada = np.asarray(inp["w_ada"], f32)
    sh["w_ada"] = np.ascontiguousarray(w_ada.reshape(L, KD, 128, 48, 128).transpose(0, 3, 2, 1, 4).reshape(L, 48, 128, KD * 128))
    w_uq = np.asarray(inp["w_uq"], f32)
    sh["wuq"] = np.ascontiguousarray(w_uq.reshape(L, 2, 128, 384).transpose(0, 2, 1, 3).reshape(L, 128, 768))
    wuqr = np.zeros_like(w_uq)
    for h in range(4):
        b = h * 96 + 64
        wuqr[:, :, b:b + 16] = w_uq[:, :, b + 16:b + 32]
        wuqr[:, :, b + 16:b + 32] = w_uq[:, :, b:b + 16]
    sh["wuqr"] = np.ascontiguousarray(wuqr.reshape(L, 2, 128, 384).transpose(0, 2, 1, 3).reshape(L, 128, 768))
    w_ukv = np.asarray(inp["w_ukv"], f32).reshape(L, 128, 4, 128)
    sh["wukvk"] = np.ascontiguousarray(w_ukv[:, :, :, :64].reshape(L, 128, 256))
    sh["wukvv"] = np.ascontiguousarray(w_ukv[:, :, :, 64:].reshape(L, 128, 256))
    rpb = np.asarray(inp["rpb"], f32)
    jq = np.arange(64)[:, None]; jk = np.arange(64)[None, :]
    dc = np.clip(jk - jq, -15, 15) + 15
    g = rpb[:, :, :, dc]
    sh["rpbT"] = np.ascontiguousarray(g.transpose(0, 3, 1, 2, 4).reshape(L, 64, 3840))
    wo_all = np.concatenate([np.asarray(inp["w_o_na"], f32), np.asarray(inp["w_o_gqa"], f32), np.asarray(inp["w_o_mla"], f32)], axis=1)
    sh["wo"] = np.ascontiguousarray(wo_all.reshape(L, 8, 128, D).transpose(0, 2, 1, 3).reshape(L, 128, 8 * D))
    sh["wout"] = np.ascontiguousarray(np.asarray(inp["w_out"], f32).reshape(L, KD, 128, D).transpose(0, 2, 1, 3).reshape(L, 128, KD * D))

    def gu_layout(w):
        lead = w.shape[:-2]
        gte = w[..., :F].reshape(*lead, KD, 128, KF, 128)
        up = w[..., F:].reshape(*lead, KD, 128, KF, 128)
        cat = np.stack([gte, up], axis=-2)
        nl = len(lead)
        perm = list(range(nl)) + [nl + 2, nl + 1, nl + 0, nl + 3, nl + 4]
        return np.ascontiguousarray(cat.transpose(perm).reshape(*lead, KF, 128, KD * 256))

    def dn_layout(w):
        lead = w.shape[:-2]
        nl = len(lead)
        a = w.reshape(*lead, KF, 128, KD, 128)
        perm = list(range(nl)) + [nl + 2, nl + 1, nl + 0, nl + 3]
        return np.ascontiguousarray(a.transpose(perm).reshape(*lead, KD, 128, KF * 128))
    sh["wgu"] = gu_layout(np.asarray(inp["w_ffn_gu"], f32))
    sh["wdn"] = dn_layout(np.asarray(inp["w_ffn_dn"], f32))
    if cfg.NM > 0:
        sh["wgum"] = gu_layout(np.asarray(inp["w_moe_gu"], f32)).reshape(cfg.NM * E * KF * 128, KD * 256)
        sh["wdnm"] = np.ascontiguousarray(np.asarray(inp["w_moe_dn"], f32).reshape(cfg.NM * E * F, D))
        sh["wr"] = np.ascontiguousarray(np.asarray(inp["w_router"], f32).reshape(cfg.NM, KD, 128, 8).transpose(0, 2, 1, 3).reshape(cfg.NM, 128, KD * 8))
    else:
        sh["wgum"] = np.zeros((E * KF * 128, KD * 256), f32)
        sh["wdnm"] = np.zeros((E * F, D), f32)
        sh["wr"] = np.zeros((1, 128, KD * 8), f32)
    pos = np.arange(S)
    rows = (pos // 64).astype(f32); cols = (pos % 64).astype(f32)

    def tables(half):
        nf = half // 2
        inv = np.power(10000.0, -np.arange(nf, dtype=f32) / nf).astype(f32)
        ang = np.concatenate([rows[:, None] * inv, cols[:, None] * inv], axis=-1)
        cos = np.concatenate([np.ones((C, half), f32), np.cos(ang)], 0).T
        sin = np.concatenate([np.zeros((C, half), f32), np.sin(ang)], 0).T
        return cos, sin
    cg, sg = tables(32)
    cosg = np.concatenate([cg, cg, cg, cg], 0)
    sing = np.concatenate([-sg, sg, -sg, sg], 0)
    cm, sm_ = tables(16)
    cosm = np.zeros((128, T), f32); sinm = np.zeros((128, T), f32)
    cosm[64:96] = np.concatenate([cm, cm], 0)
    sinm[64:96] = np.concatenate([-sm_, sm_], 0)
    col0 = np.clip(np.arange(64) - 8, 0, 48)
    inwin = (jk >= col0[:, None]) & (jk < col0[:, None] + 16)
    neg = np.where(inwin, 0.0, -30000.0).astype(f32)
    negm = np.tile(neg[:, None, :], (1, 60, 1)).reshape(64, 3840)
    ones = np.ones((128, 128), f32)
    bd = np.zeros((128, 128), f32); bd[:64, :64] = 1; bd[64:, 64:] = 1
    idb = np.zeros((128, 64), f32); idb[:64] = np.eye(64)
    ustr = np.triu(np.ones((128, 128), f32), 1)
    sh["cbf"] = np.ascontiguousarray(np.concatenate([ones, bd, idb, ustr, np.eye(128, dtype=f32)], 1).astype(BF))
    sh["ropet"] = np.ascontiguousarray(np.stack([cosg, sing, cosm, sinm], 1).astype(BF))
    sh["negm"] = np.ascontiguousarray(negm.astype(BF))
    sel64 = np.zeros((128, 128), f32); sel64[64, :64] = 1
    sele = np.zeros((128, E, 128), f32)
    for e in range(E):
        sele[e, e, :] = 1
    sh["cf32"] = np.ascontiguousarray(np.concatenate([np.eye(128, dtype=f32), sel64, sele.reshape(128, E * 128), np.full((128, 1), 1e-6, f32),
        np.tile((np.arange(10, dtype=f32) * 512)[None, :], (128, 1)), np.tile(np.arange((2 * T + E * 511) // 512, dtype=f32)[None, :], (128, 1)),
        (np.arange(KF, dtype=f32)[None, :] * 128 + np.arange(128, dtype=f32)[:, None])], 1))
    NV = L * NVL + 3 * KD
    per = []
    xin = np.asarray(inp["x"], f32); ctx = np.asarray(inp["ctx"], f32); c = np.asarray(inp["c"], f32)
    B = xin.shape[0]
    vbase = np.zeros((128, NV), f32)
    for l in range(L):
        o = l * NVL
        vbase[:, o:o + KD] = fm(inp["norm_mix"][l])
        vbase[:, o + KD:o + 2 * KD] = fm(inp["norm_ffn"][l])
        vbase[:, o + 2 * KD:o + 2 * KD + 48] = fm(inp["b_ada"][l])
        qg = np.asarray(inp["q_norm_gqa"][l], f32); kg = np.asarray(inp["k_norm_gqa"][l], f32)
        vbase[:, o + 2 * KD + 48] = np.concatenate([qg, qg])
        vbase[:, o + 2 * KD + 49] = np.concatenate([rot64(qg), rot64(qg)])
        vbase[:, o + 2 * KD + 50] = np.concatenate([kg, kg])
        vbase[:, o + 2 * KD + 51] = np.concatenate([rot64(kg), rot64(kg)])
        vbase[:, o + 2 * KD + 52:o + 2 * KD + 54] = fm(inp["q_lora_norm"][l])
        vbase[:, o + 2 * KD + 54] = np.asarray(inp["kv_lora_norm"][l], f32)
    go = L * NVL
    vbase[:, go:go + KD] = fm(inp["norm_final"])
    vbase[:, go + 2 * KD:go + 3 * KD] = fm(inp["c_ctx"])
    for b in range(B):
        m = dict(sh)
        v = vbase.copy()
        v[:, go + KD:go + 2 * KD] = fm(c[b])
        m["vecs"] = v
        m["xT"] = np.ascontiguousarray(np.concatenate([ctx[b], xin[b]], 0).T)
        per.append(m)
    return per


_CACHE = {}


def kernel(**inputs):
    cfg = Cfg()
    if "nc" not in _CACHE:
        _CACHE["nc"] = build(cfg)
    nc = _CACHE["nc"]
    in_maps = host_prep(cfg, inputs)
    res = run_bass_kernel_spmd(nc, in_maps, core_ids=list(range(len(in_maps))))
    out = np.stack([np.ascontiguousarray(r["outT"].T) for r in res.results], 0)
    return out.astype(np.float32)
```

```python
import contextlib
import os
import numpy as np
import ml_dtypes
import concourse.bass as bass
import concourse.mybir as mybir
from concourse.bass_utils import run_bass_kernel_spmd

F32, BF16 = mybir.dt.float32, mybir.dt.bfloat16
AF = mybir.ActivationFunctionType
ALU = mybir.AluOpType
AX = mybir.AxisListType
BF = ml_dtypes.bfloat16


class Cfg:
    def __init__(s, D=1024, C=256, S=4096, L=4, F=2816, E=8):
        s.D, s.C, s.S, s.L, s.F, s.E = D, C, S, L, F, E
        s.T = C + S
        s.KD = D // 128
        s.R = S // 64
        s.KF = F // 128
        s.chunks = [(0, C)] + [(C + i * 512, 512) for i in range(S // 512)]
        s.NKT = s.T // 128
        s.CT = C // 128
        s.ND = (L + 1) // 2
        s.NM = L // 2
        s.NT1 = 19 + 24


class Buf:
    __slots__ = ("name", "w", "r", "dsem", "dram")

    def __init__(s, name, dram=False):
        s.name = name
        s.w = {}
        s.r = {}
        s.dsem = None
        s.dram = dram


class Tl:
    def __init__(s, t, name):
        s.t = t
        s.b = Buf(name)


class KB:
    NDMASEM = 90

    def __init__(self, nc):
        self.nc = nc
        self.eng = {"pe": nc.tensor, "act": nc.scalar, "dve": nc.vector, "pool": nc.gpsimd, "sp": nc.sync}
        self.sems = []
        self.last = []
        self.semidx = {}
        for e in ("pe", "act", "dve", "pool"):
            self.semidx[e] = self._newsem("s_" + e)
        self.cnt = {e: 0 for e in self.semidx}
        self.waited = {e: {} for e in self.eng}
        self.dsems = []
        self.nd = 0
        self.stack = contextlib.ExitStack()
        self.ps = []
        for i in range(8):
            t = self.stack.enter_context(nc.psum_tensor("ps%d" % i, [128, 512], F32))
            self.ps.append(Tl(t, "ps%d" % i))
        self.psi = 0
        self.ntile = 0

    def _newsem(self, name):
        s = self.nc.semaphore(name).__enter__()
        self.sems.append(s)
        self.last.append(0)
        return len(self.sems) - 1

    def tile(self, st, shape, dt, name=None):
        self.ntile += 1
        name = (name or "t") + "_%d" % self.ntile
        t = st.enter_context(self.nc.sbuf_tensor(name, list(shape), dt))
        return Tl(t, name)

    def psum(self, pool=None):
        pool = pool or (0, 8)
        lo, n = pool
        key = ("psi", lo, n)
        i = getattr(self, "_rr", {}).get(key, 0)
        if not hasattr(self, "_rr"):
            self._rr = {}
        self._rr[key] = (i + 1) % n
        return self.ps[lo + i]

    def _wait(self, e, si, v):
        wd = self.waited[e]
        if e == "pe" and si == self.semidx["pe"]:
            return
        if wd.get(si, 0) < v:
            self.eng[e].wait_ge(self.sems[si], v)
            wd[si] = v

    def _deps(self, e, reads, writes):
        need = {}
        for b in reads:
            for si, v in b.w.items():
                if need.get(si, 0) < v:
                    need[si] = v
        for b in writes:
            if not b.dram:
                for si, v in b.w.items():
                    if need.get(si, 0) < v:
                        need[si] = v
            for si, v in b.r.items():
                if need.get(si, 0) < v:
                    need[si] = v
        for si, v in need.items():
            self._wait(e, si, v)

    def _mark(self, tok, reads, writes):
        si, v = tok
        for b in writes:
            if b.dram:
                if b.w.get(si, 0) < v:
                    b.w[si] = v
            else:
                b.w = {si: v}
            b.r = {}
        for b in reads:
            if any(b is w for w in writes):
                continue
            b.r[si] = v

    def op(self, e, fn, reads=(), writes=()):
        reads = [x.b if isinstance(x, Tl) else x for x in reads]
        writes = [x.b if isinstance(x, Tl) else x for x in writes]
        self._deps(e, reads, writes)
        ins = fn(self.eng[e])
        self.cnt[e] += 1
        si = self.semidx[e]
        ins.then_inc(self.sems[si], 1)
        self.last[si] = self.cnt[e]
        self._mark((si, self.cnt[e]), reads, writes)

    def dma(self, q, out, in_, reads, writes, sb):
        reads = [x.b if isinstance(x, Tl) else x for x in reads]
        writes = [x.b if isinstance(x, Tl) else x for x in writes]
        sb = sb.b if isinstance(sb, Tl) else sb
        if sb.dsem is None:
            if len(self.dsems) < self.NDMASEM:
                self.dsems.append(self._newsem("d%d" % len(self.dsems)))
                sb.dsem = self.dsems[-1]
            else:
                sb.dsem = self.dsems[self.nd % self.NDMASEM]
            self.nd += 1
        si = sb.dsem
        self._deps(q, reads, writes)
        if self.last[si] > 0:
            self._wait(q, si, self.last[si])
        ins = self.eng[q].dma_start(out=out, in_=in_)
        self.last[si] += 16
        ins.then_inc(self.sems[si], 16)
        self._mark((si, self.last[si]), reads, writes)

    def idma(self, out, out_off, in_, in_off, reads, writes, sb):
        reads = [x.b if isinstance(x, Tl) else x for x in reads]
        writes = [x.b if isinstance(x, Tl) else x for x in writes]
        sb = sb.b if isinstance(sb, Tl) else sb
        if sb.dsem is None:
            if len(self.dsems) < self.NDMASEM:
                self.dsems.append(self._newsem("d%d" % len(self.dsems)))
                sb.dsem = self.dsems[-1]
            else:
                sb.dsem = self.dsems[self.nd % self.NDMASEM]
            self.nd += 1
        si = sb.dsem
        self._deps("pool", reads, writes)
        if self.last[si] > 0:
            self._wait("pool", si, self.last[si])
        ins = self.eng["pool"].indirect_dma_start(out=out, out_offset=out_off, in_=in_, in_offset=in_off)
        self.last[si] += 16
        ins.then_inc(self.sems[si], 16)
        self._mark((si, self.last[si]), reads, writes)

    def barrier(self):
        for e in self.eng:
            for si, v in enumerate(self.last):
                if v > 0:
                    self._wait(e, si, v)


def mm(out, lhsT, rhs, start=True, stop=True):
    return lambda e: e.matmul(out, lhsT, rhs, start=start, stop=stop)


def build(cfg, debug=False, stop=None):
    nc = bass.Bass("TRN2", target_bir_lowering=False)
    D, C, S, L, F, E, T, KD, KF = cfg.D, cfg.C, cfg.S, cfg.L, cfg.F, cfg.E, cfg.T, cfg.KD, cfg.KF
    NKT, CT, R = cfg.NKT, cfg.CT, cfg.R

    def din(name, shape, dt=F32):
        return nc.dram_tensor(name, list(shape), dt, kind="ExternalInput").ap()

    def dscr(name, shape, dt=BF16):
        return nc.dram_tensor(name, list(shape), dt, kind=("ExternalOutput" if debug else "Internal")).ap()

    xT_in = din("xT", [D, T])
    NVL = 2 * KD + 48 + 6 + 3
    NV = L * NVL + 3 * KD
    vecs = din("vecs", [128, NV])
    w_ada = din("w_ada", [L, 48, 128, KD * 128])
    w1 = din("w1", [L, cfg.NT1, 128, KD * 128])
    wv = din("wv", [L, 128, KD * 384])
    wuq = din("wuq", [L, 128, 2 * 384])
    wuqr = din("wuqr", [L, 128, 2 * 384])
    wukvk = din("wukvk", [L, 128, 256])
    wukvv = din("wukvv", [L, 128, 256])
    rpbT = din("rpbT", [L, 64, 3840])
    wo = din("wo", [L, 128, 8 * D])
    wout = din("wout", [L, 128, KD * D])
    wgu = din("wgu", [max(cfg.ND, 1), KF, 128, KD * 256])
    wdn = din("wdn", [max(cfg.ND, 1), KD, 128, KF * 128])
    NMx = max(cfg.NM, 1)
    wgum = din("wgum", [NMx * E * KF * 128, KD * 256])
    wdnm = din("wdnm", [NMx * E * F, D])
    wr = din("wr", [max(cfg.NM, 1), 128, KD * 8])
    NCB = 128 + 128 + 64 + 128 + 128
    cbf = din("cbf", [128, NCB], BF16)
    ropet = din("ropet", [128, 4, T], BF16)
    negm = din("negm", [64, 3840], BF16)
    BS = 512
    NTHR = 10
    NBMAX = (2 * T + E * (BS - 1)) // BS
    NCF = 128 + 128 + E * 128 + 1 + NTHR + NBMAX + KF
    cf32 = din("cf32", [128, NCF])
    outT = nc.dram_tensor("outT", [D, S], F32, kind="ExternalOutput").ap()

    XT = nc.dram_tensor("XTs", [D, T], F32, kind="Internal").ap()
    KnaT = dscr("KnaT", [256, T])
    QnaT = dscr("QnaT", [256, T])
    KgT = dscr("KgT", [128, T])
    QgT = dscr("QgT", [512, T])
    KmT = dscr("KmT", [4 * 96, T])
    QmT = dscr("QmT", [4 * 96, T])
    Vna = dscr("Vna", [T, 4 * 65])
    Vg = dscr("Vg", [T, 2 * 65])
    Vm = dscr("Vm", [T, 4 * 65])
    GT = dscr("GT", [3 * D, T])
    YT = dscr("YT", [D, T])
    XsD = nc.dram_tensor("Xs", [NBMAX * BS, D], BF16, kind="Internal").ap()
    YsD = nc.dram_tensor("Ys", [NBMAX * BS, D], F32, kind="Internal").ap()

    kb = KB(nc)
    dXT = [Buf("XT%d" % i, dram=True) for i in range(len(cfg.chunks))]
    dQK = {n: Buf(n, dram=True) for n in ("KnaT", "QnaT", "KgT", "QgT", "KmT", "QmT", "Vna", "Vg", "Vm", "GT", "YT")}

    gst = contextlib.ExitStack()
    cb = kb.tile(gst, [128, NCB], BF16, "cbf")
    cf = kb.tile(gst, [128, NCF], F32, "cf32")
    vc = kb.tile(gst, [128, NV], F32, "vecs")
    modv = kb.tile(gst, [128, L * 48 * 2], F32, "modv")
    dummy = Buf("dram_in")
    kb.dma("sp", cb.t[:, :], cbf[:, :], [dummy], [cb], cb)
    kb.dma("sp", cf.t[:, :], cf32[:, :], [dummy], [cf], cf)
    kb.dma("sp", vc.t[:, :], vecs[:, :], [dummy], [vc], vc)
    o = 0
    ones128 = cb.t[:, o:o + 128]; o += 128
    bd64 = cb.t[:, o:o + 128]; o += 128
    id64b = cb.t[:, o:o + 64]; o += 64
    ustrict = cb.t[:, o:o + 128]; o += 128
    identb = cb.t[:, o:o + 128]; o += 128
    id128f = cf.t[:, 0:128]
    sel64 = cf.t[0:65, 128:256]
    o2 = 128 + 128 + E * 128
    epsc = cf.t[:, o2:o2 + 1]
    thr_c = cf.t[:, o2 + 1:o2 + 1 + NTHR]
    jrow_c = cf.t[:, o2 + 1 + NTHR:o2 + 1 + NTHR + NBMAX]
    cidx_c = cf.t[:, o2 + 1 + NTHR + NBMAX:o2 + 1 + NTHR + NBMAX + KF]

    def sel_e(e):
        return cf.t[0:8, 256 + e * 128:256 + (e + 1) * 128]

    def vcol(l, j, n=1):
        return vc.t[:, l * NVL + j:l * NVL + j + n]
    V_NMIX, V_NFFN, V_BADA, V_QG, V_QGR, V_KG, V_KGR, V_QL, V_KVL = 0, KD, 2 * KD, 2 * KD + 48, 2 * KD + 49, 2 * KD + 50, 2 * KD + 51, 2 * KD + 52, 2 * KD + 54
    gofs = L * NVL
    v_nfinal = vc.t[:, gofs:gofs + KD]
    v_c = vc.t[:, gofs + KD:gofs + 3 * KD]

    def mod(l, which, j, s):
        i = ((l * 48) + which * KD + j) * 2 + s
        return modv.t[:, i:i + 1]

    with contextlib.ExitStack() as st:
        sc = kb.tile(st, [128, 2 * KD], F32, "silu_c")
        sc2 = kb.tile(st, [128, KD, 2], F32, "silu_c2")
        kb.op("act", lambda e: e.activation(out=sc.t[:, :], in_=v_c, func=AF.Silu), [vc], [sc])
        for s_ in range(2):
            kb.op("dve", lambda e, s_=s_: e.tensor_copy(out=sc2.t[:, :, s_], in_=sc.t[:, s_ * KD:(s_ + 1) * KD]), [sc], [sc2])
        wst = [kb.tile(st, [128, KD * 128], F32, "wada") for _ in range(3)]
        i = 0
        for l in range(L):
            for nt in range(48):
                w_ = wst[i % 3]; i += 1
                kb.dma("sp", w_.t[:, :], w_ada[l, nt], [dummy], [w_], w_)
                p = kb.psum()
                for k in range(KD):
                    kb.op("pe", mm(p.t[:, 0:2], w_.t[:, k * 128:(k + 1) * 128], sc2.t[:, k, :], k == 0, k == KD - 1), [w_, sc2], [p])
                base = ((l * 48) + nt) * 2
                kb.op("dve", lambda e, p=p, base=base, l=l, nt=nt: e.tensor_scalar(out=modv.t[:, base:base + 2], in0=p.t[:, 0:2], scalar1=vcol(l, V_BADA + nt), scalar2=None, op0=ALU.add), [p, vc], [modv])
        kb.barrier()
    if stop == "P0":
        return nc

    def rstd_from(ps_ssq, n, dim, out_t):
        kb.op("act", lambda e: e.activation(out=out_t.t[:, 0:n], in_=ps_ssq.t[:, 0:n], func=AF.Ln, bias=epsc, scale=1.0 / dim), [ps_ssq, cf], [out_t])
        kb.op("act", lambda e: e.activation(out=out_t.t[:, 0:n], in_=out_t.t[:, 0:n], func=AF.Exp, scale=-0.5), [], [out_t])

    def norm_chunk(st_tiles, xsrc, l, ci, which_norm, which_sh, which_sc, out_bf, out_col0, out_f32=None):
        xt, sq, tmp, rs, gs = st_tiles
        t0, n = cfg.chunks[ci]
        s_ = 1 if ci == 0 else 0
        kb.dma("sp", xt.t[:, :, 0:n], xsrc[:, t0:t0 + n].rearrange("(k p) t -> p k t", p=128), [dXT[ci]], [xt], xt)
        for k in range(KD):
            kb.op("dve", lambda e, k=k: e.scalar_tensor_tensor(out=gs.t[:, k:k + 1], in0=mod(l, which_sc, k, s_), scalar=1.0, in1=vcol(l, which_norm + k), op0=ALU.add, op1=ALU.mult), [modv, vc], [gs])
        p = kb.psum()
        for k in range(KD):
            kb.op("act", lambda e, k=k: e.activation(out=sq.t[:, k, 0:n], in_=xt.t[:, k, 0:n], func=AF.Square), [xt], [sq])
            kb.op("pe", mm(p.t[:, 0:n], ones128, sq.t[:, k, 0:n], k == 0, k == KD - 1), [sq, cb], [p])
        rstd_from(p, n, D, rs)
        for k in range(KD):
            kb.op("dve", lambda e, k=k: e.tensor_tensor(out=tmp.t[:, 0:n], in0=xt.t[:, k, 0:n], in1=rs.t[:, 0:n], op=ALU.mult), [xt, rs], [tmp])
            if out_f32 is not None:
                kb.op("act", lambda e, k=k: e.activation(out=out_f32.t[:, k, 0:n], in_=tmp.t[:, 0:n], func=AF.Identity, bias=mod(l, which_sh, k, s_), scale=gs.t[:, k:k + 1]), [tmp, gs, modv], [out_f32])
                kb.op("pool", lambda e, k=k: e.tensor_copy(out=out_bf.t[:, k, out_col0:out_col0 + n], in_=out_f32.t[:, k, 0:n]), [out_f32], [out_bf])
            else:
                kb.op("act", lambda e, k=k: e.activation(out=out_bf.t[:, k, out_col0:out_col0 + n], in_=tmp.t[:, 0:n], func=AF.Identity, bias=mod(l, which_sh, k, s_), scale=gs.t[:, k:k + 1]), [tmp, gs, modv], [out_bf])

    def norm_tiles(st, wmax=512):
        return (kb.tile(st, [128, KD, wmax], F32, "xt"), kb.tile(st, [128, KD, wmax], BF16, "sq"),
                kb.tile(st, [128, wmax], F32, "tmp"), kb.tile(st, [128, wmax], F32, "rs"), kb.tile(st, [128, KD], F32, "gs"))

    def load_w_bf(stg, dst, src_ap, width, eng):
        kb.dma("sp", stg.t[:, 0:width], src_ap, [dummy], [stg], stg)
        return stg

    rr = {"ev": 0, "cast": 0}

    def cast(out_ap, out_buf, in_ap, in_buf):
        rr["cast"] ^= 1
        if rr["cast"]:
            kb.op("dve", lambda e: e.tensor_copy(out=out_ap, in_=in_ap), [in_buf], [out_buf])
        else:
            kb.op("act", lambda e: e.activation(out=out_ap, in_=in_ap, func=AF.Copy), [in_buf], [out_buf])

    def evac(out_ap, out_buf, ps_t, ps_ap, func=None):
        if func is not None:
            kb.op("act", lambda e: e.activation(out=out_ap, in_=ps_ap, func=func), [ps_t], [out_buf])
            return
        rr["ev"] ^= 1
        if rr["ev"]:
            kb.op("act", lambda e: e.activation(out=out_ap, in_=ps_ap, func=AF.Copy), [ps_t], [out_buf])
        else:
            kb.op("dve", lambda e: e.tensor_copy(out=out_ap, in_=ps_ap), [ps_t], [out_buf])

    dXs = Buf("Xs", dram=True)
    dYs = Buf("Ys", dram=True)
    WbfG = nc.dram_tensor("WbfG", [E * KF * 128, KD * 256], BF16, kind="Internal").ap()
    WbfD = nc.dram_tensor("WbfD", [E * F, D], BF16, kind="Internal").ap()
    dWbf = Buf("Wbf", dram=True)

    class Conv:
        def __init__(self):
            self.jobs = []
            self.nl = 0
            self.ncs = 0
            self.cst = None

        def add_layer(self, m_):
            assert self.ncs == len(self.jobs)
            self.jobs = []
            self.nl = 0
            self.ncs = 0
            for e_ in range(E):
                for f in range(KF):
                    r0 = ((m_ * E + e_) * KF + f) * 128
                    d0 = (e_ * KF + f) * 128
                    self.jobs.append((wgum[r0:r0 + 128, :], WbfG[d0:d0 + 128, :], False))
                for k2 in range(KF // 2):
                    r0 = (m_ * E + e_) * F + k2 * 256
                    d0 = e_ * F + k2 * 256
                    self.jobs.append((wdnm[r0:r0 + 256, :].rearrange("(a p) d -> p a d", p=128), WbfD[d0:d0 + 256, :].rearrange("(a p) d -> p a d", p=128), True))

        def active(self):
            return self.ncs < len(self.jobs)

        def attach(self, st):
            self.cst = [kb.tile(st, [128, 2048], F32, "cvs") for _ in range(3)]
            self.cbt = [kb.tile(st, [128, 2048], BF16, "cvb") for _ in range(3)]

        def _load(self):
            i = self.nl
            src, dst, three = self.jobs[i]
            sg = self.cst[i % 3]
            o = sg.t[:, :].rearrange("p (a d) -> p a d", a=2) if three else sg.t[:, :]
            kb.dma("sp", o, src, [dummy], [sg], sg)
            self.nl += 1

        def _cast_store(self):
            i = self.ncs
            src, dst, three = self.jobs[i]
            sg = self.cst[i % 3]; cb_ = self.cbt[i % 3]
            kb.op("pool", lambda e: e.tensor_copy(out=cb_.t[:, :], in_=sg.t[:, :]), [sg], [cb_])
            i_ = cb_.t[:, :].rearrange("p (a d) -> p a d", a=2) if three else cb_.t[:, :]
            kb.dma("pool", dst, i_, [cb_], [dWbf], cb_)
            self.ncs += 1

        def step(self):
            if self.cst is None:
                return
            if self.ncs < self.nl:
                self._cast_store()
            if self.nl < len(self.jobs):
                self._load()

        def drain(self, everything=False):
            if self.cst is None:
                return
            while True:
                if self.ncs < self.nl:
                    self._cast_store()
                elif everything and self.nl < len(self.jobs):
                    self._load()
                else:
                    break
            self.cst = None

    conv = Conv()

    def routed_moe(l, last):
        m_ = l // 2
        clist = list(range(len(cfg.chunks)))
        if last:
            clist = clist[1:]
        tok0 = cfg.chunks[clist[0]][0]
        TL = sum(cfg.chunks[ci][1] for ci in clist)
        NTT = TL // 128
        NB = (2 * TL + E * (BS - 1)) // BS
        SUB = BS // 128
        groups = [clist[i:i + 2] for i in range(0, len(clist), 2)]
        with contextlib.ExitStack() as st:
            selA = kb.tile(st, [128, NTT, 8], F32, "selA")
            sel1A = kb.tile(st, [128, NTT, 8], F32, "sel1A")
            gwA = kb.tile(st, [128, NTT, 8], F32, "gwA")
            posI = kb.tile(st, [128, NTT * 2], mybir.dt.int32, "posI")
            wAB = kb.tile(st, [128, NTT * 2], F32, "wAB")
            gidxI = kb.tile(st, [128, NB * KF], mybir.dt.int32, "gidxI")
            didxI = kb.tile(st, [128, NB * KF], mybir.dt.int32, "didxI")
            with contextlib.ExitStack() as st1:
                h2tm = kb.tile(st1, [128, NTT, D], BF16, "h2tm")
                wr_t = kb.tile(st1, [128, KD, 8], F32, "wr")
                kb.dma("sp", wr_t.t[:, :, :], wr[m_].rearrange("p (k e) -> p k e", e=8), [dummy], [wr_t], wr_t)
                sm = [kb.tile(st1, [128, 8], F32, "sm%d" % i) for i in range(6)]
                s1 = [kb.tile(st1, [128, 1], F32, "s1%d" % i) for i in range(5)]
                zt = kb.tile(st1, [128, SUB, D], BF16, "zeros")
                kb.op("pool", lambda e: e.memset(zt.t[:, :, :], 0.0), [], [zt])
                for j in range(NB):
                    kb.dma("sp", XsD[j * BS:(j + 1) * BS, :].rearrange("(s p) d -> p s d", p=128), zt.t[:, :, :], [zt], [dXs], zt)
                with contextlib.ExitStack() as st2:
                    h2 = kb.tile(st2, [128, KD, 512], BF16, "h2r")
                    h2f = kb.tile(st2, [128, KD, 512], F32, "h2f")
                    nt4 = norm_tiles(st2)
                    for ci in clist:
                        t0, n = cfg.chunks[ci]
                        norm_chunk(nt4, XT, l, ci, V_NFFN, 3, 4, h2, 0, out_f32=h2f)
                        for tl_ in range(n // 128):
                            tt = (t0 - tok0) // 128 + tl_
                            cs = slice(tl_ * 128, (tl_ + 1) * 128)
                            p = kb.psum()
                            for k in range(KD):
                                kb.op("pe", mm(p.t[:, 0:8], h2f.t[:, k, cs], wr_t.t[:, k, :], k == 0, k == KD - 1), [h2f, wr_t], [p])
                            Lg, m1e, L2, selm, ex, gw = sm
                            m1, m2, nm1, ss, rs1 = s1
                            kb.op("dve", lambda e: e.tensor_copy(out=Lg.t[:, :], in_=p.t[:, 0:8]), [p], [Lg])
                            kb.op("dve", lambda e: e.reduce_max(out=m1.t[:, :], in_=Lg.t[:, :], axis=AX.X), [Lg], [m1])
                            kb.op("dve", lambda e: e.tensor_scalar(out=sel1A.t[:, tt, :], in0=Lg.t[:, :], scalar1=m1.t[:, 0:1], scalar2=None, op0=ALU.is_equal), [Lg, m1], [sel1A])
                            kb.op("dve", lambda e: e.tensor_scalar(out=m1e.t[:, :], in0=sel1A.t[:, tt, :], scalar1=-1e30, scalar2=None, op0=ALU.mult), [sel1A], [m1e])
                            kb.op("dve", lambda e: e.tensor_tensor(out=L2.t[:, :], in0=Lg.t[:, :], in1=m1e.t[:, :], op=ALU.add), [Lg, m1e], [L2])
                            kb.op("dve", lambda e: e.reduce_max(out=m2.t[:, :], in_=L2.t[:, :], axis=AX.X), [L2], [m2])
                            kb.op("dve", lambda e: e.tensor_scalar(out=selA.t[:, tt, :], in0=Lg.t[:, :], scalar1=m2.t[:, 0:1], scalar2=None, op0=ALU.is_ge), [Lg, m2], [selA])
                            kb.op("dve", lambda e: e.tensor_scalar(out=nm1.t[:, :], in0=m1.t[:, :], scalar1=-1.0, scalar2=None, op0=ALU.mult), [m1], [nm1])
                            kb.op("act", lambda e: e.activation(out=ex.t[:, :], in_=Lg.t[:, :], func=AF.Exp, bias=nm1.t[:, 0:1], scale=1.0), [Lg, nm1], [ex])
                            kb.op("dve", lambda e: e.tensor_tensor(out=ex.t[:, :], in0=ex.t[:, :], in1=selA.t[:, tt, :], op=ALU.mult), [selA], [ex])
                            kb.op("dve", lambda e: e.reduce_sum(out=ss.t[:, :], in_=ex.t[:, :], axis=AX.X), [ex], [ss])
                            kb.op("dve", lambda e: e.reciprocal(out=rs1.t[:, :], in_=ss.t[:, :]), [ss], [rs1])
                            kb.op("dve", lambda e: e.tensor_scalar(out=gwA.t[:, tt, :], in0=ex.t[:, :], scalar1=rs1.t[:, 0:1], scalar2=None, op0=ALU.mult), [ex, rs1], [gwA])
                            for hf in range(2):
                                p2 = kb.psum()
                                for kk in range(KD // 2):
                                    k = hf * (KD // 2) + kk
                                    kb.op("pe", mm(p2.t[:, kk * 128:(kk + 1) * 128], h2.t[:, k, cs], identb), [h2, cb], [p2])
                                evac(h2tm.t[:, tt, hf * 512:(hf + 1) * 512], h2tm, p2, p2.t[:, 0:512])
                    kb.barrier()
                with contextlib.ExitStack() as st2:
                    selb = kb.tile(st2, [128, NTT, 8], BF16, "selb")
                    cnt = kb.tile(st2, [128, 8], F32, "cnt")
                    nblk = kb.tile(st2, [128, 8], F32, "nblk")
                    pend = kb.tile(st2, [128, 8], F32, "pend")
                    pstart = kb.tile(st2, [128, 8], F32, "pstart")
                    tmpT = kb.tile(st2, [128, NTHR], F32, "tmpT")
                    sT = kb.tile(st2, [128, 1], F32, "sT")
                    bexp = kb.tile(st2, [128, NB], F32, "bexp")
                    tmpB = kb.tile(st2, [128, NB], F32, "tmpB")
                    eoff = kb.tile(st2, [128, NB], F32, "eoff")
                    idxf = kb.tile(st2, [128, NB * KF], F32, "idxf")
                    posf = kb.tile(st2, [128, 8], F32, "posf")
                    sel2 = kb.tile(st2, [128, 8], F32, "sel2")
                    tm8 = kb.tile(st2, [128, 8], F32, "tm8")
                    posAB = kb.tile(st2, [128, NTT * 2], F32, "posAB")
                    kb.op("dve", lambda e: e.tensor_copy(out=selb.t[:, :, :], in_=selA.t[:, :, :]), [selA], [selb])
                    pc = kb.psum()
                    for tt in range(NTT):
                        kb.op("pe", mm(pc.t[:, 0:8], ones128, selb.t[:, tt, :], tt == 0, tt == NTT - 1), [selb, cb], [pc])
                    kb.op("dve", lambda e: e.tensor_copy(out=cnt.t[:, :], in_=pc.t[:, 0:8]), [pc], [cnt])
                    for e_ in range(E):
                        kb.op("dve", lambda e: e.tensor_scalar(out=tmpT.t[:, :], in0=thr_c, scalar1=cnt.t[:, e_:e_ + 1], scalar2=None, op0=ALU.is_ge), [cf, cnt], [tmpT])
                        kb.op("dve", lambda e: e.reduce_sum(out=sT.t[:, :], in_=tmpT.t[:, :], axis=AX.X), [tmpT], [sT])
                        kb.op("dve", lambda e: e.tensor_scalar(out=nblk.t[:, e_:e_ + 1], in0=sT.t[:, :], scalar1=-1.0, scalar2=float(NTHR), op0=ALU.mult, op1=ALU.add), [sT], [nblk])
                    kb.op("dve", lambda e: e.tensor_copy(out=pend.t[:, 0:1], in_=nblk.t[:, 0:1]), [nblk], [pend])
                    for e_ in range(1, E):
                        kb.op("dve", lambda e: e.tensor_tensor(out=pend.t[:, e_:e_ + 1], in0=pend.t[:, e_ - 1:e_], in1=nblk.t[:, e_:e_ + 1], op=ALU.add), [nblk], [pend])
                    kb.op("dve", lambda e: e.tensor_tensor(out=pstart.t[:, :], in0=pend.t[:, :], in1=nblk.t[:, :], op=ALU.subtract), [pend, nblk], [pstart])
                    kb.op("dve", lambda e: e.tensor_scalar(out=pstart.t[:, :], in0=pstart.t[:, :], scalar1=float(BS), scalar2=None, op0=ALU.mult), [], [pstart])
                    for e_ in range(E):
                        if e_ == 0:
                            kb.op("dve", lambda e: e.tensor_scalar(out=bexp.t[:, :], in0=jrow_c[:, 0:NB], scalar1=pend.t[:, 0:1], scalar2=None, op0=ALU.is_ge), [cf, pend], [bexp])
                        else:
                            kb.op("dve", lambda e: e.tensor_scalar(out=tmpB.t[:, :], in0=jrow_c[:, 0:NB], scalar1=pend.t[:, e_:e_ + 1], scalar2=None, op0=ALU.is_ge), [cf, pend], [tmpB])
                            kb.op("dve", lambda e: e.tensor_tensor(out=bexp.t[:, :], in0=bexp.t[:, :], in1=tmpB.t[:, :], op=ALU.add), [tmpB], [bexp])
                    kb.op("dve", lambda e: e.tensor_scalar(out=bexp.t[:, :], in0=bexp.t[:, :], scalar1=float(E - 1), scalar2=None, op0=ALU.min), [], [bexp])
                    for (mult_, base_, dstI) in ((float(KF * 128), 0.0, gidxI), (float(F), 0.0, didxI)):
                        kb.op("dve", lambda e: e.tensor_scalar(out=eoff.t[:, :], in0=bexp.t[:, :], scalar1=mult_, scalar2=base_, op0=ALU.mult, op1=ALU.add), [bexp], [eoff])
                        for j in range(NB):
                            kb.op("dve", lambda e: e.tensor_scalar(out=idxf.t[:, j * KF:(j + 1) * KF], in0=cidx_c, scalar1=eoff.t[:, j:j + 1], scalar2=None, op0=ALU.add), [cf, eoff], [idxf])
                        kb.op("dve", lambda e: e.tensor_copy(out=dstI.t[:, :], in_=idxf.t[:, :]), [idxf], [dstI])
                    for tt in range(NTT):
                        pp = kb.psum()
                        for t2_ in range(tt):
                            kb.op("pe", mm(pp.t[:, 0:8], ones128, selb.t[:, t2_, :], t2_ == 0, False), [selb, cb], [pp])
                        kb.op("pe", mm(pp.t[:, 0:8], ustrict, selb.t[:, tt, :], tt == 0, True), [selb, cb], [pp])
                        kb.op("dve", lambda e: e.tensor_tensor(out=posf.t[:, :], in0=pp.t[:, 0:8], in1=pstart.t[:, :], op=ALU.add), [pp, pstart], [posf])
                        kb.op("dve", lambda e: e.tensor_tensor(out=sel2.t[:, :], in0=selA.t[:, tt, :], in1=sel1A.t[:, tt, :], op=ALU.subtract), [selA, sel1A], [sel2])
                        for a_, (selX, selXb) in enumerate(((sel1A.t[:, tt, :], sel1A), (sel2.t[:, :], sel2))):
                            kb.op("dve", lambda e: e.tensor_tensor(out=tm8.t[:, :], in0=posf.t[:, :], in1=selX, op=ALU.mult), [posf, selXb], [tm8])
                            kb.op("dve", lambda e: e.reduce_sum(out=posAB.t[:, tt * 2 + a_:tt * 2 + a_ + 1], in_=tm8.t[:, :], axis=AX.X), [tm8], [posAB])
                            kb.op("dve", lambda e: e.tensor_tensor(out=tm8.t[:, :], in0=gwA.t[:, tt, :], in1=selX, op=ALU.mult), [gwA, selXb], [tm8])
                            kb.op("dve", lambda e: e.reduce_sum(out=wAB.t[:, tt * 2 + a_:tt * 2 + a_ + 1], in_=tm8.t[:, :], axis=AX.X), [tm8], [wAB])
                    kb.op("dve", lambda e: e.tensor_copy(out=posI.t[:, :], in_=posAB.t[:, :]), [posAB], [posI])
                    scs = [Buf("scs%d" % i_) for i_ in range(8)]
                    for tt in range(NTT):
                        for a_ in range(2):
                            kb.idma(XsD[:, :], bass.IndirectOffsetOnAxis(ap=posI.t[:, tt * 2 + a_:tt * 2 + a_ + 1], axis=0), h2tm.t[:, tt, :], None, [h2tm, posI, dXs], [Buf("snk")], scs[(tt * 2 + a_) % 8])
                    kb.barrier()
            with contextlib.ExitStack() as st1:
                Xg = [kb.tile(st1, [128, SUB, D], BF16, "Xg") for _ in range(2)]
                XTb = kb.tile(st1, [128, KD, BS], BF16, "XTb")
                actT = kb.tile(st1, [128, KF, BS], BF16, "actT")
                gbb = [kb.tile(st1, [128, KD * 256], BF16, "gbb") for _ in range(4)]
                dbb = [kb.tile(st1, [128, D], BF16, "dbb") for _ in range(4)]
                sl = [kb.tile(st1, [128, BS], BF16, "silu") for _ in range(3)]
                yst = [kb.tile(st1, [128, D], F32, "yst") for _ in range(2)]
                wc = 0
                for j in range(NB):
                    xg = Xg[j % 2]
                    kb.dma("sp", xg.t[:, :, :], XsD[j * BS:(j + 1) * BS, :].rearrange("(s p) d -> p s d", p=128), [dXs], [xg], xg)
                    for k in range(KD):
                        p = kb.psum()
                        for s_ in range(SUB):
                            kb.op("pe", mm(p.t[:, s_ * 128:(s_ + 1) * 128], xg.t[:, s_, k * 128:(k + 1) * 128], identb), [xg, cb], [p])
                        evac(XTb.t[:, k, :], XTb, p, p.t[:, 0:BS])
                    for f in range(KF):
                        gb = gbb[wc % 4]; wc += 1
                        kb.idma(gb.t[:, :], None, WbfG[:, :], bass.IndirectOffsetOnAxis(ap=gidxI.t[:, j * KF + f:j * KF + f + 1], axis=0), [gidxI, dWbf], [gb], gb)
                        pg = kb.psum(); pu = kb.psum()
                        for k in range(KD):
                            kb.op("pe", mm(pg.t[:, 0:BS], gb.t[:, k * 256:k * 256 + 128], XTb.t[:, k, :], k == 0, k == KD - 1), [gb, XTb], [pg])
                        for k in range(KD):
                            kb.op("pe", mm(pu.t[:, 0:BS], gb.t[:, k * 256 + 128:k * 256 + 256], XTb.t[:, k, :], k == 0, k == KD - 1), [gb, XTb], [pu])
                        sl_ = sl[f % 3]
                        kb.op("act", lambda e: e.activation(out=sl_.t[:, :], in_=pg.t[:, 0:BS], func=AF.Silu), [pg], [sl_])
                        kb.op("dve", lambda e: e.tensor_tensor(out=actT.t[:, f, :], in0=pu.t[:, 0:BS], in1=sl_.t[:, :], op=ALU.mult), [pu, sl_], [actT])
                    for kf in range(KF):
                        db = dbb[wc % 4]; wc += 1
                        kb.idma(db.t[:, :], None, WbfD[:, :], bass.IndirectOffsetOnAxis(ap=didxI.t[:, j * KF + kf:j * KF + kf + 1], axis=0), [didxI, dWbf], [db], db)
                        for s_ in range(SUB):
                            for hf in range(D // 512):
                                pb_ = kb.ps[(s_ * (D // 512) + hf) % 8]
                                kb.op("pe", mm(pb_.t[:, 0:512], actT.t[:, kf, s_ * 128:(s_ + 1) * 128], db.t[:, hf * 512:(hf + 1) * 512], kf == 0, kf == KF - 1), [actT, db], [pb_])
                    for s_ in range(SUB):
                        y_ = yst[s_ % 2]
                        for hf in range(D // 512):
                            pb_ = kb.ps[(s_ * (D // 512) + hf) % 8]
                            evac(y_.t[:, hf * 512:(hf + 1) * 512], y_, pb_, pb_.t[:, 0:512])
                        r0 = j * BS + s_ * 128
                        kb.dma("sp", YsD[r0:r0 + 128, :], y_.t[:, :], [y_], [dYs], y_)
                kb.barrier()
            with contextlib.ExitStack() as st1:
                yA = [kb.tile(st1, [128, D], F32, "yA") for _ in range(4)]
                yB = [kb.tile(st1, [128, D], F32, "yB") for _ in range(4)]
                u4 = [kb.tile(st1, [128, 4, D], F32, "u4") for _ in range(2)]
                xts = [kb.tile(st1, [128, 512], F32, "x5") for _ in range(3)]
                tr5 = [kb.tile(st1, [128, 512], F32, "tr5") for _ in range(2)]
                xc = 0
                for qi, ci in enumerate(clist):
                    t0, n = cfg.chunks[ci]
                    s_i = 1 if ci == 0 else 0
                    u_ = u4[qi % 2]
                    for tl_ in range(n // 128):
                        tt = (t0 - tok0) // 128 + tl_
                        a_ = yA[tt % 4]; b_ = yB[tt % 4]
                        kb.idma(a_.t[:, :], None, YsD[:, :], bass.IndirectOffsetOnAxis(ap=posI.t[:, tt * 2:tt * 2 + 1], axis=0), [posI, dYs], [a_], a_)
                        kb.idma(b_.t[:, :], None, YsD[:, :], bass.IndirectOffsetOnAxis(ap=posI.t[:, tt * 2 + 1:tt * 2 + 2], axis=0), [posI, dYs], [b_], b_)
                        kb.op("dve", lambda e: e.tensor_scalar(out=u_.t[:, tl_, :], in0=a_.t[:, :], scalar1=wAB.t[:, tt * 2:tt * 2 + 1], scalar2=None, op0=ALU.mult), [a_, wAB], [u_])
                        kb.op("dve", lambda e: e.scalar_tensor_tensor(out=u_.t[:, tl_, :], in0=b_.t[:, :], scalar=wAB.t[:, tt * 2 + 1:tt * 2 + 2], in1=u_.t[:, tl_, :], op0=ALU.mult, op1=ALU.add), [b_, wAB], [u_])
                    for k in range(KD):
                        p = kb.psum()
                        for tl_ in range(n // 128):
                            kb.op("pe", mm(p.t[:, tl_ * 128:(tl_ + 1) * 128], u_.t[:, tl_, k * 128:(k + 1) * 128], id128f), [u_, cf], [p])
                        tr_ = tr5[xc % 2]
                        x_ = xts[xc % 3]; xc += 1
                        js = slice(k * 128, (k + 1) * 128)
                        kb.dma("sp", x_.t[:, 0:n], XT[js, t0:t0 + n], [dummy], [x_], x_)
                        kb.op("act", lambda e: e.activation(out=tr_.t[:, 0:n], in_=p.t[:, 0:n], func=AF.Identity, scale=mod(l, 5, k, s_i)), [p, modv], [tr_])
                        kb.op("dve", lambda e: e.tensor_tensor(out=x_.t[:, 0:n], in0=x_.t[:, 0:n], in1=tr_.t[:, 0:n], op=ALU.add), [tr_], [x_])
                        kb.dma("pool", XT[js, t0:t0 + n], x_.t[:, 0:n], [x_], [Buf("snk")], x_)
                kb.barrier()

    for l in range(L):
        last = l == L - 1
        moe = l % 2 == 1
        xsrc = xT_in if l == 0 else XT
        nch = len(cfg.chunks)
        halves = [list(range(0, (nch + 1) // 2)), list(range((nch + 1) // 2, nch))]
        for hchunks in halves:
          if not hchunks:
              continue
          hb = cfg.chunks[hchunks[0]][0]
          W = sum(cfg.chunks[ci][1] for ci in hchunks)
          with contextlib.ExitStack() as st:
            hT = kb.tile(st, [128, KD, W], BF16, "hT")
            with contextlib.ExitStack() as st2:
                nts = [norm_tiles(st2) for _ in range(2)]
                for i_, ci in enumerate(hchunks):
                    t0, n = cfg.chunks[ci]
                    norm_chunk(nts[i_ % 2], xsrc, l, ci, V_NMIX, 0, 1, hT, t0 - hb)
                kb.barrier()
            rt = kb.tile(st, [128, 4, W], BF16, "ropet")
            if stop == "P1a":
                return nc
            kb.dma("sp", rt.t[:, :, :], ropet[:, :, hb:hb + W], [dummy], [rt], rt)
            ropeg_cos = rt.t[:, 0, :]; ropeg_sin = rt.t[:, 1, :]; ropem_cos = rt.t[:, 2, :]; ropem_sin = rt.t[:, 3, :]
            stgs = [kb.tile(st, [128, KD * 128], F32, "wstg") for _ in range(3)]
            wbs = [kb.tile(st, [128, KD, 128], BF16, "wb") for _ in range(4)]
            outs = [kb.tile(st, [128, 512], BF16, "o1") for _ in range(4)]
            sqb = [kb.tile(st, [128, 512], BF16, "sqb") for _ in range(2)]
            rsb = [kb.tile(st, [128, 512], F32, "rsb") for _ in range(2)]
            tf = [kb.tile(st, [128, 512], F32, "tf") for _ in range(4)]
            cnt = {"w": 0, "o": 0, "s": 0, "t": 0, "r": 0}

            wseq = [0, 1, 2, 3, 7, 8, 9, 13, 10, 14, 11, 15, 12, 16] + list(range(19, 19 + 24)) + [4, 17, 18, 5, 6]
            wstate = {"issued": 0, "ready": {}}

            def issue_next():
                i = wstate["issued"]
                if i >= len(wseq):
                    return
                sg = stgs[i % 3]; wb_ = wbs[i % 4]
                kb.dma("sp", sg.t[:, :], w1[l, wseq[i]], [dummy], [sg], sg)
                cast(wb_.t[:, :, :], wb_, sg.t[:, :].rearrange("p (k c) -> p k c", k=KD), sg)
                wstate["ready"][i] = wb_
                wstate["issued"] += 1

            def getw(nt):
                i = cnt["w"]; cnt["w"] += 1
                assert wseq[i] == nt
                while wstate["issued"] <= i + 1 and wstate["issued"] < len(wseq):
                    issue_next()
                return wstate["ready"].pop(i)

            def proj(wb_, ci):
                t0, n = cfg.chunks[ci]
                p = kb.psum()
                for k in range(KD):
                    kb.op("pe", mm(p.t[:, 0:n], wb_.t[:, k, :], hT.t[:, k, t0 - hb:t0 - hb + n], k == 0, k == KD - 1), [wb_, hT], [p])
                return p

            def nxt(lst, key):
                i = cnt[key]; cnt[key] += 1
                return lst[i % len(lst)]

            def store(o_, n, dst_ap, dbuf):
                kb.dma("pool", dst_ap, o_.t[:, 0:n], [o_], [dbuf], o_)

            def plain_group(tiles, dst, dbuf, func=None):
                for j, nt in enumerate(tiles):
                    wb_ = getw(nt)
                    for ci in hchunks:
                        t0, n = cfg.chunks[ci]
                        p = proj(wb_, ci)
                        o_ = nxt(outs, "o")
                        evac(o_.t[:, 0:n], o_, p, p.t[:, 0:n], func)
                        store(o_, n, dst[j * 128:(j + 1) * 128, t0:t0 + n], dbuf)

            def sq_rstd(plist, ones_ap, dim, n):
                rs_ = nxt(rsb, "r")
                pc = kb.psum()
                for i_, pa in enumerate(plist):
                    sq_ = nxt(sqb, "s")
                    kb.op("act", lambda e: e.activation(out=sq_.t[:, 0:n], in_=pa.t[:, 0:n], func=AF.Square), [pa], [sq_])
                    kb.op("pe", mm(pc.t[:, 0:n], ones_ap, sq_.t[:, 0:n], i_ == 0, i_ == len(plist) - 1), [sq_, cb], [pc])
                rstd_from(pc, n, dim, rs_)
                return rs_

            def rope_norm_group(tiles, rtiles, gcol, grcol, dst, dbuf):
                for j in range(len(tiles)):
                    wa = getw(tiles[j]); wr_ = getw(rtiles[j])
                    for ci in hchunks:
                        t0, n = cfg.chunks[ci]
                        c0 = t0 - hb
                        pa = proj(wa, ci); pb = proj(wr_, ci)
                        STEP = int(os.environ.get("STEP", "99"))
                        if STEP < 1:
                            continue
                        rs_ = sq_rstd([pa], bd64, 64, n)
                        t1 = nxt(tf, "t"); t2 = nxt(tf, "t")
                        if STEP < 2:
                            continue
                        kb.op("act", lambda e: e.activation(out=t1.t[:, 0:n], in_=pa.t[:, 0:n], func=AF.Identity, scale=vcol(l, gcol)), [pa, vc], [t1])
                        kb.op("act", lambda e: e.activation(out=t2.t[:, 0:n], in_=pb.t[:, 0:n], func=AF.Identity, scale=vcol(l, grcol)), [pb, vc], [t2])
                        kb.op("dve", lambda e: e.tensor_tensor(out=t1.t[:, 0:n], in0=t1.t[:, 0:n], in1=ropeg_cos[:, c0:c0 + n], op=ALU.mult), [rt], [t1])
                        kb.op("dve", lambda e: e.tensor_tensor(out=t2.t[:, 0:n], in0=t2.t[:, 0:n], in1=ropeg_sin[:, c0:c0 + n], op=ALU.mult), [rt], [t2])
                        if STEP < 3:
                            continue
                        kb.op("pool", lambda e: e.tensor_tensor(out=t1.t[:, 0:n], in0=t1.t[:, 0:n], in1=t2.t[:, 0:n], op=ALU.add), [t2], [t1])
                        o_ = nxt(outs, "o")
                        if STEP < 4:
                            continue
                        kb.op("dve", lambda e: e.tensor_tensor(out=o_.t[:, 0:n], in0=t1.t[:, 0:n], in1=rs_.t[:, 0:n], op=ALU.mult), [t1, rs_], [o_])
                        store(o_, n, dst[j * 128:(j + 1) * 128, t0:t0 + n], dbuf)

            plain_group([0, 1], KnaT, dQK["KnaT"])
            if stop == "P1b":
                kb.barrier(); return nc
            rope_norm_group([2], [3], V_KG, V_KGR, KgT, dQK["KgT"])
            if stop == "P1c":
                kb.barrier(); return nc
            plain_group([7, 8], QnaT, dQK["QnaT"])
            rope_norm_group([9, 10, 11, 12], [13, 14, 15, 16], V_QG, V_QGR, QgT, dQK["QgT"])
            plain_group(list(range(19, 19 + 24)), GT, dQK["GT"], func=AF.Sigmoid)
            if stop == "P1d":
                kb.barrier(); return nc
            ckvn = kb.tile(st, [128, W], BF16, "ckvn")
            cqn = kb.tile(st, [128, 2, W], BF16, "cqn")
            krope = kb.tile(st, [128, W], BF16, "krope")
            wa = getw(4)
            for ci in hchunks:
                t0, n = cfg.chunks[ci]
                c0 = t0 - hb
                pa = proj(wa, ci)
                rs_ = sq_rstd([pa], ones128, 128, n)
                t1 = nxt(tf, "t")
                kb.op("act", lambda e: e.activation(out=t1.t[:, 0:n], in_=pa.t[:, 0:n], func=AF.Identity, scale=vcol(l, V_KVL)), [pa, vc], [t1])
                kb.op("dve", lambda e: e.tensor_tensor(out=ckvn.t[:, c0:c0 + n], in0=t1.t[:, 0:n], in1=rs_.t[:, 0:n], op=ALU.mult), [t1, rs_], [ckvn])
            wa = getw(17); wb2 = getw(18)
            for ci in hchunks:
                t0, n = cfg.chunks[ci]
                c0 = t0 - hb
                pa = proj(wa, ci); pb = proj(wb2, ci)
                rs_ = sq_rstd([pa, pb], ones128, 256, n)
                t1 = nxt(tf, "t"); t2 = nxt(tf, "t")
                kb.op("act", lambda e: e.activation(out=t1.t[:, 0:n], in_=pa.t[:, 0:n], func=AF.Identity, scale=vcol(l, V_QL)), [pa, vc], [t1])
                kb.op("act", lambda e: e.activation(out=t2.t[:, 0:n], in_=pb.t[:, 0:n], func=AF.Identity, scale=vcol(l, V_QL + 1)), [pb, vc], [t2])
                kb.op("dve", lambda e: e.tensor_tensor(out=cqn.t[:, 0, c0:c0 + n], in0=t1.t[:, 0:n], in1=rs_.t[:, 0:n], op=ALU.mult), [t1, rs_], [cqn])
                kb.op("dve", lambda e: e.tensor_tensor(out=cqn.t[:, 1, c0:c0 + n], in0=t2.t[:, 0:n], in1=rs_.t[:, 0:n], op=ALU.mult), [t2, rs_], [cqn])
            wa = getw(5); wb2 = getw(6)
            for ci in hchunks:
                t0, n = cfg.chunks[ci]
                c0 = t0 - hb
                pa = proj(wa, ci); pb = proj(wb2, ci)
                t1 = nxt(tf, "t"); t2 = nxt(tf, "t")
                kb.op("dve", lambda e: e.tensor_tensor(out=t1.t[64:96, 0:n], in0=pa.t[64:96, 0:n], in1=ropem_cos[64:96, c0:c0 + n], op=ALU.mult), [pa, rt], [t1])
                kb.op("dve", lambda e: e.tensor_tensor(out=t2.t[64:96, 0:n], in0=pb.t[64:96, 0:n], in1=ropem_sin[64:96, c0:c0 + n], op=ALU.mult), [pb, rt], [t2])
                kb.op("dve", lambda e: e.tensor_tensor(out=krope.t[64:96, c0:c0 + n], in0=t1.t[64:96, 0:n], in1=t2.t[64:96, 0:n], op=ALU.add), [t1, t2], [krope])
            w2s = kb.tile(st, [128, 1536], F32, "w2s")
            wuq_b = kb.tile(st, [128, 2, 384], BF16, "wuq")
            wuqr_b = kb.tile(st, [128, 2, 384], BF16, "wuqr")
            wkk_b = kb.tile(st, [128, 256], BF16, "wkk")
            wkv_b = kb.tile(st, [128, 256], BF16, "wkv")
            wv_b = kb.tile(st, [128, KD, 384], BF16, "wvb")
            for src_, dst_, wd_, db_ in ((wuq[l], wuq_b.t[:, :, :].rearrange("p a b -> p (a b)"), 768, wuq_b), (wuqr[l], wuqr_b.t[:, :, :].rearrange("p a b -> p (a b)"), 768, wuqr_b),
                                        (wukvk[l], wkk_b.t[:, :], 256, wkk_b), (wukvv[l], wkv_b.t[:, :], 256, wkv_b),
                                        (wv[l][:, 0:1536], wv_b.t[:, 0:KD // 2, :].rearrange("p a b -> p (a b)"), 1536, wv_b),
                                        (wv[l][:, 1536:3072], wv_b.t[:, KD // 2:KD, :].rearrange("p a b -> p (a b)"), 1536, wv_b)):
                kb.dma("sp", w2s.t[:, 0:wd_], src_, [dummy], [w2s], w2s)
                kb.op("dve", lambda e: e.tensor_copy(out=dst_, in_=w2s.t[:, 0:wd_]), [w2s], [db_])
            kst = [kb.tile(st, [128, 512], BF16, "kst") for _ in range(3)]
            if stop == "P1e":
                kb.barrier(); return nc
            for ci in hchunks:
                t0, n = cfg.chunks[ci]
                c0 = t0 - hb
                for h in range(4):
                    p = kb.psum()
                    kb.op("pe", mm(p.t[0:64, 0:n], wkk_b.t[:, h * 64:(h + 1) * 64], ckvn.t[:, c0:c0 + n]), [wkk_b, ckvn], [p])
                    o_ = nxt(kst, "o")
                    evac(o_.t[0:64, 0:n], o_, p, p.t[0:64, 0:n])
                    kb.op("pool", lambda e: e.tensor_copy(out=o_.t[64:96, 0:n], in_=krope.t[64:96, c0:c0 + n]), [krope], [o_])
                    kb.dma("pool", KmT[h * 96:(h + 1) * 96, t0:t0 + n], o_.t[0:96, 0:n], [o_], [dQK["KmT"]], o_)
                    p = kb.psum(); pr = kb.psum()
                    for k in range(2):
                        kb.op("pe", mm(p.t[0:96, 0:n], wuq_b.t[:, k, h * 96:(h + 1) * 96], cqn.t[:, k, c0:c0 + n], k == 0, k == 1), [wuq_b, cqn], [p])
                    for k in range(2):
                        kb.op("pe", mm(pr.t[0:96, 0:n], wuqr_b.t[:, k, h * 96:(h + 1) * 96], cqn.t[:, k, c0:c0 + n], k == 0, k == 1), [wuqr_b, cqn], [pr])
                    o_ = nxt(kst, "o")
                    evac(o_.t[0:64, 0:n], o_, p, p.t[0:64, 0:n])
                    t1 = nxt(tf, "t"); t2 = nxt(tf, "t")
                    kb.op("dve", lambda e: e.tensor_tensor(out=t1.t[64:96, 0:n], in0=p.t[64:96, 0:n], in1=ropem_cos[64:96, c0:c0 + n], op=ALU.mult), [p, rt], [t1])
                    kb.op("dve", lambda e: e.tensor_tensor(out=t2.t[64:96, 0:n], in0=pr.t[64:96, 0:n], in1=ropem_sin[64:96, c0:c0 + n], op=ALU.mult), [pr, rt], [t2])
                    kb.op("dve", lambda e: e.tensor_tensor(out=o_.t[64:96, 0:n], in0=t1.t[64:96, 0:n], in1=t2.t[64:96, 0:n], op=ALU.add), [t1, t2], [o_])
                    kb.dma("pool", QmT[h * 96:(h + 1) * 96, t0:t0 + n], o_.t[0:96, 0:n], [o_], [dQK["QmT"]], o_)
            if stop == "P1f":
                kb.barrier(); return nc
            vst = [kb.tile(st, [128, 10, 65], BF16, "vst") for _ in range(3)]
            for v_ in vst:
                kb.op("pool", lambda e: e.memset(v_.t[:, :, :], 1.0), [], [v_])
            for tt in range(hb // 128, (hb + W) // 128):
                c0 = tt * 128 - hb
                p = kb.psum(); p2 = kb.psum()
                for k in range(KD):
                    kb.op("pe", mm(p.t[:, 0:384], hT.t[:, k, c0:c0 + 128], wv_b.t[:, k, :], k == 0, k == KD - 1), [hT, wv_b], [p])
                kb.op("pe", mm(p2.t[:, 0:256], ckvn.t[:, c0:c0 + 128], wkv_b.t[:, :]), [ckvn, wkv_b], [p2])
                v_ = nxt(vst, "o")
                kb.op("act", lambda e: e.activation(out=v_.t[:, 0:6, 0:64], in_=p.t[:, 0:384].rearrange("p (h d) -> p h d", h=6), func=AF.Copy), [p], [v_])
                kb.op("dve", lambda e: e.tensor_copy(out=v_.t[:, 6:10, 0:64], in_=p2.t[:, 0:256].rearrange("p (h d) -> p h d", h=4)), [p2], [v_])
                rs_ = slice(tt * 128, (tt + 1) * 128)
                kb.dma("pool", Vna[rs_, :].rearrange("t (h d) -> t h d", h=4), v_.t[:, 0:4, :], [v_], [dQK["Vna"]], v_)
                kb.dma("pool", Vg[rs_, :].rearrange("t (h d) -> t h d", h=2), v_.t[:, 4:6, :], [v_], [dQK["Vg"]], v_)
                kb.dma("pool", Vm[rs_, :].rearrange("t (h d) -> t h d", h=4), v_.t[:, 6:10, :], [v_], [dQK["Vm"]], v_)
            kb.barrier()

        if stop == "P1":
            return nc
        PS_S = (0, 4); PS_O = (4, 2); PS_B = (6, 2)

        def attn_phase(name, dk, nh, nkv, Ksrc, Qsrc, Vsrc, scale, yrow0, na=False):
            with contextlib.ExitStack() as st:
                pk = 128 if dk == 64 else dk
                Kt = kb.tile(st, [pk, nkv, T], BF16, "K" + name)
                Vt = kb.tile(st, [128, NKT, nkv, 65], BF16, "V" + name)
                if pk != dk:
                    kb.op("pool", lambda e: e.memset(Kt.t[dk:pk, :, :], 0.0), [], [Kt])
                kb.dma("sp", Kt.t[0:dk, :, :], Ksrc.rearrange("(h d) t -> d h t", d=dk), [dQK["K%sT" % name]], [Kt], Kt)
                kb.dma("sp", Vt.t[:, :, :, :], Vsrc.rearrange("(k p) (h d) -> p k h d", p=128, d=65), [dQK["V" + name]], [Vt], Vt)
                if na:
                    Vo = kb.tile(st, [128, NKT - CT - 1, nkv, 65], BF16, "Vo")
                    kb.dma("sp", Vo.t[:, :, :, :], Vsrc[C + 64:T - 64, :].rearrange("(k p) (h d) -> p k h d", p=128, d=65), [dQK["V" + name]], [Vo], Vo)
                    rst = kb.tile(st, [64, 3840], F32, "rpbst")
                    Tc = kb.tile(st, [128, 4, 15, 64], BF16, "Tcat")
                    kb.op("pool", lambda e: e.memset(Tc.t[64:128, :, :, :], 0.0), [], [Tc])
                    kb.dma("sp", rst.t[:, :], rpbT[l], [dummy], [rst], rst)
                    ngm = kb.tile(st, [64, 3840], BF16, "negm")
                    kb.dma("sp", ngm.t[:, :], negm[:, :], [dummy], [ngm], ngm)
                    kb.op("dve", lambda e: e.scalar_tensor_tensor(out=Tc.t[0:64, :, :, :].rearrange("p a b c -> p (a b c)"), in0=rst.t[:, :], scalar=8.0, in1=ngm.t[:, :], op0=ALU.mult, op1=ALU.add), [rst, ngm], [Tc])
                Qs = [kb.tile(st, [pk, nh, 512], BF16, "Q" + name) for _ in range(2)]
                if pk != dk:
                    for q_ in Qs:
                        kb.op("pool", lambda e, q_=q_: e.memset(q_.t[dk:pk, :, :], 0.0), [], [q_])
                Ps = [kb.tile(st, [128, 512], BF16, "P" + name) for _ in range(4)]
                Rt = kb.tile(st, [65, 512], F32, "Rt")
                bcs = [kb.tile(st, [64, 512], F32, "bc") for _ in range(2)]
                ys = [kb.tile(st, [64, 512], BF16, "ys") for _ in range(3)]
                kb.op("pool", lambda e: e.memset(Rt.t[:, :], 0.0), [], [Rt])
                if conv.active():
                    conv.attach(st)
                c_ = {"p": 0, "y": 0, "b": 0}
                pnorm = {"f": None}
                clist = list(range(len(cfg.chunks)))
                if last:
                    clist = clist[1:]
                for qi, ci in enumerate(clist):
                    t0, n = cfg.chunks[ci]
                    Q = Qs[qi % 2]
                    kb.dma("sp", Q.t[0:dk, :, 0:n], Qsrc[:, t0:t0 + n].rearrange("(h d) t -> d h t", d=dk), [dQK["Q%sT" % name]], [Q], Q)
                    for h in range(nh):
                        kvh = h * nkv // nh
                        po = kb.psum(PS_O)
                        if ci == 0 or not na:
                            kts = list(range(CT)) if ci == 0 else list(range(NKT))
                            LA = 3
                            pend = []
                            for i_, kt in enumerate(kts):
                                ps_ = kb.psum(PS_S)
                                kb.op("pe", mm(ps_.t[:, 0:n], Kt.t[:, kvh, kt * 128:(kt + 1) * 128], Q.t[:, h, 0:n]), [Kt, Q], [ps_])
                                pend.append((ps_, kt))
                                if i_ == min(LA, len(kts) - 1) and pnorm["f"] is not None:
                                    pnorm["f"](); pnorm["f"] = None
                                if len(pend) > LA or i_ == len(kts) - 1:
                                    while pend and (len(pend) > LA or i_ == len(kts) - 1):
                                        ps2, kt2 = pend.pop(0)
                                        P = Ps[c_["p"] % 4]; c_["p"] += 1
                                        kb.op("act", lambda e, P=P, ps2=ps2: e.activation(out=P.t[:, 0:n], in_=ps2.t[:, 0:n], func=AF.Exp, scale=scale), [ps2], [P])
                                        kb.op("pe", mm(po.t[0:65, 0:n], Vt.t[:, kt2, kvh, :], P.t[:, 0:n], kt2 == kts[0], kt2 == kts[-1]), [Vt, P], [po])
                        else:
                            r0 = (t0 - C) // 64
                            pendu = []

                            def finish_unit(u):
                                ps_u, groups_u, qs_u = u
                                ng = len(groups_u)
                                P = Ps[c_["p"] % 4]; c_["p"] += 1
                                kb.op("act", lambda e: e.activation(out=P.t[:, 0:ng * 64], in_=ps_u.t[:, 0:ng * 64], func=AF.Exp, scale=scale), [ps_u], [P])
                                for gi, (tk, vt, vb, dr) in enumerate(groups_u):
                                    kb.op("pe", mm(po.t[0:65, qs_u], vt, P.t[:, gi * 64:(gi + 1) * 64], gi == 0, gi == ng - 1), [vb, P], [po])
                            for rl in range(n // 64):
                                r = r0 + rl
                                row0 = min(max(r - 4, 0), R - 8)
                                qs = slice(rl * 64, (rl + 1) * 64)
                                ps_ = kb.psum(PS_S)
                                groups = []
                                for g in range(4):
                                    tk = C + (row0 + 2 * g) * 64
                                    if row0 % 2 == 0:
                                        vt = Vt.t[:, tk // 128, kvh, :]
                                        vb = Vt
                                    else:
                                        vt = Vo.t[:, (tk - C - 64) // 128, kvh, :]
                                        vb = Vo
                                    groups.append((tk, vt, vb, row0 + 2 * g - r))
                                for g in range(CT):
                                    groups.append((g * 128, Vt.t[:, g, kvh, :], Vt, None))
                                for gi, (tk, vt, vb, dr) in enumerate(groups):
                                    osl = slice(gi * 64, (gi + 1) * 64)
                                    kb.op("pe", mm(ps_.t[:, osl], Kt.t[:, kvh, tk:tk + 128], Q.t[:, h, qs], True, dr is None), [Kt, Q], [ps_])
                                    if dr is not None:
                                        kb.op("pe", mm(ps_.t[:, osl], Tc.t[:, h, dr + 7:dr + 9, :].rearrange("p a b -> p (a b)"), id64b, False, True), [Tc, cb], [ps_])
                                pendu.append((ps_, groups, qs))
                                if rl == 0 and pnorm["f"] is not None:
                                    pnorm["f"](); pnorm["f"] = None
                                if len(pendu) > 2:
                                    finish_unit(pendu.pop(0))
                            while pendu:
                                finish_unit(pendu.pop(0))
                        kb.op("dve", lambda e, po=po: e.reciprocal(out=Rt.t[64:65, 0:n], in_=po.t[64:65, 0:n]), [po], [Rt])
                        pb_ = kb.psum(PS_B)
                        kb.op("pe", mm(pb_.t[:, 0:n], sel64, Rt.t[0:65, 0:n]), [cf, Rt], [pb_])

                        def rest(po=po, pb_=pb_, h=h, t0=t0, n=n):
                            bc = bcs[c_["b"] % 2]; c_["b"] += 1
                            kb.op("dve", lambda e: e.tensor_copy(out=bc.t[:, 0:n], in_=pb_.t[0:64, 0:n]), [pb_], [bc])
                            y_ = ys[c_["y"] % 3]; c_["y"] += 1
                            kb.op("dve", lambda e: e.tensor_tensor(out=y_.t[:, 0:n], in0=po.t[0:64, 0:n], in1=bc.t[:, 0:n], op=ALU.mult), [po, bc], [y_])
                            kb.dma("pool", YT[yrow0 + h * 64:yrow0 + (h + 1) * 64, t0:t0 + n], y_.t[:, 0:n], [y_], [dQK["YT"]], y_)
                            conv.step()
                        pnorm["f"] = rest
                if pnorm["f"] is not None:
                    pnorm["f"](); pnorm["f"] = None
                conv.drain(everything=(moe and name == "m"))
                kb.barrier()

        if l + 1 < L and (l + 1) % 2 == 1:
            conv.add_layer((l + 1) // 2)
        elif l == 0 and moe:
            conv.add_layer(0)
        attn_phase("na", 64, 4, 4, KnaT, QnaT, Vna, 0.125, 0, na=True)
        if stop == "P2a":
            return nc
        attn_phase("g", 64, 8, 2, KgT, QgT, Vg, 0.125, 256)
        attn_phase("m", 96, 4, 4, KmT, QmT, Vm, 96 ** -0.5, 768)

        if stop == "P2":
            return nc
        with contextlib.ExitStack() as st:
            stg = [kb.tile(st, [128, 2048], F32, "w3s") for _ in range(2)]
            wo_b = kb.tile(st, [128, 8, D], BF16, "wo")
            wout_b = kb.tile(st, [128, KD, D], BF16, "wout")
            i = 0
            for src_, dst_ in ((wo[l], wo_b), (wout[l], wout_b)):
                for k in range(0, 8, 2):
                    sg = stg[i % 2]; i += 1
                    kb.dma("sp", sg.t[:, :], src_[:, k * D:(k + 2) * D], [dummy], [sg], sg)
                    cast(dst_.t[:, k:k + 2, :], dst_, sg.t[:, :].rearrange("p (a b) -> p a b", a=2), sg)
            Ys = [kb.tile(st, [128, 8, 512], BF16, "Y") for _ in range(2)]
            Gs = [kb.tile(st, [128, 24, 512], BF16, "G") for _ in range(2)]
            Xs = [kb.tile(st, [128, KD, 512], F32, "X") for _ in range(2)]
            mT = [kb.tile(st, [128, KD, 512], BF16, "m") for _ in range(2)]
            tfs = [kb.tile(st, [128, 512], F32, "t3") for _ in range(4)]
            tc = 0
            clist = list(range(len(cfg.chunks)))
            if last:
                clist = clist[1:]
            for qi, ci in enumerate(clist):
                t0, n = cfg.chunks[ci]
                s_ = 1 if ci == 0 else 0
                Y = Ys[qi % 2]; G = Gs[qi % 2]; X = Xs[qi % 2]; m_ = mT[qi % 2]
                kb.dma("sp", Y.t[:, :, 0:n], YT[:, t0:t0 + n].rearrange("(k p) t -> p k t", p=128), [dQK["YT"]], [Y], Y)
                kb.dma("sp", G.t[:, :, 0:n], GT[:, t0:t0 + n].rearrange("(k p) t -> p k t", p=128), [dQK["GT"]], [G], G)
                kb.dma("sp", X.t[:, :, 0:n], xsrc[:, t0:t0 + n].rearrange("(k p) t -> p k t", p=128), [dXT[ci]], [X], X)
                for j in range(KD):
                    js = slice(j * 128, (j + 1) * 128)
                    pa = kb.psum(); pb = kb.psum(); pc = kb.psum()
                    for k in range(2):
                        kb.op("pe", mm(pa.t[:, 0:n], wo_b.t[:, k, js], Y.t[:, k, 0:n], k == 0, k == 1), [wo_b, Y], [pa])
                    for k in range(4):
                        kb.op("pe", mm(pb.t[:, 0:n], wo_b.t[:, 2 + k, js], Y.t[:, 2 + k, 0:n], k == 0, k == 3), [wo_b, Y], [pb])
                    for k in range(2):
                        kb.op("pe", mm(pc.t[:, 0:n], wo_b.t[:, 6 + k, js], Y.t[:, 6 + k, 0:n], k == 0, k == 1), [wo_b, Y], [pc])
                    t1 = tfs[tc % 4]; t2 = tfs[(tc + 1) % 4]; tc += 2
                    kb.op("dve", lambda e, t1=t1, pa=pa, G=G, j=j: e.tensor_tensor(out=t1.t[:, 0:n], in0=pa.t[:, 0:n], in1=G.t[:, j, 0:n], op=ALU.mult), [pa, G], [t1])
                    kb.op("dve", lambda e, t2=t2, pb=pb, G=G, j=j: e.tensor_tensor(out=t2.t[:, 0:n], in0=pb.t[:, 0:n], in1=G.t[:, 8 + j, 0:n], op=ALU.mult), [pb, G], [t2])
                    kb.op("pool", lambda e, t1=t1, t2=t2: e.tensor_tensor(out=t1.t[:, 0:n], in0=t1.t[:, 0:n], in1=t2.t[:, 0:n], op=ALU.add), [t2], [t1])
                    kb.op("dve", lambda e, t2=t2, pc=pc, G=G, j=j: e.tensor_tensor(out=t2.t[:, 0:n], in0=pc.t[:, 0:n], in1=G.t[:, 16 + j, 0:n], op=ALU.mult), [pc, G], [t2])
                    kb.op("pool", lambda e, t1=t1, t2=t2, m_=m_, j=j: e.tensor_tensor(out=m_.t[:, j, 0:n], in0=t1.t[:, 0:n], in1=t2.t[:, 0:n], op=ALU.add), [t1, t2], [m_])
                for j in range(KD):
                    js = slice(j * 128, (j + 1) * 128)
                    p = kb.psum()
                    for k in range(KD):
                        kb.op("pe", mm(p.t[:, 0:n], wout_b.t[:, k, js], m_.t[:, k, 0:n], k == 0, k == KD - 1), [wout_b, m_], [p])
                    tr_ = tfs[tc % 4]; tc += 1
                    kb.op("act", lambda e, p=p, j=j, s_=s_, tr_=tr_: e.activation(out=tr_.t[:, 0:n], in_=p.t[:, 0:n], func=AF.Identity, scale=mod(l, 2, j, s_)), [p, modv], [tr_])
                    kb.op("dve", lambda e, X=X, j=j, tr_=tr_: e.tensor_tensor(out=X.t[:, j, 0:n], in0=X.t[:, j, 0:n], in1=tr_.t[:, 0:n], op=ALU.add), [tr_], [X])
                kb.dma("pool", XT[:, t0:t0 + n].rearrange("(k p) t -> p k t", p=128), X.t[:, :, 0:n], [X], [dXT[ci]], X)
            kb.barrier()

        if stop == "P3":
            return nc
        if moe and not os.environ.get("DENSE_MOE"):
            routed_moe(l, last)
            continue
        clist = list(range(len(cfg.chunks)))
        if last:
            clist = clist[1:]
        groups = [clist[i:i + 2] for i in range(0, len(clist), 2)]
        with contextlib.ExitStack() as st:
            h2 = kb.tile(st, [128, KD, 1024], BF16, "h2")
            if moe:
                acc = kb.tile(st, [128, KD, 1024], F32, "acc")
                gwbc = kb.tile(st, [128, E, 1024], BF16, "gwbc")
                wr_t = kb.tile(st, [128, KD, 8], F32, "wr")
                gwT = kb.tile(st, [8, 1024], F32, "gwT")
                sm = [kb.tile(st, [128, 8], F32, "sm%d" % i) for i in range(6)]
                s1 = [kb.tile(st, [128, 1], F32, "s1%d" % i) for i in range(5)]
                kb.dma("sp", wr_t.t[:, :, :], wr[l // 2].rearrange("p (k e) -> p k e", e=8), [dummy], [wr_t], wr_t)
            wc = 0
            wcs = [0]
            xc = 0
            for grp in groups:
                cols = []
                c0 = 0
                with contextlib.ExitStack() as st2:
                    nt4 = norm_tiles(st2)
                    h2f = kb.tile(st2, [128, KD, 512], F32, "h2f") if moe else None
                    for ci in grp:
                        t0, n = cfg.chunks[ci]
                        cols.append((ci, t0, n, c0))
                        norm_chunk(nt4, XT, l, ci, V_NFFN, 3, 4, h2, c0, out_f32=h2f)
                        if moe:
                            for tt in range(n // 128):
                                p = kb.psum()
                                for k in range(KD):
                                    kb.op("pe", mm(p.t[:, 0:8], h2f.t[:, k, tt * 128:(tt + 1) * 128], wr_t.t[:, k, :], k == 0, k == KD - 1), [h2f, wr_t], [p])
                                Lg, m1e, L2, selm, ex, gw = sm
                                m1, m2, nm1, ss, rs1 = s1
                                kb.op("dve", lambda e: e.tensor_copy(out=Lg.t[:, :], in_=p.t[:, 0:8]), [p], [Lg])
                                kb.op("dve", lambda e: e.reduce_max(out=m1.t[:, :], in_=Lg.t[:, :], axis=AX.X), [Lg], [m1])
                                kb.op("dve", lambda e: e.tensor_scalar(out=m1e.t[:, :], in0=Lg.t[:, :], scalar1=m1.t[:, 0:1], scalar2=-1e30, op0=ALU.is_equal, op1=ALU.mult), [Lg, m1], [m1e])
                                kb.op("dve", lambda e: e.tensor_tensor(out=L2.t[:, :], in0=Lg.t[:, :], in1=m1e.t[:, :], op=ALU.add), [Lg, m1e], [L2])
                                kb.op("dve", lambda e: e.reduce_max(out=m2.t[:, :], in_=L2.t[:, :], axis=AX.X), [L2], [m2])
                                kb.op("dve", lambda e: e.tensor_scalar(out=selm.t[:, :], in0=Lg.t[:, :], scalar1=m2.t[:, 0:1], scalar2=None, op0=ALU.is_ge), [Lg, m2], [selm])
                                kb.op("dve", lambda e: e.tensor_scalar(out=nm1.t[:, :], in0=m1.t[:, :], scalar1=-1.0, scalar2=None, op0=ALU.mult), [m1], [nm1])
                                kb.op("act", lambda e: e.activation(out=ex.t[:, :], in_=Lg.t[:, :], func=AF.Exp, bias=nm1.t[:, 0:1], scale=1.0), [Lg, nm1], [ex])
                                kb.op("dve", lambda e: e.tensor_tensor(out=ex.t[:, :], in0=ex.t[:, :], in1=selm.t[:, :], op=ALU.mult), [selm], [ex])
                                kb.op("dve", lambda e: e.reduce_sum(out=ss.t[:, :], in_=ex.t[:, :], axis=AX.X), [ex], [ss])
                                kb.op("dve", lambda e: e.reciprocal(out=rs1.t[:, :], in_=ss.t[:, :]), [ss], [rs1])
                                kb.op("dve", lambda e: e.tensor_scalar(out=gw.t[:, :], in0=ex.t[:, :], scalar1=rs1.t[:, 0:1], scalar2=None, op0=ALU.mult), [ex, rs1], [gw])
                                p2 = kb.psum()
                                kb.op("pe", mm(p2.t[0:8, 0:128], gw.t[:, :], id128f), [gw, cf], [p2])
                                kb.op("dve", lambda e: e.tensor_copy(out=gwT.t[:, c0 + tt * 128:c0 + (tt + 1) * 128], in_=p2.t[0:8, 0:128]), [p2], [gwT])
                            for e_ in range(E):
                                p = kb.psum()
                                kb.op("pe", mm(p.t[:, 0:n], sel_e(e_), gwT.t[:, c0:c0 + n]), [cf, gwT], [p])
                                evac(gwbc.t[:, e_, c0:c0 + n], gwbc, p, p.t[:, 0:n])
                        c0 += n
                    kb.barrier()
                with contextlib.ExitStack() as st2:
                    act = kb.tile(st2, [128, KF, 1024], BF16, "act")
                    gus = [kb.tile(st2, [128, KD * 256], F32, "gus") for _ in range(3)]
                    gub = [kb.tile(st2, [128, KD, 256], BF16, "gub") for _ in range(3)]
                    dns = [kb.tile(st2, [128, KF * 128], F32, "dns") for _ in range(3)]
                    dnb = [kb.tile(st2, [128, KF, 128], BF16, "dnb") for _ in range(3)]
                    xts = [kb.tile(st2, [128, 512], F32, "x4") for _ in range(3)]
                    sl = [kb.tile(st2, [128, 512], BF16, "silu") for _ in range(3)]
                    t5 = [kb.tile(st2, [128, 512], BF16, "t5") for _ in range(2)]
                    for e_ in range(E if moe else 1):
                        gsrc = wgu[l // 2]
                        dsrc = wdn[l // 2]
                        def issue_gu(f):
                            nonlocal_wc = wcs[0]; wcs[0] += 1
                            sg = gus[nonlocal_wc % 3]; gb = gub[nonlocal_wc % 3]
                            kb.dma("sp", sg.t[:, :], gsrc[f], [dummy], [sg], sg)
                            cast(gb.t[:, :, :], gb, sg.t[:, :].rearrange("p (k c) -> p k c", k=KD), sg)
                            return gb

                        def issue_dn(j):
                            nonlocal_wc = wcs[0]; wcs[0] += 1
                            sg = dns[nonlocal_wc % 3]; db = dnb[nonlocal_wc % 3]
                            kb.dma("sp", sg.t[:, :], dsrc[j], [dummy], [sg], sg)
                            cast(db.t[:, :, :], db, sg.t[:, :].rearrange("p (k c) -> p k c", k=KF), sg)
                            return db
                        gb_next = issue_gu(0)
                        for f in range(KF):
                            gb = gb_next
                            gb_next = issue_gu(f + 1) if f + 1 < KF else None
                            if f + 1 == KF:
                                db_next = issue_dn(0)
                            for (ci, t0, n, c0) in cols:
                                pg = kb.psum(); pu = kb.psum()
                                for k in range(KD):
                                    kb.op("pe", mm(pg.t[:, 0:n], gb.t[:, k, 0:128], h2.t[:, k, c0:c0 + n], k == 0, k == KD - 1), [gb, h2], [pg])
                                for k in range(KD):
                                    kb.op("pe", mm(pu.t[:, 0:n], gb.t[:, k, 128:256], h2.t[:, k, c0:c0 + n], k == 0, k == KD - 1), [gb, h2], [pu])
                                s_ = sl[xc % 3]; xc += 1
                                kb.op("act", lambda e: e.activation(out=s_.t[:, 0:n], in_=pg.t[:, 0:n], func=AF.Silu), [pg], [s_])
                                if moe:
                                    t_ = t5[xc % 2]
                                    kb.op("dve", lambda e: e.tensor_tensor(out=t_.t[:, 0:n], in0=pu.t[:, 0:n], in1=s_.t[:, 0:n], op=ALU.mult), [pu, s_], [t_])
                                    kb.op("pool", lambda e: e.tensor_tensor(out=act.t[:, f, c0:c0 + n], in0=t_.t[:, 0:n], in1=gwbc.t[:, e_, c0:c0 + n], op=ALU.mult), [t_, gwbc], [act])
                                else:
                                    kb.op("dve", lambda e: e.tensor_tensor(out=act.t[:, f, c0:c0 + n], in0=pu.t[:, 0:n], in1=s_.t[:, 0:n], op=ALU.mult), [pu, s_], [act])
                        for j in range(KD):
                            db = db_next
                            db_next = issue_dn(j + 1) if j + 1 < KD else None
                            for (ci, t0, n, c0) in cols:
                                s_i = 1 if ci == 0 else 0
                                p = kb.psum()
                                for k in range(KF):
                                    kb.op("pe", mm(p.t[:, 0:n], db.t[:, k, :], act.t[:, k, c0:c0 + n], k == 0, k == KF - 1), [db, act], [p])
                                fin = (not moe) or e_ == E - 1
                                if moe and e_ == 0:
                                    evac(acc.t[:, j, c0:c0 + n], acc, p, p.t[:, 0:n])
                                elif moe:
                                    kb.op("dve", lambda e: e.tensor_tensor(out=acc.t[:, j, c0:c0 + n], in0=p.t[:, 0:n], in1=acc.t[:, j, c0:c0 + n], op=ALU.add), [p], [acc])
                                if fin:
                                    x_ = xts[xc % 3]; xc += 1
                                    js = slice(j * 128, (j + 1) * 128)
                                    kb.dma("sp", x_.t[:, 0:n], XT[js, t0:t0 + n], [dummy], [x_], x_)
                                    if moe:
                                        kb.op("dve", lambda e: e.scalar_tensor_tensor(out=x_.t[:, 0:n], in0=acc.t[:, j, c0:c0 + n], scalar=mod(l, 5, j, s_i), in1=x_.t[:, 0:n], op0=ALU.mult, op1=ALU.add), [acc, modv], [x_])
                                    else:
                                        tq_ = t5[xc % 2]
                                        tq32 = xts[(xc + 1) % 3]
                                        kb.op("act", lambda e: e.activation(out=tq32.t[:, 0:n], in_=p.t[:, 0:n], func=AF.Identity, scale=mod(l, 5, j, s_i)), [p, modv], [tq32])
                                        kb.op("dve", lambda e: e.tensor_tensor(out=x_.t[:, 0:n], in0=x_.t[:, 0:n], in1=tq32.t[:, 0:n], op=ALU.add), [tq32], [x_])
                                    kb.dma("pool", XT[js, t0:t0 + n], x_.t[:, 0:n], [x_], [Buf("snk")], x_)
                    kb.barrier()

    with contextlib.ExitStack() as st:
        xt = [kb.tile(st, [128, KD, 512], F32, "xf") for _ in range(2)]
        sq = [kb.tile(st, [128, KD, 512], BF16, "sqf") for _ in range(2)]
        rs = [kb.tile(st, [128, 512], F32, "rsf") for _ in range(2)]
        for qi, ci in enumerate(range(1, len(cfg.chunks))):
            t0, n = cfg.chunks[ci]
            x_ = xt[qi % 2]; s_ = sq[qi % 2]; r_ = rs[qi % 2]
            kb.dma("sp", x_.t[:, :, 0:n], XT[:, t0:t0 + n].rearrange("(k p) t -> p k t", p=128), [dXT[ci]], [x_], x_)
            p = kb.psum()
            for k in range(KD):
                kb.op("act", lambda e, k=k, x_=x_, s_=s_: e.activation(out=s_.t[:, k, 0:n], in_=x_.t[:, k, 0:n], func=AF.Square), [x_], [s_])
                kb.op("pe", mm(p.t[:, 0:n], ones128, s_.t[:, k, 0:n], k == 0, k == KD - 1), [s_, cb], [p])
            rstd_from(p, n, D, r_)
            for k in range(KD):
                kb.op("dve", lambda e, k=k, x_=x_, r_=r_: e.scalar_tensor_tensor(out=x_.t[:, k, 0:n], in0=x_.t[:, k, 0:n], scalar=v_nfinal[:, k:k + 1], in1=r_.t[:, 0:n], op0=ALU.mult, op1=ALU.mult), [r_, vc], [x_])
            kb.dma("pool", outT[:, t0 - C:t0 - C + n].rearrange("(k p) t -> p k t", p=128), x_.t[:, :, 0:n], [x_], [dXT[ci]], x_)
        kb.barrier()
    return nc


def _tile_cols(w, KD):
    D = w.shape[0]
    return np.ascontiguousarray(w.reshape(KD, 128, w.shape[1]).transpose(1, 0, 2).reshape(128, -1))


def host_prep(cfg, inp):
    D, C, S, L, F, E, T, KD, KF = cfg.D, cfg.C, cfg.S, cfg.L, cfg.F, cfg.E, cfg.T, cfg.KD, cfg.KF
    f32 = np.float32
    sh = {}
    NVL = 2 * KD + 48 + 6 + 3

    def fm(v):
        return np.asarray(v, f32).reshape(-1, 128).T

    def rot64(g):
        return np.concatenate([g[32:], g[:32]])
    w_in = np.asarray(inp["w_in"], f32)
    KVW = 928
    o_kna, o_vna, o_kg, o_vg, o_ckv, o_kr = 0, 256, 512, 640, 768, 896
    o_qna, o_qg, o_cq, o_gate = KVW, KVW + 256, KVW + 768, KVW + 1024

    def rotcols(base, nheads, d):
        idx = []
        for h in range(nheads):
            idx += list(range(base + h * d + d // 2, base + (h + 1) * d)) + list(range(base + h * d, base + h * d + d // 2))
        return idx
    w1 = np.zeros((L, cfg.NT1, 128, KD * 128), f32)
    wvv = np.zeros((L, 128, KD * 384), f32)
    for l in range(L):
        W = w_in[l]
        tiles = []
        tiles += [W[:, o_kna:o_kna + 128], W[:, o_kna + 128:o_kna + 256]]
        tiles += [W[:, o_kg:o_kg + 128], W[:, rotcols(o_kg, 2, 64)]]
        tiles += [W[:, o_ckv:o_ckv + 128]]
        t5 = np.zeros((D, 128), f32); t5[:, 64:96] = W[:, o_kr:o_kr + 32]
        t6 = np.zeros((D, 128), f32); t6[:, 64:96] = W[:, rotcols(o_kr, 1, 32)]
        tiles += [t5, t6]
        tiles += [W[:, o_qna:o_qna + 128], W[:, o_qna + 128:o_qna + 256]]
        tiles += [W[:, o_qg + j * 128:o_qg + (j + 1) * 128] for j in range(4)]
        rc = rotcols(o_qg, 8, 64)
        tiles += [W[:, rc[j * 128:(j + 1) * 128]] for j in range(4)]
        tiles += [W[:, o_cq:o_cq + 128], W[:, o_cq + 128:o_cq + 256]]
        tiles += [W[:, o_gate + j * 128:o_gate + (j + 1) * 128] for j in range(24)]
        for i, t in enumerate(tiles):
            w1[l, i] = _tile_cols(t, KD)
        Wv = np.concatenate([W[:, o_vna:o_vna + 256], W[:, o_vg:o_vg + 128]], axis=1)
        wvv[l] = _tile_cols(Wv, KD)
    sh["w1"] = w1
    sh["wv"] = wvv
    w_ada = np.asarray(inp["w_ada"], f32)
    sh["w_ada"] = np.ascontiguousarray(w_ada.reshape(L, KD, 128, 48, 128).transpose(0, 3, 2, 1, 4).reshape(L, 48, 128, KD * 128))
    w_uq = np.asarray(inp["w_uq"], f32)
    sh["wuq"] = np.ascontiguousarray(w_uq.reshape(L, 2, 128, 384).transpose(0, 2, 1, 3).reshape(L, 128, 768))
    wuqr = np.zeros_like(w_uq)
    for h in range(4):
        b = h * 96 + 64
        wuqr[:, :, b:b + 16] = w_uq[:, :, b + 16:b + 32]
        wuqr[:, :, b + 16:b + 32] = w_uq[:, :, b:b + 16]
    sh["wuqr"] = np.ascontiguousarray(wuqr.reshape(L, 2, 128, 384).transpose(0, 2, 1, 3).reshape(L, 128, 768))
    w_ukv = np.asarray(inp["w_ukv"], f32).reshape(L, 128, 4, 128)
    sh["wukvk"] = np.ascontiguousarray(w_ukv[:, :, :, :64].reshape(L, 128, 256))
    sh["wukvv"] = np.ascontiguousarray(w_ukv[:, :, :, 64:].reshape(L, 128, 256))
    rpb = np.asarray(inp["rpb"], f32)
    jq = np.arange(64)[:, None]; jk = np.arange(64)[None, :]
    dc = np.clip(jk - jq, -15, 15) + 15
    g = rpb[:, :, :, dc]
    sh["rpbT"] = np.ascontiguousarray(g.transpose(0, 3, 1, 2, 4).reshape(L, 64, 3840))
    wo_all = np.concatenate([np.asarray(inp["w_o_na"], f32), np.asarray(inp["w_o_gqa"], f32), np.asarray(inp["w_o_mla"], f32)], axis=1)
    sh["wo"] = np.ascontiguousarray(wo_all.reshape(L, 8, 128, D).transpose(0, 2, 1, 3).reshape(L, 128, 8 * D))
    sh["wout"] = np.ascontiguousarray(np.asarray(inp["w_out"], f32).reshape(L, KD, 128, D).transpose(0, 2, 1, 3).reshape(L, 128, KD * D))

    def gu_layout(w):
        lead = w.shape[:-2]
        gte = w[..., :F].reshape(*lead, KD, 128, KF, 128)
        up = w[..., F:].reshape(*lead, KD, 128, KF, 128)
        cat = np.stack([gte, up], axis=-2)
        nl = len(lead)
        perm = list(range(nl)) + [nl + 2, nl + 1, nl + 0, nl + 3, nl + 4]
        return np.ascontiguousarray(cat.transpose(perm).reshape(*lead, KF, 128, KD * 256))

    def dn_layout(w):
        lead = w.shape[:-2]
        nl = len(lead)
        a = w.reshape(*lead, KF, 128, KD, 128)
        perm = list(range(nl)) + [nl + 2, nl + 1, nl + 0, nl + 3]
        return np.ascontiguousarray(a.transpose(perm).reshape(*lead, KD, 128, KF * 128))
    sh["wgu"] = gu_layout(np.asarray(inp["w_ffn_gu"], f32))
    sh["wdn"] = dn_layout(np.asarray(inp["w_ffn_dn"], f32))
    if cfg.NM > 0:
        sh["wgum"] = gu_layout(np.asarray(inp["w_moe_gu"], f32)).reshape(cfg.NM * E * KF * 128, KD * 256)
        sh["wdnm"] = np.ascontiguousarray(np.asarray(inp["w_moe_dn"], f32).reshape(cfg.NM * E * F, D))
        sh["wr"] = np.ascontiguousarray(np.asarray(inp["w_router"], f32).reshape(cfg.NM, KD, 128, 8).transpose(0, 2, 1, 3).reshape(cfg.NM, 128, KD * 8))
    else:
        sh["wgum"] = np.zeros((E * KF * 128, KD * 256), f32)
        sh["wdnm"] = np.zeros((E * F, D), f32)
        sh["wr"] = np.zeros((1, 128, KD * 8), f32)
    pos = np.arange(S)
    rows = (pos // 64).astype(f32); cols = (pos % 64).astype(f32)

    def tables(half):
        nf = half // 2
        inv = np.power(10000.0, -np.arange(nf, dtype=f32) / nf).astype(f32)
        ang = np.concatenate([rows[:, None] * inv, cols[:, None] * inv], axis=-1)
        cos = np.concatenate([np.ones((C, half), f32), np.cos(ang)], 0).T
        sin = np.concatenate([np.zeros((C, half), f32), np.sin(ang)], 0).T
        return cos, sin
    cg, sg = tables(32)
    cosg = np.concatenate([cg, cg, cg, cg], 0)
    sing = np.concatenate([-sg, sg, -sg, sg], 0)
    cm, sm_ = tables(16)
    cosm = np.zeros((128, T), f32); sinm = np.zeros((128, T), f32)
    cosm[64:96] = np.concatenate([cm, cm], 0)
    sinm[64:96] = np.concatenate([-sm_, sm_], 0)
    col0 = np.clip(np.arange(64) - 8, 0, 48)
    inwin = (jk >= col0[:, None]) & (jk < col0[:, None] + 16)
    neg = np.where(inwin, 0.0, -30000.0).astype(f32)
    negm = np.tile(neg[:, None, :], (1, 60, 1)).reshape(64, 3840)
    ones = np.ones((128, 128), f32)
    bd = np.zeros((128, 128), f32); bd[:64, :64] = 1; bd[64:, 64:] = 1
    idb = np.zeros((128, 64), f32); idb[:64] = np.eye(64)
    ustr = np.triu(np.ones((128, 128), f32), 1)
    sh["cbf"] = np.ascontiguousarray(np.concatenate([ones, bd, idb, ustr, np.eye(128, dtype=f32)], 1).astype(BF))
    sh["ropet"] = np.ascontiguousarray(np.stack([cosg, sing, cosm, sinm], 1).astype(BF))
    sh["negm"] = np.ascontiguousarray(negm.astype(BF))
    sel64 = np.zeros((128, 128), f32); sel64[64, :64] = 1
    sele = np.zeros((128, E, 128), f32)
    for e in range(E):
        sele[e, e, :] = 1
    sh["cf32"] = np.ascontiguousarray(np.concatenate([np.eye(128, dtype=f32), sel64, sele.reshape(128, E * 128), np.full((128, 1), 1e-6, f32),
        np.tile((np.arange(10, dtype=f32) * 512)[None, :], (128, 1)), np.tile(np.arange((2 * T + E * 511) // 512, dtype=f32)[None, :], (128, 1)),
        (np.arange(KF, dtype=f32)[None, :] * 128 + np.arange(128, dtype=f32)[:, None])], 1))
    NV = L * NVL + 3 * KD
    per = []
    xin = np.asarray(inp["x"], f32); ctx = np.asarray(inp["ctx"], f32); c = np.asarray(inp["c"], f32)
    B = xin.shape[0]
    vbase = np.zeros((128, NV), f32)
    for l in range(L):
        o = l * NVL
        vbase[:, o:o + KD] = fm(inp["norm_mix"][l])
        vbase[:, o + KD:o + 2 * KD] = fm(inp["norm_ffn"][l])
        vbase[:, o + 2 * KD:o + 2 * KD + 48] = fm(inp["b_ada"][l])
        qg = np.asarray(inp["q_norm_gqa"][l], f32); kg = np.asarray(inp["k_norm_gqa"][l], f32)
        vbase[:, o + 2 * KD + 48] = np.concatenate([qg, qg])
        vbase[:, o + 2 * KD + 49] = np.concatenate([rot64(qg), rot64(qg)])
        vbase[:, o + 2 * KD + 50] = np.concatenate([kg, kg])
        vbase[:, o + 2 * KD + 51] = np.concatenate([rot64(kg), rot64(kg)])
        vbase[:, o + 2 * KD + 52:o + 2 * KD + 54] = fm(inp["q_lora_norm"][l])
        vbase[:, o + 2 * KD + 54] = np.asarray(inp["kv_lora_norm"][l], f32)
    go = L * NVL
    vbase[:, go:go + KD] = fm(inp["norm_final"])
    vbase[:, go + 2 * KD:go + 3 * KD] = fm(inp["c_ctx"])
    for b in range(B):
        m = dict(sh)
        v = vbase.copy()
        v[:, go + KD:go + 2 * KD] = fm(c[b])
        m["vecs"] = v
        m["xT"] = np.ascontiguousarray(np.concatenate([ctx[b], xin[b]], 0).T)
        per.append(m)
    return per


_CACHE = {}


def kernel(**inputs):
    cfg = Cfg()
    if "nc" not in _CACHE:
        _CACHE["nc"] = build(cfg)
    nc = _CACHE["nc"]
    in_maps = host_prep(cfg, inputs)
    res = run_bass_kernel_spmd(nc, in_maps, core_ids=list(range(len(in_maps))))
    out = np.stack([np.ascontiguousarray(r["outT"].T) for r in res.results], 0)
    return out.astype(np.float32)
```

```python
import contextlib
import os
import numpy as np
import ml_dtypes
import concourse.bass as bass
import concourse.mybir as mybir
from concourse.bass_utils import run_bass_kernel_spmd

F32, BF16 = mybir.dt.float32, mybir.dt.bfloat16
AF = mybir.ActivationFunctionType
ALU = mybir.AluOpType
AX = mybir.AxisListType
BF = ml_dtypes.bfloat16


class Cfg:
    def __init__(s, D=1024, C=256, S=4096, L=4, F=2816, E=8):
        s.D, s.C, s.S, s.L, s.F, s.E = D, C, S, L, F, E
        s.T = C + S
        s.KD = D // 128
        s.R = S // 64
        s.KF = F // 128
        s.chunks = [(0, C)] + [(C + i * 512, 512) for i in range(S // 512)]
        s.NKT = s.T // 128
        s.CT = C // 128
        s.ND = (L + 1) // 2
        s.NM = L // 2
        s.NT1 = 19 + 24


class Buf:
    __slots__ = ("name", "w", "r", "dsem", "dram")

    def __init__(s, name, dram=False):
        s.name = name
        s.w = {}
        s.r = {}
        s.dsem = None
        s.dram = dram


class Tl:
    def __init__(s, t, name):
        s.t = t
        s.b = Buf(name)


class KB:
    NDMASEM = 90

    def __init__(self, nc):
        self.nc = nc
        self.eng = {"pe": nc.tensor, "act": nc.scalar, "dve": nc.vector, "pool": nc.gpsimd, "sp": nc.sync}
        self.sems = []
        self.last = []
        self.semidx = {}
        for e in ("pe", "act", "dve", "pool"):
            self.semidx[e] = self._newsem("s_" + e)
        self.cnt = {e: 0 for e in self.semidx}
        self.waited = {e: {} for e in self.eng}
        self.dsems = []
        self.nd = 0
        self.stack = contextlib.ExitStack()
        self.ps = []
        for i in range(8):
            t = self.stack.enter_context(nc.psum_tensor("ps%d" % i, [128, 512], F32))
            self.ps.append(Tl(t, "ps%d" % i))
        self.psi = 0
        self.ntile = 0

    def _newsem(self, name):
        s = self.nc.semaphore(name).__enter__()
        self.sems.append(s)
        self.last.append(0)
        return len(self.sems) - 1

    def tile(self, st, shape, dt, name=None):
        self.ntile += 1
        name = (name or "t") + "_%d" % self.ntile
        t = st.enter_context(self.nc.sbuf_tensor(name, list(shape), dt))
        return Tl(t, name)

    def psum(self, pool=None):
        pool = pool or (0, 8)
        lo, n = pool
        key = ("psi", lo, n)
        i = getattr(self, "_rr", {}).get(key, 0)
        if not hasattr(self, "_rr"):
            self._rr = {}
        self._rr[key] = (i + 1) % n
        return self.ps[lo + i]

    def _wait(self, e, si, v):
        wd = self.waited[e]
        if e == "pe" and si == self.semidx["pe"]:
            return
        if wd.get(si, 0) < v:
            self.eng[e].wait_ge(self.sems[si], v)
            wd[si] = v

    def _deps(self, e, reads, writes):
        need = {}
        for b in reads:
            for si, v in b.w.items():
                if need.get(si, 0) < v:
                    need[si] = v
        for b in writes:
            if not b.dram:
                for si, v in b.w.items():
                    if need.get(si, 0) < v:
                        need[si] = v
            for si, v in b.r.items():
                if need.get(si, 0) < v:
                    need[si] = v
        for si, v in need.items():
            self._wait(e, si, v)

    def _mark(self, tok, reads, writes):
        si, v = tok
        for b in writes:
            if b.dram:
                if b.w.get(si, 0) < v:
                    b.w[si] = v
            else:
                b.w = {si: v}
            b.r = {}
        for b in reads:
            if any(b is w for w in writes):
                continue
            b.r[si] = v

    def op(self, e, fn, reads=(), writes=()):
        reads = [x.b if isinstance(x, Tl) else x for x in reads]
        writes = [x.b if isinstance(x, Tl) else x for x in writes]
        self._deps(e, reads, writes)
        ins = fn(self.eng[e])
        self.cnt[e] += 1
        si = self.semidx[e]
        ins.then_inc(self.sems[si], 1)
        self.last[si] = self.cnt[e]
        self._mark((si, self.cnt[e]), reads, writes)

    def dma(self, q, out, in_, reads, writes, sb):
        reads = [x.b if isinstance(x, Tl) else x for x in reads]
        writes = [x.b if isinstance(x, Tl) else x for x in writes]
        sb = sb.b if isinstance(sb, Tl) else sb
        if sb.dsem is None:
            if len(self.dsems) < self.NDMASEM:
                self.dsems.append(self._newsem("d%d" % len(self.dsems)))
                sb.dsem = self.dsems[-1]
            else:
                sb.dsem = self.dsems[self.nd % self.NDMASEM]
            self.nd += 1
        si = sb.dsem
        self._deps(q, reads, writes)
        if self.last[si] > 0:
            self._wait(q, si, self.last[si])
        ins = self.eng[q].dma_start(out=out, in_=in_)
        self.last[si] += 16
        ins.then_inc(self.sems[si], 16)
        self._mark((si, self.last[si]), reads, writes)

    def idma(self, out, out_off, in_, in_off, reads, writes, sb):
        reads = [x.b if isinstance(x, Tl) else x for x in reads]
        writes = [x.b if isinstance(x, Tl) else x for x in writes]
        sb = sb.b if isinstance(sb, Tl) else sb
        if sb.dsem is None:
            if len(self.dsems) < self.NDMASEM:
                self.dsems.append(self._newsem("d%d" % len(self.dsems)))
                sb.dsem = self.dsems[-1]
            else:
                sb.dsem = self.dsems[self.nd % self.NDMASEM]
            self.nd += 1
        si = sb.dsem
        self._deps("pool", reads, writes)
        if self.last[si] > 0:
            self._wait("pool", si, self.last[si])
        ins = self.eng["pool"].indirect_dma_start(out=out, out_offset=out_off, in_=in_, in_offset=in_off)
        self.last[si] += 16
        ins.then_inc(self.sems[si], 16)
        self._mark((si, self.last[si]), reads, writes)

    def barrier(self):
        for e in self.eng:
            for si, v in enumerate(self.last):
                if v > 0:
                    self._wait(e, si, v)


def mm(out, lhsT, rhs, start=True, stop=True):
    return lambda e: e.matmul(out, lhsT, rhs, start=start, stop=stop)


def build(cfg, debug=False, stop=None):
    nc = bass.Bass("TRN2", target_bir_lowering=False)
    D, C, S, L, F, E, T, KD, KF = cfg.D, cfg.C, cfg.S, cfg.L, cfg.F, cfg.E, cfg.T, cfg.KD, cfg.KF
    NKT, CT, R = cfg.NKT, cfg.CT, cfg.R

    def din(name, shape, dt=F32):
        return nc.dram_tensor(name, list(shape), dt, kind="ExternalInput").ap()

    def dscr(name, shape, dt=BF16):
        return nc.dram_tensor(name, list(shape), dt, kind=("ExternalOutput" if debug else "Internal")).ap()

    xT_in = din("xT", [D, T])
    NVL = 2 * KD + 48 + 6 + 3
    NV = L * NVL + 3 * KD
    vecs = din("vecs", [128, NV])
    w_ada = din("w_ada", [L, 48, 128, KD * 128])
    w1 = din("w1", [L, cfg.NT1, 128, KD * 128])
    wv = din("wv", [L, 128, KD * 384])
    wuq = din("wuq", [L, 128, 2 * 384])
    wuqr = din("wuqr", [L, 128, 2 * 384])
    wukvk = din("wukvk", [L, 128, 256])
    wukvv = din("wukvv", [L, 128, 256])
    rpbT = din("rpbT", [L, 64, 3840])
    wo = din("wo", [L, 128, 8 * D])
    wout = din("wout", [L, 128, KD * D])
    wgu = din("wgu", [max(cfg.ND, 1), KF, 128, KD * 256])
    wdn = din("wdn", [max(cfg.ND, 1), KD, 128, KF * 128])
    NMx = max(cfg.NM, 1)
    wgum = din("wgum", [NMx * E * KF * 128, KD * 256])
    wdnm = din("wdnm", [NMx * E * F, D])
    wr = din("wr", [max(cfg.NM, 1), 128, KD * 8])
    NCB = 128 + 128 + 64 + 128 + 128
    cbf = din("cbf", [128, NCB], BF16)
    ropet = din("ropet", [128, 4, T], BF16)
    negm = din("negm", [64, 3840], BF16)
    BS = 512
    NTHR = 10
    NBMAX = (2 * T + E * (BS - 1)) // BS
    NCF = 128 + 128 + E * 128 + 1 + NTHR + NBMAX + KF
    cf32 = din("cf32", [128, NCF])
    outT = nc.dram_tensor("outT", [D, S], F32, kind="ExternalOutput").ap()

    XT = nc.dram_tensor("XTs", [D, T], F32, kind="Internal").ap()
    KnaT = dscr("KnaT", [256, T])
    QnaT = dscr("QnaT", [256, T])
    KgT = dscr("KgT", [128, T])
    QgT = dscr("QgT", [512, T])
    KmT = dscr("KmT", [4 * 96, T])
    QmT = dscr("QmT", [4 * 96, T])
    Vna = dscr("Vna", [T, 4 * 65])
    Vg = dscr("Vg", [T, 2 * 65])
    Vm = dscr("Vm", [T, 4 * 65])
    GT = dscr("GT", [3 * D, T])
    YT = dscr("YT", [D, T])
    XsD = nc.dram_tensor("Xs", [NBMAX * BS, D], BF16, kind="Internal").ap()
    YsD = nc.dram_tensor("Ys", [NBMAX * BS, D], F32, kind="Internal").ap()

    kb = KB(nc)
    dXT = [Buf("XT%d" % i, dram=True) for i in range(len(cfg.chunks))]
    dQK = {n: Buf(n, dram=True) for n in ("KnaT", "QnaT", "KgT", "QgT", "KmT", "QmT", "Vna", "Vg", "Vm", "GT", "YT")}

    gst = contextlib.ExitStack()
    cb = kb.tile(gst, [128, NCB], BF16, "cbf")
    cf = kb.tile(gst, [128, NCF], F32, "cf32")
    vc = kb.tile(gst, [128, NV], F32, "vecs")
    modv = kb.tile(gst, [128, L * 48 * 2], F32, "modv")
    dummy = Buf("dram_in")
    kb.dma("sp", cb.t[:, :], cbf[:, :], [dummy], [cb], cb)
    kb.dma("sp", cf.t[:, :], cf32[:, :], [dummy], [cf], cf)
    kb.dma("sp", vc.t[:, :], vecs[:, :], [dummy], [vc], vc)
    o = 0
    ones128 = cb.t[:, o:o + 128]; o += 128
    bd64 = cb.t[:, o:o + 128]; o += 128
    id64b = cb.t[:, o:o + 64]; o += 64
    ustrict = cb.t[:, o:o + 128]; o += 128
    identb = cb.t[:, o:o + 128]; o += 128
    id128f = cf.t[:, 0:128]
    sel64 = cf.t[0:65, 128:256]
    o2 = 128 + 128 + E * 128
    epsc = cf.t[:, o2:o2 + 1]
    thr_c = cf.t[:, o2 + 1:o2 + 1 + NTHR]
    jrow_c = cf.t[:, o2 + 1 + NTHR:o2 + 1 + NTHR + NBMAX]
    cidx_c = cf.t[:, o2 + 1 + NTHR + NBMAX:o2 + 1 + NTHR + NBMAX + KF]

    def sel_e(e):
        return cf.t[0:8, 256 + e * 128:256 + (e + 1) * 128]

    def vcol(l, j, n=1):
        return vc.t[:, l * NVL + j:l * NVL + j + n]
    V_NMIX, V_NFFN, V_BADA, V_QG, V_QGR, V_KG, V_KGR, V_QL, V_KVL = 0, KD, 2 * KD, 2 * KD + 48, 2 * KD + 49, 2 * KD + 50, 2 * KD + 51, 2 * KD + 52, 2 * KD + 54
    gofs = L * NVL
    v_nfinal = vc.t[:, gofs:gofs + KD]
    v_c = vc.t[:, gofs + KD:gofs + 3 * KD]

    def mod(l, which, j, s):
        i = ((l * 48) + which * KD + j) * 2 + s
        return modv.t[:, i:i + 1]

    with contextlib.ExitStack() as st:
        sc = kb.tile(st, [128, 2 * KD], F32, "silu_c")
        sc2 = kb.tile(st, [128, KD, 2], F32, "silu_c2")
        kb.op("act", lambda e: e.activation(out=sc.t[:, :], in_=v_c, func=AF.Silu), [vc], [sc])
        for s_ in range(2):
            kb.op("dve", lambda e, s_=s_: e.tensor_copy(out=sc2.t[:, :, s_], in_=sc.t[:, s_ * KD:(s_ + 1) * KD]), [sc], [sc2])
        wst = [kb.tile(st, [128, KD * 128], F32, "wada") for _ in range(3)]
        i = 0
        for l in range(L):
            for nt in range(48):
                w_ = wst[i % 3]; i += 1
                kb.dma("sp", w_.t[:, :], w_ada[l, nt], [dummy], [w_], w_)
                p = kb.psum()
                for k in range(KD):
                    kb.op("pe", mm(p.t[:, 0:2], w_.t[:, k * 128:(k + 1) * 128], sc2.t[:, k, :], k == 0, k == KD - 1), [w_, sc2], [p])
                base = ((l * 48) + nt) * 2
                kb.op("dve", lambda e, p=p, base=base, l=l, nt=nt: e.tensor_scalar(out=modv.t[:, base:base + 2], in0=p.t[:, 0:2], scalar1=vcol(l, V_BADA + nt), scalar2=None, op0=ALU.add), [p, vc], [modv])
        kb.barrier()
    if stop == "P0":
        return nc

    def rstd_from(ps_ssq, n, dim, out_t):
        kb.op("act", lambda e: e.activation(out=out_t.t[:, 0:n], in_=ps_ssq.t[:, 0:n], func=AF.Ln, bias=epsc, scale=1.0 / dim), [ps_ssq, cf], [out_t])
        kb.op("act", lambda e: e.activation(out=out_t.t[:, 0:n], in_=out_t.t[:, 0:n], func=AF.Exp, scale=-0.5), [], [out_t])

    def norm_chunk(st_tiles, xsrc, l, ci, which_norm, which_sh, which_sc, out_bf, out_col0, out_f32=None):
        xt, sq, tmp, rs, gs = st_tiles
        t0, n = cfg.chunks[ci]
        s_ = 1 if ci == 0 else 0
        kb.dma("sp", xt.t[:, :, 0:n], xsrc[:, t0:t0 + n].rearrange("(k p) t -> p k t", p=128), [dXT[ci]], [xt], xt)
        for k in range(KD):
            kb.op("dve", lambda e, k=k: e.scalar_tensor_tensor(out=gs.t[:, k:k + 1], in0=mod(l, which_sc, k, s_), scalar=1.0, in1=vcol(l, which_norm + k), op0=ALU.add, op1=ALU.mult), [modv, vc], [gs])
        p = kb.psum()
        for k in range(KD):
            kb.op("act", lambda e, k=k: e.activation(out=sq.t[:, k, 0:n], in_=xt.t[:, k, 0:n], func=AF.Square), [xt], [sq])
            kb.op("pe", mm(p.t[:, 0:n], ones128, sq.t[:, k, 0:n], k == 0, k == KD - 1), [sq, cb], [p])
        rstd_from(p, n, D, rs)
        for k in range(KD):
            kb.op("dve", lambda e, k=k: e.tensor_tensor(out=tmp.t[:, 0:n], in0=xt.t[:, k, 0:n], in1=rs.t[:, 0:n], op=ALU.mult), [xt, rs], [tmp])
            if out_f32 is not None:
                kb.op("act", lambda e, k=k: e.activation(out=out_f32.t[:, k, 0:n], in_=tmp.t[:, 0:n], func=AF.Identity, bias=mod(l, which_sh, k, s_), scale=gs.t[:, k:k + 1]), [tmp, gs, modv], [out_f32])
                kb.op("pool", lambda e, k=k: e.tensor_copy(out=out_bf.t[:, k, out_col0:out_col0 + n], in_=out_f32.t[:, k, 0:n]), [out_f32], [out_bf])
            else:
                kb.op("act", lambda e, k=k: e.activation(out=out_bf.t[:, k, out_col0:out_col0 + n], in_=tmp.t[:, 0:n], func=AF.Identity, bias=mod(l, which_sh, k, s_), scale=gs.t[:, k:k + 1]), [tmp, gs, modv], [out_bf])

    def norm_tiles(st, wmax=512):
        return (kb.tile(st, [128, KD, wmax], F32, "xt"), kb.tile(st, [128, KD, wmax], BF16, "sq"),
                kb.tile(st, [128, wmax], F32, "tmp"), kb.tile(st, [128, wmax], F32, "rs"), kb.tile(st, [128, KD], F32, "gs"))

    def load_w_bf(stg, dst, src_ap, width, eng):
        kb.dma("sp", stg.t[:, 0:width], src_ap, [dummy], [stg], stg)
        return stg

    rr = {"ev": 0, "cast": 0}

    def cast(out_ap, out_buf, in_ap, in_buf):
        rr["cast"] ^= 1
        if rr["cast"]:
            kb.op("dve", lambda e: e.tensor_copy(out=out_ap, in_=in_ap), [in_buf], [out_buf])
        else:
            kb.op("act", lambda e: e.activation(out=out_ap, in_=in_ap, func=AF.Copy), [in_buf], [out_buf])

    def evac(out_ap, out_buf, ps_t, ps_ap, func=None):
        if func is not None:
            kb.op("act", lambda e: e.activation(out=out_ap, in_=ps_ap, func=func), [ps_t], [out_buf])
            return
        rr["ev"] ^= 1
        if rr["ev"]:
            kb.op("act", lambda e: e.activation(out=out_ap, in_=ps_ap, func=AF.Copy), [ps_t], [out_buf])
        else:
            kb.op("dve", lambda e: e.tensor_copy(out=out_ap, in_=ps_ap), [ps_t], [out_buf])

    dXs = Buf("Xs", dram=True)
    dYs = Buf("Ys", dram=True)
    WbfG = nc.dram_tensor("WbfG", [E * KF * 128, KD * 256], BF16, kind="Internal").ap()
    WbfD = nc.dram_tensor("WbfD", [E * F, D], BF16, kind="Internal").ap()
    dWbf = Buf("Wbf", dram=True)

    class Conv:
        def __init__(self):
            self.jobs = []
            self.nl = 0
            self.ncs = 0
            self.cst = None

        def add_layer(self, m_):
            assert self.ncs == len(self.jobs)
            self.jobs = []
            self.nl = 0
            self.ncs = 0
            for e_ in range(E):
                for f in range(KF):
                    r0 = ((m_ * E + e_) * KF + f) * 128
                    d0 = (e_ * KF + f) * 128
                    self.jobs.append((wgum[r0:r0 + 128, :], WbfG[d0:d0 + 128, :], False))
                for k2 in range(KF // 2):
                    r0 = (m_ * E + e_) * F + k2 * 256
                    d0 = e_ * F + k2 * 256
                    self.jobs.append((wdnm[r0:r0 + 256, :].rearrange("(a p) d -> p a d", p=128), WbfD[d0:d0 + 256, :].rearrange("(a p) d -> p a d", p=128), True))

        def active(self):
            return self.ncs < len(self.jobs)

        def attach(self, st):
            self.cst = [kb.tile(st, [128, 2048], F32, "cvs") for _ in range(3)]
            self.cbt = [kb.tile(st, [128, 2048], BF16, "cvb") for _ in range(3)]

        def _load(self):
            i = self.nl
            src, dst, three = self.jobs[i]
            sg = self.cst[i % 3]
            o = sg.t[:, :].rearrange("p (a d) -> p a d", a=2) if three else sg.t[:, :]
            kb.dma("sp", o, src, [dummy], [sg], sg)
            self.nl += 1

        def _cast_store(self):
            i = self.ncs
            src, dst, three = self.jobs[i]
            sg = self.cst[i % 3]; cb_ = self.cbt[i % 3]
            kb.op("pool", lambda e: e.tensor_copy(out=cb_.t[:, :], in_=sg.t[:, :]), [sg], [cb_])
            i_ = cb_.t[:, :].rearrange("p (a d) -> p a d", a=2) if three else cb_.t[:, :]
            kb.dma("pool", dst, i_, [cb_], [dWbf], cb_)
            self.ncs += 1

        def step(self):
            if self.cst is None:
                return
            if self.ncs < self.nl:
                self._cast_store()
            if self.nl < len(self.jobs):
                self._load()

        def drain(self, everything=False):
            if self.cst is None:
                return
            while True:
                if self.ncs < self.nl:
                    self._cast_store()
                elif everything and self.nl < len(self.jobs):
                    self._load()
                else:
                    break
            self.cst = None

    conv = Conv()

    def routed_moe(l, last):
        m_ = l // 2
        clist = list(range(len(cfg.chunks)))
        if last:
            clist = clist[1:]
        tok0 = cfg.chunks[clist[0]][0]
        TL = sum(cfg.chunks[ci][1] for ci in clist)
        NTT = TL // 128
        NB = (2 * TL + E * (BS - 1)) // BS
        SUB = BS // 128
        groups = [clist[i:i + 2] for i in range(0, len(clist), 2)]
        with contextlib.ExitStack() as st:
            selA = kb.tile(st, [128, NTT, 8], F32, "selA")
            sel1A = kb.tile(st, [128, NTT, 8], F32, "sel1A")
            gwA = kb.tile(st, [128, NTT, 8], F32, "gwA")
            posI = kb.tile(st, [128, NTT * 2], mybir.dt.int32, "posI")
            wAB = kb.tile(st, [128, NTT * 2], F32, "wAB")
            gidxI = kb.tile(st, [128, NB * KF], mybir.dt.int32, "gidxI")
            didxI = kb.tile(st, [128, NB * KF], mybir.dt.int32, "didxI")
            with contextlib.ExitStack() as st1:
                h2tm = kb.tile(st1, [128, NTT, D], BF16, "h2tm")
                wr_t = kb.tile(st1, [128, KD, 8], F32, "wr")
                kb.dma("sp", wr_t.t[:, :, :], wr[m_].rearrange("p (k e) -> p k e", e=8), [dummy], [wr_t], wr_t)
                sm = [kb.tile(st1, [128, 8], F32, "sm%d" % i) for i in range(6)]
                s1 = [kb.tile(st1, [128, 1], F32, "s1%d" % i) for i in range(5)]
                zt = kb.tile(st1, [128, SUB, D], BF16, "zeros")
                kb.op("pool", lambda e: e.memset(zt.t[:, :, :], 0.0), [], [zt])
                for j in range(NB):
                    kb.dma("sp", XsD[j * BS:(j + 1) * BS, :].rearrange("(s p) d -> p s d", p=128), zt.t[:, :, :], [zt], [dXs], zt)
                with contextlib.ExitStack() as st2:
                    h2 = kb.tile(st2, [128, KD, 512], BF16, "h2r")
                    h2f = kb.tile(st2, [128, KD, 512], F32, "h2f")
                    nt4 = norm_tiles(st2)
                    for ci in clist:
                        t0, n = cfg.chunks[ci]
                        norm_chunk(nt4, XT, l, ci, V_NFFN, 3, 4, h2, 0, out_f32=h2f)
                        for tl_ in range(n // 128):
                            tt = (t0 - tok0) // 128 + tl_
                            cs = slice(tl_ * 128, (tl_ + 1) * 128)
                            p = kb.psum()
                            for k in range(KD):
                                kb.op("pe", mm(p.t[:, 0:8], h2f.t[:, k, cs], wr_t.t[:, k, :], k == 0, k == KD - 1), [h2f, wr_t], [p])
                            Lg, m1e, L2, selm, ex, gw = sm
                            m1, m2, nm1, ss, rs1 = s1
                            kb.op("dve", lambda e: e.tensor_copy(out=Lg.t[:, :], in_=p.t[:, 0:8]), [p], [Lg])
                            kb.op("dve", lambda e: e.reduce_max(out=m1.t[:, :], in_=Lg.t[:, :], axis=AX.X), [Lg], [m1])
                            kb.op("dve", lambda e: e.tensor_scalar(out=sel1A.t[:, tt, :], in0=Lg.t[:, :], scalar1=m1.t[:, 0:1], scalar2=None, op0=ALU.is_equal), [Lg, m1], [sel1A])
                            kb.op("dve", lambda e: e.tensor_scalar(out=m1e.t[:, :], in0=sel1A.t[:, tt, :], scalar1=-1e30, scalar2=None, op0=ALU.mult), [sel1A], [m1e])
                            kb.op("dve", lambda e: e.tensor_tensor(out=L2.t[:, :], in0=Lg.t[:, :], in1=m1e.t[:, :], op=ALU.add), [Lg, m1e], [L2])
                            kb.op("dve", lambda e: e.reduce_max(out=m2.t[:, :], in_=L2.t[:, :], axis=AX.X), [L2], [m2])
                            kb.op("dve", lambda e: e.tensor_scalar(out=selA.t[:, tt, :], in0=Lg.t[:, :], scalar1=m2.t[:, 0:1], scalar2=None, op0=ALU.is_ge), [Lg, m2], [selA])
                            kb.op("dve", lambda e: e.tensor_scalar(out=nm1.t[:, :], in0=m1.t[:, :], scalar1=-1.0, scalar2=None, op0=ALU.mult), [m1], [nm1])
                            kb.op("act", lambda e: e.activation(out=ex.t[:, :], in_=Lg.t[:, :], func=AF.Exp, bias=nm1.t[:, 0:1], scale=1.0), [Lg, nm1], [ex])
                            kb.op("dve", lambda e: e.tensor_tensor(out=ex.t[:, :], in0=ex.t[:, :], in1=selA.t[:, tt, :], op=ALU.mult), [selA], [ex])
                            kb.op("dve", lambda e: e.reduce_sum(out=ss.t[:, :], in_=ex.t[:, :], axis=AX.X), [ex], [ss])
                            kb.op("dve", lambda e: e.reciprocal(out=rs1.t[:, :], in_=ss.t[:, :]), [ss], [rs1])
                            kb.op("dve", lambda e: e.tensor_scalar(out=gwA.t[:, tt, :], in0=ex.t[:, :], scalar1=rs1.t[:, 0:1], scalar2=None, op0=ALU.mult), [ex, rs1], [gwA])
                            for hf in range(2):
                                p2 = kb.psum()
                                for kk in range(KD // 2):
                                    k = hf * (KD // 2) + kk
                                    kb.op("pe", mm(p2.t[:, kk * 128:(kk + 1) * 128], h2.t[:, k, cs], identb), [h2, cb], [p2])
                                evac(h2tm.t[:, tt, hf * 512:(hf + 1) * 512], h2tm, p2, p2.t[:, 0:512])
                    kb.barrier()
                with contextlib.ExitStack() as st2:
                    selb = kb.tile(st2, [128, NTT, 8], BF16, "selb")
                    cnt = kb.tile(st2, [128, 8], F32, "cnt")
                    nblk = kb.tile(st2, [128, 8], F32, "nblk")
                    pend = kb.tile(st2, [128, 8], F32, "pend")
                    pstart = kb.tile(st2, [128, 8], F32, "pstart")
                    tmpT = kb.tile(st2, [128, NTHR], F32, "tmpT")
                    sT = kb.tile(st2, [128, 1], F32, "sT")
                    bexp = kb.tile(st2, [128, NB], F32, "bexp")
                    tmpB = kb.tile(st2, [128, NB], F32, "tmpB")
                    eoff = kb.tile(st2, [128, NB], F32, "eoff")
                    idxf = kb.tile(st2, [128, NB * KF], F32, "idxf")
                    posf = kb.tile(st2, [128, 8], F32, "posf")
                    sel2 = kb.tile(st2, [128, 8], F32, "sel2")
                    tm8 = kb.tile(st2, [128, 8], F32, "tm8")
                    posAB = kb.tile(st2, [128, NTT * 2], F32, "posAB")
                    kb.op("dve", lambda e: e.tensor_copy(out=selb.t[:, :, :], in_=selA.t[:, :, :]), [selA], [selb])
                    pc = kb.psum()
                    for tt in range(NTT):
                        kb.op("pe", mm(pc.t[:, 0:8], ones128, selb.t[:, tt, :], tt == 0, tt == NTT - 1), [selb, cb], [pc])
                    kb.op("dve", lambda e: e.tensor_copy(out=cnt.t[:, :], in_=pc.t[:, 0:8]), [pc], [cnt])
                    for e_ in range(E):
                        kb.op("dve", lambda e: e.tensor_scalar(out=tmpT.t[:, :], in0=thr_c, scalar1=cnt.t[:, e_:e_ + 1], scalar2=None, op0=ALU.is_ge), [cf, cnt], [tmpT])
                        kb.op("dve", lambda e: e.reduce_sum(out=sT.t[:, :], in_=tmpT.t[:, :], axis=AX.X), [tmpT], [sT])
                        kb.op("dve", lambda e: e.tensor_scalar(out=nblk.t[:, e_:e_ + 1], in0=sT.t[:, :], scalar1=-1.0, scalar2=float(NTHR), op0=ALU.mult, op1=ALU.add), [sT], [nblk])
                    kb.op("dve", lambda e: e.tensor_copy(out=pend.t[:, 0:1], in_=nblk.t[:, 0:1]), [nblk], [pend])
                    for e_ in range(1, E):
                        kb.op("dve", lambda e: e.tensor_tensor(out=pend.t[:, e_:e_ + 1], in0=pend.t[:, e_ - 1:e_], in1=nblk.t[:, e_:e_ + 1], op=ALU.add), [nblk], [pend])
                    kb.op("dve", lambda e: e.tensor_tensor(out=pstart.t[:, :], in0=pend.t[:, :], in1=nblk.t[:, :], op=ALU.subtract), [pend, nblk], [pstart])
                    kb.op("dve", lambda e: e.tensor_scalar(out=pstart.t[:, :], in0=pstart.t[:, :], scalar1=float(BS), scalar2=None, op0=ALU.mult), [], [pstart])
                    for e_ in range(E):
                        if e_ == 0:
                            kb.op("dve", lambda e: e.tensor_scalar(out=bexp.t[:, :], in0=jrow_c[:, 0:NB], scalar1=pend.t[:, 0:1], scalar2=None, op0=ALU.is_ge), [cf, pend], [bexp])
                        else:
                            kb.op("dve", lambda e: e.tensor_scalar(out=tmpB.t[:, :], in0=jrow_c[:, 0:NB], scalar1=pend.t[:, e_:e_ + 1], scalar2=None, op0=ALU.is_ge), [cf, pend], [tmpB])
                            kb.op("dve", lambda e: e.tensor_tensor(out=bexp.t[:, :], in0=bexp.t[:, :], in1=tmpB.t[:, :], op=ALU.add), [tmpB], [bexp])
                    kb.op("dve", lambda e: e.tensor_scalar(out=bexp.t[:, :], in0=bexp.t[:, :], scalar1=float(E - 1), scalar2=None, op0=ALU.min), [], [bexp])
                    for (mult_, base_, dstI) in ((float(KF * 128), 0.0, gidxI), (float(F), 0.0, didxI)):
                        kb.op("dve", lambda e: e.tensor_scalar(out=eoff.t[:, :], in0=bexp.t[:, :], scalar1=mult_, scalar2=base_, op0=ALU.mult, op1=ALU.add), [bexp], [eoff])
                        for j in range(NB):
                            kb.op("dve", lambda e: e.tensor_scalar(out=idxf.t[:, j * KF:(j + 1) * KF], in0=cidx_c, scalar1=eoff.t[:, j:j + 1], scalar2=None, op0=ALU.add), [cf, eoff], [idxf])
                        kb.op("dve", lambda e: e.tensor_copy(out=dstI.t[:, :], in_=idxf.t[:, :]), [idxf], [dstI])
                    for tt in range(NTT):
                        pp = kb.psum()
                        for t2_ in range(tt):
                            kb.op("pe", mm(pp.t[:, 0:8], ones128, selb.t[:, t2_, :], t2_ == 0, False), [selb, cb], [pp])
                        kb.op("pe", mm(pp.t[:, 0:8], ustrict, selb.t[:, tt, :], tt == 0, True), [selb, cb], [pp])
                        kb.op("dve", lambda e: e.tensor_tensor(out=posf.t[:, :], in0=pp.t[:, 0:8], in1=pstart.t[:, :], op=ALU.add), [pp, pstart], [posf])
                        kb.op("dve", lambda e: e.tensor_tensor(out=sel2.t[:, :], in0=selA.t[:, tt, :], in1=sel1A.t[:, tt, :], op=ALU.subtract), [selA, sel1A], [sel2])
                        for a_, (selX, selXb) in enumerate(((sel1A.t[:, tt, :], sel1A), (sel2.t[:, :], sel2))):
                            kb.op("dve", lambda e: e.tensor_tensor(out=tm8.t[:, :], in0=posf.t[:, :], in1=selX, op=ALU.mult), [posf, selXb], [tm8])
                            kb.op("dve", lambda e: e.reduce_sum(out=posAB.t[:, tt * 2 + a_:tt * 2 + a_ + 1], in_=tm8.t[:, :], axis=AX.X), [tm8], [posAB])
                            kb.op("dve", lambda e: e.tensor_tensor(out=tm8.t[:, :], in0=gwA.t[:, tt, :], in1=selX, op=ALU.mult), [gwA, selXb], [tm8])
                            kb.op("dve", lambda e: e.reduce_sum(out=wAB.t[:, tt * 2 + a_:tt * 2 + a_ + 1], in_=tm8.t[:, :], axis=AX.X), [tm8], [wAB])
                    kb.op("dve", lambda e: e.tensor_copy(out=posI.t[:, :], in_=posAB.t[:, :]), [posAB], [posI])
                    scs = [Buf("scs%d" % i_) for i_ in range(8)]
                    for tt in range(NTT):
                        for a_ in range(2):
                            kb.idma(XsD[:, :], bass.IndirectOffsetOnAxis(ap=posI.t[:, tt * 2 + a_:tt * 2 + a_ + 1], axis=0), h2tm.t[:, tt, :], None, [h2tm, posI, dXs], [Buf("snk")], scs[(tt * 2 + a_) % 8])
                    kb.barrier()
            with contextlib.ExitStack() as st1:
                Xg = [kb.tile(st1, [128, SUB, D], BF16, "Xg") for _ in range(2)]
                XTb = kb.tile(st1, [128, KD, BS], BF16, "XTb")
                actT = kb.tile(st1, [128, KF, BS], BF16, "actT")
                gbb = [kb.tile(st1, [128, KD * 256], BF16, "gbb") for _ in range(4)]
                dbb = [kb.tile(st1, [128, D], BF16, "dbb") for _ in range(4)]
                sl = [kb.tile(st1, [128, BS], BF16, "silu") for _ in range(3)]
                yst = [kb.tile(st1, [128, D], F32, "yst") for _ in range(2)]
                wc = 0
                for j in range(NB):
                    xg = Xg[j % 2]
                    kb.dma("sp", xg.t[:, :, :], XsD[j * BS:(j + 1) * BS, :].rearrange("(s p) d -> p s d", p=128), [dXs], [xg], xg)
                    for k in range(KD):
                        p = kb.psum()
                        for s_ in range(SUB):
                            kb.op("pe", mm(p.t[:, s_ * 128:(s_ + 1) * 128], xg.t[:, s_, k * 128:(k + 1) * 128], identb), [xg, cb], [p])
                        evac(XTb.t[:, k, :], XTb, p, p.t[:, 0:BS])
                    for f in range(KF):
                        gb = gbb[wc % 4]; wc += 1
                        kb.idma(gb.t[:, :], None, WbfG[:, :], bass.IndirectOffsetOnAxis(ap=gidxI.t[:, j * KF + f:j * KF + f + 1], axis=0), [gidxI, dWbf], [gb], gb)
                        pg = kb.psum(); pu = kb.psum()
                        for k in range(KD):
                            kb.op("pe", mm(pg.t[:, 0:BS], gb.t[:, k * 256:k * 256 + 128], XTb.t[:, k, :], k == 0, k == KD - 1), [gb, XTb], [pg])
                        for k in range(KD):
                            kb.op("pe", mm(pu.t[:, 0:BS], gb.t[:, k * 256 + 128:k * 256 + 256], XTb.t[:, k, :], k == 0, k == KD - 1), [gb, XTb], [pu])
                        sl_ = sl[f % 3]
                        kb.op("act", lambda e: e.activation(out=sl_.t[:, :], in_=pg.t[:, 0:BS], func=AF.Silu), [pg], [sl_])
                        kb.op("dve", lambda e: e.tensor_tensor(out=actT.t[:, f, :], in0=pu.t[:, 0:BS], in1=sl_.t[:, :], op=ALU.mult), [pu, sl_], [actT])
                    for kf in range(KF):
                        db = dbb[wc % 4]; wc += 1
                        kb.idma(db.t[:, :], None, WbfD[:, :], bass.IndirectOffsetOnAxis(ap=didxI.t[:, j * KF + kf:j * KF + kf + 1], axis=0), [didxI, dWbf], [db], db)
                        for s_ in range(SUB):
                            for hf in range(D // 512):
                                pb_ = kb.ps[(s_ * (D // 512) + hf) % 8]
                                kb.op("pe", mm(pb_.t[:, 0:512], actT.t[:, kf, s_ * 128:(s_ + 1) * 128], db.t[:, hf * 512:(hf + 1) * 512], kf == 0, kf == KF - 1), [actT, db], [pb_])
                    for s_ in range(SUB):
                        y_ = yst[s_ % 2]
                        for hf in range(D // 512):
                            pb_ = kb.ps[(s_ * (D // 512) + hf) % 8]
                            evac(y_.t[:, hf * 512:(hf + 1) * 512], y_, pb_, pb_.t[:, 0:512])
                        r0 = j * BS + s_ * 128
                        kb.dma("sp", YsD[r0:r0 + 128, :], y_.t[:, :], [y_], [dYs], y_)
                kb.barrier()
            with contextlib.ExitStack() as st1:
                yA = [kb.tile(st1, [128, D], F32, "yA") for _ in range(4)]
                yB = [kb.tile(st1, [128, D], F32, "yB") for _ in range(4)]
                u4 = [kb.tile(st1, [128, 4, D], F32, "u4") for _ in range(2)]
                xts = [kb.tile(st1, [128, 512], F32, "x5") for _ in range(3)]
                tr5 = [kb.tile(st1, [128, 512], F32, "tr5") for _ in range(2)]
                xc = 0
                for qi, ci in enumerate(clist):
                    t0, n = cfg.chunks[ci]
                    s_i = 1 if ci == 0 else 0
                    u_ = u4[qi % 2]
                    for tl_ in range(n // 128):
                        tt = (t0 - tok0) // 128 + tl_
                        a_ = yA[tt % 4]; b_ = yB[tt % 4]
                        kb.idma(a_.t[:, :], None, YsD[:, :], bass.IndirectOffsetOnAxis(ap=posI.t[:, tt * 2:tt * 2 + 1], axis=0), [posI, dYs], [a_], a_)
                        kb.idma(b_.t[:, :], None, YsD[:, :], bass.IndirectOffsetOnAxis(ap=posI.t[:, tt * 2 + 1:tt * 2 + 2], axis=0), [posI, dYs], [b_], b_)
                        kb.op("dve", lambda e: e.tensor_scalar(out=u_.t[:, tl_, :], in0=a_.t[:, :], scalar1=wAB.t[:, tt * 2:tt * 2 + 1], scalar2=None, op0=ALU.mult), [a_, wAB], [u_])
                        kb.op("dve", lambda e: e.scalar_tensor_tensor(out=u_.t[:, tl_, :], in0=b_.t[:, :], scalar=wAB.t[:, tt * 2 + 1:tt * 2 + 2], in1=u_.t[:, tl_, :], op0=ALU.mult, op1=ALU.add), [b_, wAB], [u_])
                    for k in range(KD):
                        p = kb.psum()
                        for tl_ in range(n // 128):
                            kb.op("pe", mm(p.t[:, tl_ * 128:(tl_ + 1) * 128], u_.t[:, tl_, k * 128:(k + 1) * 128], id128f), [u_, cf], [p])
                        tr_ = tr5[xc % 2]
                        x_ = xts[xc % 3]; xc += 1
                        js = slice(k * 128, (k + 1) * 128)
                        kb.dma("sp", x_.t[:, 0:n], XT[js, t0:t0 + n], [dummy], [x_], x_)
                        kb.op("act", lambda e: e.activation(out=tr_.t[:, 0:n], in_=p.t[:, 0:n], func=AF.Identity, scale=mod(l, 5, k, s_i)), [p, modv], [tr_])
                        kb.op("dve", lambda e: e.tensor_tensor(out=x_.t[:, 0:n], in0=x_.t[:, 0:n], in1=tr_.t[:, 0:n], op=ALU.add), [tr_], [x_])
                        kb.dma("pool", XT[js, t0:t0 + n], x_.t[:, 0:n], [x_], [Buf("snk")], x_)
                kb.barrier()

    for l in range(L):
        last = l == L - 1
        moe = l % 2 == 1
        xsrc = xT_in if l == 0 else XT
        nch = len(cfg.chunks)
        halves = [list(range(0, (nch + 1) // 2)), list(range((nch + 1) // 2, nch))]
        for hchunks in halves:
          if not hchunks:
              continue
          hb = cfg.chunks[hchunks[0]][0]
          W = sum(cfg.chunks[ci][1] for ci in hchunks)
          with contextlib.ExitStack() as st:
            hT = kb.tile(st, [128, KD, W], BF16, "hT")
            with contextlib.ExitStack() as st2:
                nts = [norm_tiles(st2) for _ in range(2)]
                for i_, ci in enumerate(hchunks):
                    t0, n = cfg.chunks[ci]
                    norm_chunk(nts[i_ % 2], xsrc, l, ci, V_NMIX, 0, 1, hT, t0 - hb)
                kb.barrier()
            rt = kb.tile(st, [128, 4, W], BF16, "ropet")
            if stop == "P1a":
                return nc
            kb.dma("sp", rt.t[:, :, :], ropet[:, :, hb:hb + W], [dummy], [rt], rt)
            ropeg_cos = rt.t[:, 0, :]; ropeg_sin = rt.t[:, 1, :]; ropem_cos = rt.t[:, 2, :]; ropem_sin = rt.t[:, 3, :]
            stgs = [kb.tile(st, [128, KD * 128], F32, "wstg") for _ in range(3)]
            wbs = [kb.tile(st, [128, KD, 128], BF16, "wb") for _ in range(4)]
            outs = [kb.tile(st, [128, 512], BF16, "o1") for _ in range(4)]
            sqb = [kb.tile(st, [128, 512], BF16, "sqb") for _ in range(2)]
            rsb = [kb.tile(st, [128, 512], F32, "rsb") for _ in range(2)]
            tf = [kb.tile(st, [128, 512], F32, "tf") for _ in range(4)]
            cnt = {"w": 0, "o": 0, "s": 0, "t": 0, "r": 0}

            wseq = [0, 1, 2, 3, 7, 8, 9, 13, 10, 14, 11, 15, 12, 16] + list(range(19, 19 + 24)) + [4, 17, 18, 5, 6]
            wstate = {"issued": 0, "ready": {}}

            def issue_next():
                i = wstate["issued"]
                if i >= len(wseq):
                    return
                sg = stgs[i % 3]; wb_ = wbs[i % 4]
                kb.dma("sp", sg.t[:, :], w1[l, wseq[i]], [dummy], [sg], sg)
                cast(wb_.t[:, :, :], wb_, sg.t[:, :].rearrange("p (k c) -> p k c", k=KD), sg)
                wstate["ready"][i] = wb_
                wstate["issued"] += 1

            def getw(nt):
                i = cnt["w"]; cnt["w"] += 1
                assert wseq[i] == nt
                while wstate["issued"] <= i + 1 and wstate["issued"] < len(wseq):
                    issue_next()
                return wstate["ready"].pop(i)

            def proj(wb_, ci):
                t0, n = cfg.chunks[ci]
                p = kb.psum()
                for k in range(KD):
                    kb.op("pe", mm(p.t[:, 0:n], wb_.t[:, k, :], hT.t[:, k, t0 - hb:t0 - hb + n], k == 0, k == KD - 1), [wb_, hT], [p])
                return p

            def nxt(lst, key):
                i = cnt[key]; cnt[key] += 1
                return lst[i % len(lst)]

            def store(o_, n, dst_ap, dbuf):
                kb.dma("pool", dst_ap, o_.t[:, 0:n], [o_], [dbuf], o_)

            def plain_group(tiles, dst, dbuf, func=None):
                for j, nt in enumerate(tiles):
                    wb_ = getw(nt)
                    for ci in hchunks:
                        t0, n = cfg.chunks[ci]
                        p = proj(wb_, ci)
                        o_ = nxt(outs, "o")
                        evac(o_.t[:, 0:n], o_, p, p.t[:, 0:n], func)
                        store(o_, n, dst[j * 128:(j + 1) * 128, t0:t0 + n], dbuf)

            def sq_rstd(plist, ones_ap, dim, n):
                rs_ = nxt(rsb, "r")
                pc = kb.psum()
                for i_, pa in enumerate(plist):
                    sq_ = nxt(sqb, "s")
                    kb.op("act", lambda e: e.activation(out=sq_.t[:, 0:n], in_=pa.t[:, 0:n], func=AF.Square), [pa], [sq_])
                    kb.op("pe", mm(pc.t[:, 0:n], ones_ap, sq_.t[:, 0:n], i_ == 0, i_ == len(plist) - 1), [sq_, cb], [pc])
                rstd_from(pc, n, dim, rs_)
                return rs_

            def rope_norm_group(tiles, rtiles, gcol, grcol, dst, dbuf):
                for j in range(len(tiles)):
                    wa = getw(tiles[j]); wr_ = getw(rtiles[j])
                    for ci in hchunks:
                        t0, n = cfg.chunks[ci]
                        c0 = t0 - hb
                        pa = proj(wa, ci); pb = proj(wr_, ci)
                        STEP = int(os.environ.get("STEP", "99"))
                        if STEP < 1:
                            continue
                        rs_ = sq_rstd([pa], bd64, 64, n)
                        t1 = nxt(tf, "t"); t2 = nxt(tf, "t")
                        if STEP < 2:
                            continue
                        kb.op("act", lambda e: e.activation(out=t1.t[:, 0:n], in_=pa.t[:, 0:n], func=AF.Identity, scale=vcol(l, gcol)), [pa, vc], [t1])
                        kb.op("act", lambda e: e.activation(out=t2.t[:, 0:n], in_=pb.t[:, 0:n], func=AF.Identity, scale=vcol(l, grcol)), [pb, vc], [t2])
                        kb.op("dve", lambda e: e.tensor_tensor(out=t1.t[:, 0:n], in0=t1.t[:, 0:n], in1=ropeg_cos[:, c0:c0 + n], op=ALU.mult), [rt], [t1])
                        kb.op("dve", lambda e: e.tensor_tensor(out=t2.t[:, 0:n], in0=t2.t[:, 0:n], in1=ropeg_sin[:, c0:c0 + n], op=ALU.mult), [rt], [t2])
                        if STEP < 3:
                            continue
                        kb.op("pool", lambda e: e.tensor_tensor(out=t1.t[:, 0:n], in0=t1.t[:, 0:n], in1=t2.t[:, 0:n], op=ALU.add), [t2], [t1])
                        o_ = nxt(outs, "o")
                        if STEP < 4:
                            continue
                        kb.op("dve", lambda e: e.tensor_tensor(out=o_.t[:, 0:n], in0=t1.t[:, 0:n], in1=rs_.t[:, 0:n], op=ALU.mult), [t1, rs_], [o_])
                        store(o_, n, dst[j * 128:(j + 1) * 128, t0:t0 + n], dbuf)

            plain_group([0, 1], KnaT, dQK["KnaT"])
            if stop == "P1b":
                kb.barrier(); return nc
            rope_norm_group([2], [3], V_KG, V_KGR, KgT, dQK["KgT"])
            if stop == "P1c":
                kb.barrier(); return nc
            plain_group([7, 8], QnaT, dQK["QnaT"])
            rope_norm_group([9, 10, 11, 12], [13, 14, 15, 16], V_QG, V_QGR, QgT, dQK["QgT"])
            plain_group(list(range(19, 19 + 24)), GT, dQK["GT"], func=AF.Sigmoid)
            if stop == "P1d":
                kb.barrier(); return nc
            ckvn = kb.tile(st, [128, W], BF16, "ckvn")
            cqn = kb.tile(st, [128, 2, W], BF16, "cqn")
            krope = kb.tile(st, [128, W], BF16, "krope")
            wa = getw(4)
            for ci in hchunks:
                t0, n = cfg.chunks[ci]
                c0 = t0 - hb
                pa = proj(wa, ci)
                rs_ = sq_rstd([pa], ones128, 128, n)
                t1 = nxt(tf, "t")
                kb.op("act", lambda e: e.activation(out=t1.t[:, 0:n], in_=pa.t[:, 0:n], func=AF.Identity, scale=vcol(l, V_KVL)), [pa, vc], [t1])
                kb.op("dve", lambda e: e.tensor_tensor(out=ckvn.t[:, c0:c0 + n], in0=t1.t[:, 0:n], in1=rs_.t[:, 0:n], op=ALU.mult), [t1, rs_], [ckvn])
            wa = getw(17); wb2 = getw(18)
            for ci in hchunks:
                t0, n = cfg.chunks[ci]
                c0 = t0 - hb
                pa = proj(wa, ci); pb = proj(wb2, ci)
                rs_ = sq_rstd([pa, pb], ones128, 256, n)
                t1 = nxt(tf, "t"); t2 = nxt(tf, "t")
                kb.op("act", lambda e: e.activation(out=t1.t[:, 0:n], in_=pa.t[:, 0:n], func=AF.Identity, scale=vcol(l, V_QL)), [pa, vc], [t1])
                kb.op("act", lambda e: e.activation(out=t2.t[:, 0:n], in_=pb.t[:, 0:n], func=AF.Identity, scale=vcol(l, V_QL + 1)), [pb, vc], [t2])
                kb.op("dve", lambda e: e.tensor_tensor(out=cqn.t[:, 0, c0:c0 + n], in0=t1.t[:, 0:n], in1=rs_.t[:, 0:n], op=ALU.mult), [t1, rs_], [cqn])
                kb.op("dve", lambda e: e.tensor_tensor(out=cqn.t[:, 1, c0:c0 + n], in0=t2.t[:, 0:n], in1=rs_.t[:, 0:n], op=ALU.mult), [t2, rs_], [cqn])
            wa = getw(5); wb2 = getw(6)
            for ci in hchunks:
                t0, n = cfg.chunks[ci]
                c0 = t0 - hb
                pa = proj(wa, ci); pb = proj(wb2, ci)
                t1 = nxt(tf, "t"); t2 = nxt(tf, "t")
                kb.op("dve", lambda e: e.tensor_tensor(out=t1.t[64:96, 0:n], in0=pa.t[64:96, 0:n], in1=ropem_cos[64:96, c0:c0 + n], op=ALU.mult), [pa, rt], [t1])
                kb.op("dve", lambda e: e.tensor_tensor(out=t2.t[64:96, 0:n], in0=pb.t[64:96, 0:n], in1=ropem_sin[64:96, c0:c0 + n], op=ALU.mult), [pb, rt], [t2])
                kb.op("dve", lambda e: e.tensor_tensor(out=krope.t[64:96, c0:c0 + n], in0=t1.t[64:96, 0:n], in1=t2.t[64:96, 0:n], op=ALU.add), [t1, t2], [krope])
            w2s = kb.tile(st, [128, 1536], F32, "w2s")
            wuq_b = kb.tile(st, [128, 2, 384], BF16, "wuq")
            wuqr_b = kb.tile(st, [128, 2, 384], BF16, "wuqr")
            wkk_b = kb.tile(st, [128, 256], BF16, "wkk")
            wkv_b = kb.tile(st, [128, 256], BF16, "wkv")
            wv_b = kb.tile(st, [128, KD, 384], BF16, "wvb")
            for src_, dst_, wd_, db_ in ((wuq[l], wuq_b.t[:, :, :].rearrange("p a b -> p (a b)"), 768, wuq_b), (wuqr[l], wuqr_b.t[:, :, :].rearrange("p a b -> p (a b)"), 768, wuqr_b),
                                        (wukvk[l], wkk_b.t[:, :], 256, wkk_b), (wukvv[l], wkv_b.t[:, :], 256, wkv_b),
                                        (wv[l][:, 0:1536], wv_b.t[:, 0:KD // 2, :].rearrange("p a b -> p (a b)"), 1536, wv_b),
                                        (wv[l][:, 1536:3072], wv_b.t[:, KD // 2:KD, :].rearrange("p a b -> p (a b)"), 1536, wv_b)):
                kb.dma("sp", w2s.t[:, 0:wd_], src_, [dummy], [w2s], w2s)
                kb.op("dve", lambda e: e.tensor_copy(out=dst_, in_=w2s.t[:, 0:wd_]), [w2s], [db_])
            kst = [kb.tile(st, [128, 512], BF16, "kst") for _ in range(3)]
            if stop == "P1e":
                kb.barrier(); return nc
            for ci in hchunks:
                t0, n = cfg.chunks[ci]
                c0 = t0 - hb
                for h in range(4):
                    p = kb.psum()
                    kb.op("pe", mm(p.t[0:64, 0:n], wkk_b.t[:, h * 64:(h + 1) * 64], ckvn.t[:, c0:c0 + n]), [wkk_b, ckvn], [p])
                    o_ = nxt(kst, "o")
                    evac(o_.t[0:64, 0:n], o_, p, p.t[0:64, 0:n])
                    kb.op("pool", lambda e: e.tensor_copy(out=o_.t[64:96, 0:n], in_=krope.t[64:96, c0:c0 + n]), [krope], [o_])
                    kb.dma("pool", KmT[h * 96:(h + 1) * 96, t0:t0 + n], o_.t[0:96, 0:n], [o_], [dQK["KmT"]], o_)
                    p = kb.psum(); pr = kb.psum()
                    for k in range(2):
                        kb.op("pe", mm(p.t[0:96, 0:n], wuq_b.t[:, k, h * 96:(h + 1) * 96], cqn.t[:, k, c0:c0 + n], k == 0, k == 1), [wuq_b, cqn], [p])
                    for k in range(2):
                        kb.op("pe", mm(pr.t[0:96, 0:n], wuqr_b.t[:, k, h * 96:(h + 1) * 96], cqn.t[:, k, c0:c0 + n], k == 0, k == 1), [wuqr_b, cqn], [pr])
                    o_ = nxt(kst, "o")
                    evac(o_.t[0:64, 0:n], o_, p, p.t[0:64, 0:n])
                    t1 = nxt(tf, "t"); t2 = nxt(tf, "t")
                    kb.op("dve", lambda e: e.tensor_tensor(out=t1.t[64:96, 0:n], in0=p.t[64:96, 0:n], in1=ropem_cos[64:96, c0:c0 + n], op=ALU.mult), [p, rt], [t1])
                    kb.op("dve", lambda e: e.tensor_tensor(out=t2.t[64:96, 0:n], in0=pr.t[64:96, 0:n], in1=ropem_sin[64:96, c0:c0 + n], op=ALU.mult), [pr, rt], [t2])
                    kb.op("dve", lambda e: e.tensor_tensor(out=o_.t[64:96, 0:n], in0=t1.t[64:96, 0:n], in1=t2.t[64:96, 0:n], op=ALU.add), [t1, t2], [o_])
                    kb.dma("pool", QmT[h * 96:(h + 1) * 96, t0:t0 + n], o_.t[0:96, 0:n], [o_], [dQK["QmT"]], o_)
            if stop == "P1f":
                kb.barrier(); return nc
            vst = [kb.tile(st, [128, 10, 65], BF16, "vst") for _ in range(3)]
            for v_ in vst:
                kb.op("pool", lambda e: e.memset(v_.t[:, :, :], 1.0), [], [v_])
            for tt in range(hb // 128, (hb + W) // 128):
                c0 = tt * 128 - hb
                p = kb.psum(); p2 = kb.psum()
                for k in range(KD):
                    kb.op("pe", mm(p.t[:, 0:384], hT.t[:, k, c0:c0 + 128], wv_b.t[:, k, :], k == 0, k == KD - 1), [hT, wv_b], [p])
                kb.op("pe", mm(p2.t[:, 0:256], ckvn.t[:, c0:c0 + 128], wkv_b.t[:, :]), [ckvn, wkv_b], [p2])
                v_ = nxt(vst, "o")
                kb.op("act", lambda e: e.activation(out=v_.t[:, 0:6, 0:64], in_=p.t[:, 0:384].rearrange("p (h d) -> p h d", h=6), func=AF.Copy), [p], [v_])
                kb.op("dve", lambda e: e.tensor_copy(out=v_.t[:, 6:10, 0:64], in_=p2.t[:, 0:256].rearrange("p (h d) -> p h d", h=4)), [p2], [v_])
                rs_ = slice(tt * 128, (tt + 1) * 128)
                kb.dma("pool", Vna[rs_, :].rearrange("t (h d) -> t h d", h=4), v_.t[:, 0:4, :], [v_], [dQK["Vna"]], v_)
                kb.dma("pool", Vg[rs_, :].rearrange("t (h d) -> t h d", h=2), v_.t[:, 4:6, :], [v_], [dQK["Vg"]], v_)
                kb.dma("pool", Vm[rs_, :].rearrange("t (h d) -> t h d", h=4), v_.t[:, 6:10, :], [v_], [dQK["Vm"]], v_)
            kb.barrier()

        if stop == "P1":
            return nc
        PS_S = (0, 4); PS_O = (4, 2); PS_B = (6, 2)

        def attn_phase(name, dk, nh, nkv, Ksrc, Qsrc, Vsrc, scale, yrow0, na=False):
            with contextlib.ExitStack() as st:
                pk = 128 if dk == 64 else dk
                Kt = kb.tile(st, [pk, nkv, T], BF16, "K" + name)
                Vt = kb.tile(st, [128, NKT, nkv, 65], BF16, "V" + name)
                if pk != dk:
                    kb.op("pool", lambda e: e.memset(Kt.t[dk:pk, :, :], 0.0), [], [Kt])
                kb.dma("sp", Kt.t[0:dk, :, :], Ksrc.rearrange("(h d) t -> d h t", d=dk), [dQK["K%sT" % name]], [Kt], Kt)
                kb.dma("sp", Vt.t[:, :, :, :], Vsrc.rearrange("(k p) (h d) -> p k h d", p=128, d=65), [dQK["V" + name]], [Vt], Vt)
                if na:
                    Vo = kb.tile(st, [128, NKT - CT - 1, nkv, 65], BF16, "Vo")
                    kb.dma("sp", Vo.t[:, :, :, :], Vsrc[C + 64:T - 64, :].rearrange("(k p) (h d) -> p k h d", p=128, d=65), [dQK["V" + name]], [Vo], Vo)
                    rst = kb.tile(st, [64, 3840], F32, "rpbst")
                    Tc = kb.tile(st, [128, 4, 15, 64], BF16, "Tcat")
                    kb.op("pool", lambda e: e.memset(Tc.t[64:128, :, :, :], 0.0), [], [Tc])
                    kb.dma("sp", rst.t[:, :], rpbT[l], [dummy], [rst], rst)
                    ngm = kb.tile(st, [64, 3840], BF16, "negm")
                    kb.dma("sp", ngm.t[:, :], negm[:, :], [dummy], [ngm], ngm)
                    kb.op("dve", lambda e: e.scalar_tensor_tensor(out=Tc.t[0:64, :, :, :].rearrange("p a b c -> p (a b c)"), in0=rst.t[:, :], scalar=8.0, in1=ngm.t[:, :], op0=ALU.mult, op1=ALU.add), [rst, ngm], [Tc])
                Qs = [kb.tile(st, [pk, nh, 512], BF16, "Q" + name) for _ in range(2)]
                if pk != dk:
                    for q_ in Qs:
                        kb.op("pool", lambda e, q_=q_: e.memset(q_.t[dk:pk, :, :], 0.0), [], [q_])
                Ps = [kb.tile(st, [128, 512], BF16, "P" + name) for _ in range(4)]
                Rt = kb.tile(st, [65, 512], F32, "Rt")
                bcs = [kb.tile(st, [64, 512], F32, "bc") for _ in range(2)]
                ys = [kb.tile(st, [64, 512], BF16, "ys") for _ in range(3)]
                kb.op("pool", lambda e: e.memset(Rt.t[:, :], 0.0), [], [Rt])
                if conv.active():
                    conv.attach(st)
                c_ = {"p": 0, "y": 0, "b": 0}
                pnorm = {"f": None}
                clist = list(range(len(cfg.chunks)))
                if last:
                    clist = clist[1:]
                for qi, ci in enumerate(clist):
                    t0, n = cfg.chunks[ci]
                    Q = Qs[qi % 2]
                    kb.dma("sp", Q.t[0:dk, :, 0:n], Qsrc[:, t0:t0 + n].rearrange("(h d) t -> d h t", d=dk), [dQK["Q%sT" % name]], [Q], Q)
                    for h in range(nh):
                        kvh = h * nkv // nh
                        po = kb.psum(PS_O)
                        if ci == 0 or not na:
                            kts = list(range(CT)) if ci == 0 else list(range(NKT))
                            LA = 3
                            pend = []
                            for i_, kt in enumerate(kts):
                                ps_ = kb.psum(PS_S)
                                kb.op("pe", mm(ps_.t[:, 0:n], Kt.t[:, kvh, kt * 128:(kt + 1) * 128], Q.t[:, h, 0:n]), [Kt, Q], [ps_])
                                pend.append((ps_, kt))
                                if i_ == min(8, len(kts) - 1) and pnorm["f"] is not None:
                                    pnorm["f"](); pnorm["f"] = None
                                if len(pend) > LA or i_ == len(kts) - 1:
                                    while pend and (len(pend) > LA or i_ == len(kts) - 1):
                                        ps2, kt2 = pend.pop(0)
                                        P = Ps[c_["p"] % 4]; c_["p"] += 1
                                        kb.op("act", lambda e, P=P, ps2=ps2: e.activation(out=P.t[:, 0:n], in_=ps2.t[:, 0:n], func=AF.Exp, scale=scale), [ps2], [P])
                                        kb.op("pe", mm(po.t[0:65, 0:n], Vt.t[:, kt2, kvh, :], P.t[:, 0:n], kt2 == kts[0], kt2 == kts[-1]), [Vt, P], [po])
                        else:
                            r0 = (t0 - C) // 64
                            pendu = []

                            def finish_unit(u):
                                ps_u, groups_u, qs_u = u
                                ng = len(groups_u)
                                P = Ps[c_["p"] % 4]; c_["p"] += 1
                                kb.op("act", lambda e: e.activation(out=P.t[:, 0:ng * 64], in_=ps_u.t[:, 0:ng * 64], func=AF.Exp, scale=scale), [ps_u], [P])
                                for gi, (tk, vt, vb, dr) in enumerate(groups_u):
                                    kb.op("pe", mm(po.t[0:65, qs_u], vt, P.t[:, gi * 64:(gi + 1) * 64], gi == 0, gi == ng - 1), [vb, P], [po])
                            for rl in range(n // 64):
                                r = r0 + rl
                                row0 = min(max(r - 4, 0), R - 8)
                                qs = slice(rl * 64, (rl + 1) * 64)
                                ps_ = kb.psum(PS_S)
                                groups = []
                                for g in range(4):
                                    tk = C + (row0 + 2 * g) * 64
                                    if row0 % 2 == 0:
                                        vt = Vt.t[:, tk // 128, kvh, :]
                                        vb = Vt
                                    else:
                                        vt = Vo.t[:, (tk - C - 64) // 128, kvh, :]
                                        vb = Vo
                                    groups.append((tk, vt, vb, row0 + 2 * g - r))
                                for g in range(CT):
                                    groups.append((g * 128, Vt.t[:, g, kvh, :], Vt, None))
                                for gi, (tk, vt, vb, dr) in enumerate(groups):
                                    osl = slice(gi * 64, (gi + 1) * 64)
                                    kb.op("pe", mm(ps_.t[:, osl], Kt.t[:, kvh, tk:tk + 128], Q.t[:, h, qs], True, dr is None), [Kt, Q], [ps_])
                                    if dr is not None:
                                        kb.op("pe", mm(ps_.t[:, osl], Tc.t[:, h, dr + 7:dr + 9, :].rearrange("p a b -> p (a b)"), id64b, False, True), [Tc, cb], [ps_])
                                pendu.append((ps_, groups, qs))
                                if rl == min(2, n // 64 - 1) and pnorm["f"] is not None:
                                    pnorm["f"](); pnorm["f"] = None
                                if len(pendu) > 2:
                                    finish_unit(pendu.pop(0))
                            while pendu:
                                finish_unit(pendu.pop(0))
                        kb.op("dve", lambda e, po=po: e.reciprocal(out=Rt.t[64:65, 0:n], in_=po.t[64:65, 0:n]), [po], [Rt])

                        def rest(po=po, h=h, t0=t0, n=n):
                            pb_ = kb.psum(PS_B)
                            kb.op("pe", mm(pb_.t[:, 0:n], sel64, Rt.t[0:65, 0:n]), [cf, Rt], [pb_])
                            bc = bcs[c_["b"] % 2]; c_["b"] += 1
                            kb.op("dve", lambda e: e.tensor_copy(out=bc.t[:, 0:n], in_=pb_.t[0:64, 0:n]), [pb_], [bc])
                            y_ = ys[c_["y"] % 3]; c_["y"] += 1
                            kb.op("dve", lambda e: e.tensor_tensor(out=y_.t[:, 0:n], in0=po.t[0:64, 0:n], in1=bc.t[:, 0:n], op=ALU.mult), [po, bc], [y_])
                            kb.dma("pool", YT[yrow0 + h * 64:yrow0 + (h + 1) * 64, t0:t0 + n], y_.t[:, 0:n], [y_], [dQK["YT"]], y_)
                            conv.step()
                        pnorm["f"] = rest
                if pnorm["f"] is not None:
                    pnorm["f"](); pnorm["f"] = None
                conv.drain(everything=(moe and name == "m"))
                kb.barrier()

        if l + 1 < L and (l + 1) % 2 == 1:
            conv.add_layer((l + 1) // 2)
        elif l == 0 and moe:
            conv.add_layer(0)
        attn_phase("na", 64, 4, 4, KnaT, QnaT, Vna, 0.125, 0, na=True)
        if stop == "P2a":
            return nc
        attn_phase("g", 64, 8, 2, KgT, QgT, Vg, 0.125, 256)
        attn_phase("m", 96, 4, 4, KmT, QmT, Vm, 96 ** -0.5, 768)

        if stop == "P2":
            return nc
        with contextlib.ExitStack() as st:
            stg = [kb.tile(st, [128, 2048], F32, "w3s") for _ in range(2)]
            wo_b = kb.tile(st, [128, 8, D], BF16, "wo")
            wout_b = kb.tile(st, [128, KD, D], BF16, "wout")
            i = 0
            for src_, dst_ in ((wo[l], wo_b), (wout[l], wout_b)):
                for k in range(0, 8, 2):
                    sg = stg[i % 2]; i += 1
                    kb.dma("sp", sg.t[:, :], src_[:, k * D:(k + 2) * D], [dummy], [sg], sg)
                    cast(dst_.t[:, k:k + 2, :], dst_, sg.t[:, :].rearrange("p (a b) -> p a b", a=2), sg)
            Ys = [kb.tile(st, [128, 8, 512], BF16, "Y") for _ in range(2)]
            Gs = [kb.tile(st, [128, 24, 512], BF16, "G") for _ in range(2)]
            Xs = [kb.tile(st, [128, KD, 512], F32, "X") for _ in range(2)]
            mT = [kb.tile(st, [128, KD, 512], BF16, "m") for _ in range(2)]
            tfs = [kb.tile(st, [128, 512], F32, "t3") for _ in range(4)]
            tc = 0
            clist = list(range(len(cfg.chunks)))
            if last:
                clist = clist[1:]
            for qi, ci in enumerate(clist):
                t0, n = cfg.chunks[ci]
                s_ = 1 if ci == 0 else 0
                Y = Ys[qi % 2]; G = Gs[qi % 2]; X = Xs[qi % 2]; m_ = mT[qi % 2]
                kb.dma("sp", Y.t[:, :, 0:n], YT[:, t0:t0 + n].rearrange("(k p) t -> p k t", p=128), [dQK["YT"]], [Y], Y)
                kb.dma("sp", G.t[:, :, 0:n], GT[:, t0:t0 + n].rearrange("(k p) t -> p k t", p=128), [dQK["GT"]], [G], G)
                kb.dma("sp", X.t[:, :, 0:n], xsrc[:, t0:t0 + n].rearrange("(k p) t -> p k t", p=128), [dXT[ci]], [X], X)
                for j in range(KD):
                    js = slice(j * 128, (j + 1) * 128)
                    pa = kb.psum(); pb = kb.psum(); pc = kb.psum()
                    for k in range(2):
                        kb.op("pe", mm(pa.t[:, 0:n], wo_b.t[:, k, js], Y.t[:, k, 0:n], k == 0, k == 1), [wo_b, Y], [pa])
                    for k in range(4):
                        kb.op("pe", mm(pb.t[:, 0:n], wo_b.t[:, 2 + k, js], Y.t[:, 2 + k, 0:n], k == 0, k == 3), [wo_b, Y], [pb])
                    for k in range(2):
                        kb.op("pe", mm(pc.t[:, 0:n], wo_b.t[:, 6 + k, js], Y.t[:, 6 + k, 0:n], k == 0, k == 1), [wo_b, Y], [pc])
                    t1 = tfs[tc % 4]; t2 = tfs[(tc + 1) % 4]; tc += 2
                    kb.op("dve", lambda e, t1=t1, pa=pa, G=G, j=j: e.tensor_tensor(out=t1.t[:, 0:n], in0=pa.t[:, 0:n], in1=G.t[:, j, 0:n], op=ALU.mult), [pa, G], [t1])
                    kb.op("dve", lambda e, t2=t2, pb=pb, G=G, j=j: e.tensor_tensor(out=t2.t[:, 0:n], in0=pb.t[:, 0:n], in1=G.t[:, 8 + j, 0:n], op=ALU.mult), [pb, G], [t2])
                    kb.op("pool", lambda e, t1=t1, t2=t2: e.tensor_tensor(out=t1.t[:, 0:n], in0=t1.t[:, 0:n], in1=t2.t[:, 0:n], op=ALU.add), [t2], [t1])
                    kb.op("dve", lambda e, t2=t2, pc=pc, G=G, j=j: e.tensor_tensor(out=t2.t[:, 0:n], in0=pc.t[:, 0:n], in1=G.t[:, 16 + j, 0:n], op=ALU.mult), [pc, G], [t2])
                    kb.op("pool", lambda e, t1=t1, t2=t2, m_=m_, j=j: e.tensor_tensor(out=m_.t[:, j, 0:n], in0=t1.t[:, 0:n], in1=t2.t[:, 0:n], op=ALU.add), [t1, t2], [m_])
                for j in range(KD):
                    js = slice(j * 128, (j + 1) * 128)
                    p = kb.psum()
                    for k in range(KD):
                        kb.op("pe", mm(p.t[:, 0:n], wout_b.t[:, k, js], m_.t[:, k, 0:n], k == 0, k == KD - 1), [wout_b, m_], [p])
                    tr_ = tfs[tc % 4]; tc += 1
                    kb.op("act", lambda e, p=p, j=j, s_=s_, tr_=tr_: e.activation(out=tr_.t[:, 0:n], in_=p.t[:, 0:n], func=AF.Identity, scale=mod(l, 2, j, s_)), [p, modv], [tr_])
                    kb.op("dve", lambda e, X=X, j=j, tr_=tr_: e.tensor_tensor(out=X.t[:, j, 0:n], in0=X.t[:, j, 0:n], in1=tr_.t[:, 0:n], op=ALU.add), [tr_], [X])
                kb.dma("pool", XT[:, t0:t0 + n].rearrange("(k p) t -> p k t", p=128), X.t[:, :, 0:n], [X], [dXT[ci]], X)
            kb.barrier()

        if stop == "P3":
            return nc
        if moe and not os.environ.get("DENSE_MOE"):
            routed_moe(l, last)
            continue
        clist = list(range(len(cfg.chunks)))
        if last:
            clist = clist[1:]
        groups = [clist[i:i + 2] for i in range(0, len(clist), 2)]
        with contextlib.ExitStack() as st:
            h2 = kb.tile(st, [128, KD, 1024], BF16, "h2")
            if moe:
                acc = kb.tile(st, [128, KD, 1024], F32, "acc")
                gwbc = kb.tile(st, [128, E, 1024], BF16, "gwbc")
                wr_t = kb.tile(st, [128, KD, 8], F32, "wr")
                gwT = kb.tile(st, [8, 1024], F32, "gwT")
                sm = [kb.tile(st, [128, 8], F32, "sm%d" % i) for i in range(6)]
                s1 = [kb.tile(st, [128, 1], F32, "s1%d" % i) for i in range(5)]
                kb.dma("sp", wr_t.t[:, :, :], wr[l // 2].rearrange("p (k e) -> p k e", e=8), [dummy], [wr_t], wr_t)
            wc = 0
            wcs = [0]
            xc = 0
            for grp in groups:
                cols = []
                c0 = 0
                with contextlib.ExitStack() as st2:
                    nt4 = norm_tiles(st2)
                    h2f = kb.tile(st2, [128, KD, 512], F32, "h2f") if moe else None
                    for ci in grp:
                        t0, n = cfg.chunks[ci]
                        cols.append((ci, t0, n, c0))
                        norm_chunk(nt4, XT, l, ci, V_NFFN, 3, 4, h2, c0, out_f32=h2f)
                        if moe:
                            for tt in range(n // 128):
                                p = kb.psum()
                                for k in range(KD):
                                    kb.op("pe", mm(p.t[:, 0:8], h2f.t[:, k, tt * 128:(tt + 1) * 128], wr_t.t[:, k, :], k == 0, k == KD - 1), [h2f, wr_t], [p])
                                Lg, m1e, L2, selm, ex, gw = sm
                                m1, m2, nm1, ss, rs1 = s1
                                kb.op("dve", lambda e: e.tensor_copy(out=Lg.t[:, :], in_=p.t[:, 0:8]), [p], [Lg])
                                kb.op("dve", lambda e: e.reduce_max(out=m1.t[:, :], in_=Lg.t[:, :], axis=AX.X), [Lg], [m1])
                                kb.op("dve", lambda e: e.tensor_scalar(out=m1e.t[:, :], in0=Lg.t[:, :], scalar1=m1.t[:, 0:1], scalar2=-1e30, op0=ALU.is_equal, op1=ALU.mult), [Lg, m1], [m1e])
                                kb.op("dve", lambda e: e.tensor_tensor(out=L2.t[:, :], in0=Lg.t[:, :], in1=m1e.t[:, :], op=ALU.add), [Lg, m1e], [L2])
                                kb.op("dve", lambda e: e.reduce_max(out=m2.t[:, :], in_=L2.t[:, :], axis=AX.X), [L2], [m2])
                                kb.op("dve", lambda e: e.tensor_scalar(out=selm.t[:, :], in0=Lg.t[:, :], scalar1=m2.t[:, 0:1], scalar2=None, op0=ALU.is_ge), [Lg, m2], [selm])
                                kb.op("dve", lambda e: e.tensor_scalar(out=nm1.t[:, :], in0=m1.t[:, :], scalar1=-1.0, scalar2=None, op0=ALU.mult), [m1], [nm1])
                                kb.op("act", lambda e: e.activation(out=ex.t[:, :], in_=Lg.t[:, :], func=AF.Exp, bias=nm1.t[:, 0:1], scale=1.0), [Lg, nm1], [ex])
                                kb.op("dve", lambda e: e.tensor_tensor(out=ex.t[:, :], in0=ex.t[:, :], in1=selm.t[:, :], op=ALU.mult), [selm], [ex])
                                kb.op("dve", lambda e: e.reduce_sum(out=ss.t[:, :], in_=ex.t[:, :], axis=AX.X), [ex], [ss])
                                kb.op("dve", lambda e: e.reciprocal(out=rs1.t[:, :], in_=ss.t[:, :]), [ss], [rs1])
                                kb.op("dve", lambda e: e.tensor_scalar(out=gw.t[:, :], in0=ex.t[:, :], scalar1=rs1.t[:, 0:1], scalar2=None, op0=ALU.mult), [ex, rs1], [gw])
                                p2 = kb.psum()
                                kb.op("pe", mm(p2.t[0:8, 0:128], gw.t[:, :], id128f), [gw, cf], [p2])
                                kb.op("dve", lambda e: e.tensor_copy(out=gwT.t[:, c0 + tt * 128:c0 + (tt + 1) * 128], in_=p2.t[0:8, 0:128]), [p2], [gwT])
                            for e_ in range(E):
                                p = kb.psum()
                                kb.op("pe", mm(p.t[:, 0:n], sel_e(e_), gwT.t[:, c0:c0 + n]), [cf, gwT], [p])
                                evac(gwbc.t[:, e_, c0:c0 + n], gwbc, p, p.t[:, 0:n])
                        c0 += n
                    kb.barrier()
                with contextlib.ExitStack() as st2:
                    act = kb.tile(st2, [128, KF, 1024], BF16, "act")
                    gus = [kb.tile(st2, [128, KD * 256], F32, "gus") for _ in range(3)]
                    gub = [kb.tile(st2, [128, KD, 256], BF16, "gub") for _ in range(3)]
                    dns = [kb.tile(st2, [128, KF * 128], F32, "dns") for _ in range(3)]
                    dnb = [kb.tile(st2, [128, KF, 128], BF16, "dnb") for _ in range(3)]
                    xts = [kb.tile(st2, [128, 512], F32, "x4") for _ in range(3)]
                    sl = [kb.tile(st2, [128, 512], BF16, "silu") for _ in range(3)]
                    t5 = [kb.tile(st2, [128, 512], BF16, "t5") for _ in range(2)]
                    for e_ in range(E if moe else 1):
                        gsrc = wgu[l // 2]
                        dsrc = wdn[l // 2]
                        def issue_gu(f):
                            nonlocal_wc = wcs[0]; wcs[0] += 1
                            sg = gus[nonlocal_wc % 3]; gb = gub[nonlocal_wc % 3]
                            kb.dma("sp", sg.t[:, :], gsrc[f], [dummy], [sg], sg)
                            cast(gb.t[:, :, :], gb, sg.t[:, :].rearrange("p (k c) -> p k c", k=KD), sg)
                            return gb

                        def issue_dn(j):
                            nonlocal_wc = wcs[0]; wcs[0] += 1
                            sg = dns[nonlocal_wc % 3]; db = dnb[nonlocal_wc % 3]
                            kb.dma("sp", sg.t[:, :], dsrc[j], [dummy], [sg], sg)
                            cast(db.t[:, :, :], db, sg.t[:, :].rearrange("p (k c) -> p k c", k=KF), sg)
                            return db
                        gb_next = issue_gu(0)
                        for f in range(KF):
                            gb = gb_next
                            gb_next = issue_gu(f + 1) if f + 1 < KF else None
                            if f + 1 == KF:
                                db_next = issue_dn(0)
                            for (ci, t0, n, c0) in cols:
                                pg = kb.psum(); pu = kb.psum()
                                for k in range(KD):
                                    kb.op("pe", mm(pg.t[:, 0:n], gb.t[:, k, 0:128], h2.t[:, k, c0:c0 + n], k == 0, k == KD - 1), [gb, h2], [pg])
                                for k in range(KD):
                                    kb.op("pe", mm(pu.t[:, 0:n], gb.t[:, k, 128:256], h2.t[:, k, c0:c0 + n], k == 0, k == KD - 1), [gb, h2], [pu])
                                s_ = sl[xc % 3]; xc += 1
                                kb.op("act", lambda e: e.activation(out=s_.t[:, 0:n], in_=pg.t[:, 0:n], func=AF.Silu), [pg], [s_])
                                if moe:
                                    t_ = t5[xc % 2]
                                    kb.op("dve", lambda e: e.tensor_tensor(out=t_.t[:, 0:n], in0=pu.t[:, 0:n], in1=s_.t[:, 0:n], op=ALU.mult), [pu, s_], [t_])
                                    kb.op("pool", lambda e: e.tensor_tensor(out=act.t[:, f, c0:c0 + n], in0=t_.t[:, 0:n], in1=gwbc.t[:, e_, c0:c0 + n], op=ALU.mult), [t_, gwbc], [act])
                                else:
                                    kb.op("dve", lambda e: e.tensor_tensor(out=act.t[:, f, c0:c0 + n], in0=pu.t[:, 0:n], in1=s_.t[:, 0:n], op=ALU.mult), [pu, s_], [act])
                        for j in range(KD):
                            db = db_next
                            db_next = issue_dn(j + 1) if j + 1 < KD else None
                            for (ci, t0, n, c0) in cols:
                                s_i = 1 if ci == 0 else 0
                                p = kb.psum()
                                for k in range(KF):
                                    kb.op("pe", mm(p.t[:, 0:n], db.t[:, k, :], act.t[:, k, c0:c0 + n], k == 0, k == KF - 1), [db, act], [p])
                                fin = (not moe) or e_ == E - 1
                                if moe and e_ == 0:
                                    evac(acc.t[:, j, c0:c0 + n], acc, p, p.t[:, 0:n])
                                elif moe:
                                    kb.op("dve", lambda e: e.tensor_tensor(out=acc.t[:, j, c0:c0 + n], in0=p.t[:, 0:n], in1=acc.t[:, j, c0:c0 + n], op=ALU.add), [p], [acc])
                                if fin:
                                    x_ = xts[xc % 3]; xc += 1
                                    js = slice(j * 128, (j + 1) * 128)
                                    kb.dma("sp", x_.t[:, 0:n], XT[js, t0:t0 + n], [dummy], [x_], x_)
                                    if moe:
                                        kb.op("dve", lambda e: e.scalar_tensor_tensor(out=x_.t[:, 0:n], in0=acc.t[:, j, c0:c0 + n], scalar=mod(l, 5, j, s_i), in1=x_.t[:, 0:n], op0=ALU.mult, op1=ALU.add), [acc, modv], [x_])
                                    else:
                                        tq_ = t5[xc % 2]
                                        tq32 = xts[(xc + 1) % 3]
                                        kb.op("act", lambda e: e.activation(out=tq32.t[:, 0:n], in_=p.t[:, 0:n], func=AF.Identity, scale=mod(l, 5, j, s_i)), [p, modv], [tq32])
                                        kb.op("dve", lambda e: e.tensor_tensor(out=x_.t[:, 0:n], in0=x_.t[:, 0:n], in1=tq32.t[:, 0:n], op=ALU.add), [tq32], [x_])
                                    kb.dma("pool", XT[js, t0:t0 + n], x_.t[:, 0:n], [x_], [Buf("snk")], x_)
                    kb.barrier()

    with contextlib.ExitStack() as st:
        xt = [kb.tile(st, [128, KD, 512], F32, "xf") for _ in range(2)]
        sq = [kb.tile(st, [128, KD, 512], BF16, "sqf") for _ in range(2)]
        rs = [kb.tile(st, [128, 512], F32, "rsf") for _ in range(2)]
        for qi, ci in enumerate(range(1, len(cfg.chunks))):
            t0, n = cfg.chunks[ci]
            x_ = xt[qi % 2]; s_ = sq[qi % 2]; r_ = rs[qi % 2]
            kb.dma("sp", x_.t[:, :, 0:n], XT[:, t0:t0 + n].rearrange("(k p) t -> p k t", p=128), [dXT[ci]], [x_], x_)
            p = kb.psum()
            for k in range(KD):
                kb.op("act", lambda e, k=k, x_=x_, s_=s_: e.activation(out=s_.t[:, k, 0:n], in_=x_.t[:, k, 0:n], func=AF.Square), [x_], [s_])
                kb.op("pe", mm(p.t[:, 0:n], ones128, s_.t[:, k, 0:n], k == 0, k == KD - 1), [s_, cb], [p])
            rstd_from(p, n, D, r_)
            for k in range(KD):
                kb.op("dve", lambda e, k=k, x_=x_, r_=r_: e.scalar_tensor_tensor(out=x_.t[:, k, 0:n], in0=x_.t[:, k, 0:n], scalar=v_nfinal[:, k:k + 1], in1=r_.t[:, 0:n], op0=ALU.mult, op1=ALU.mult), [r_, vc], [x_])
            kb.dma("pool", outT[:, t0 - C:t0 - C + n].rearrange("(k p) t -> p k t", p=128), x_.t[:, :, 0:n], [x_], [dXT[ci]], x_)
        kb.barrier()
    return nc


def _tile_cols(w, KD):
    D = w.shape[0]
    return np.ascontiguousarray(w.reshape(KD, 128, w.shape[1]).transpose(1, 0, 2).reshape(128, -1))


def host_prep(cfg, inp):
    D, C, S, L, F, E, T, KD, KF = cfg.D, cfg.C, cfg.S, cfg.L, cfg.F, cfg.E, cfg.T, cfg.KD, cfg.KF
    f32 = np.float32
    sh = {}
    NVL = 2 * KD + 48 + 6 + 3

    def fm(v):
        return np.asarray(v, f32).reshape(-1, 128).T

    def rot64(g):
        return np.concatenate([g[32:], g[:32]])
    w_in = np.asarray(inp["w_in"], f32)
    KVW = 928
    o_kna, o_vna, o_kg, o_vg, o_ckv, o_kr = 0, 256, 512, 640, 768, 896
    o_qna, o_qg, o_cq, o_gate = KVW, KVW + 256, KVW + 768, KVW + 1024

    def rotcols(base, nheads, d):
        idx = []
        for h in range(nheads):
            idx += list(range(base + h * d + d // 2, base + (h + 1) * d)) + list(range(base + h * d, base + h * d + d // 2))
        return idx
    w1 = np.zeros((L, cfg.NT1, 128, KD * 128), f32)
    wvv = np.zeros((L, 128, KD * 384), f32)
    for l in range(L):
        W = w_in[l]
        tiles = []
        tiles += [W[:, o_kna:o_kna + 128], W[:, o_kna + 128:o_kna + 256]]
        tiles += [W[:, o_kg:o_kg + 128], W[:, rotcols(o_kg, 2, 64)]]
        tiles += [W[:, o_ckv:o_ckv + 128]]
        t5 = np.zeros((D, 128), f32); t5[:, 64:96] = W[:, o_kr:o_kr + 32]
        t6 = np.zeros((D, 128), f32); t6[:, 64:96] = W[:, rotcols(o_kr, 1, 32)]
        tiles += [t5, t6]
        tiles += [W[:, o_qna:o_qna + 128], W[:, o_qna + 128:o_qna + 256]]
        tiles += [W[:, o_qg + j * 128:o_qg + (j + 1) * 128] for j in range(4)]
        rc = rotcols(o_qg, 8, 64)
        tiles += [W[:, rc[j * 128:(j + 1) * 128]] for j in range(4)]
        tiles += [W[:, o_cq:o_cq + 128], W[:, o_cq + 128:o_cq + 256]]
        tiles += [W[:, o_gate + j * 128:o_gate + (j + 1) * 128] for j in range(24)]
        for i, t in enumerate(tiles):
            w1[l, i] = _tile_cols(t, KD)
        Wv = np.concatenate([W[:, o_vna:o_vna + 256], W[:, o_vg:o_vg + 128]], axis=1)
        wvv[l] = _tile_cols(Wv, KD)
    sh["w1"] = w1
    sh["wv"] = wvv
    w_ada = np.asarray(inp["w_ada"], f32)
    sh["w_ada"] = np.ascontiguousarray(w_ada.reshape(L, KD, 128, 48, 128).transpose(0, 3, 2, 1, 4).reshape(L, 48, 128, KD * 128))
    w_uq = np.asarray(inp["w_uq"], f32)
    sh["wuq"] = np.ascontiguousarray(w_uq.reshape(L, 2, 128, 384).transpose(0, 2, 1, 3).reshape(L, 128, 768))
    wuqr = np.zeros_like(w_uq)
    for h in range(4):
        b = h * 96 + 64
        wuqr[:, :, b:b + 16] = w_uq[:, :, b + 16:b + 32]
        wuqr[:, :, b + 16:b + 32] = w_uq[:, :, b:b + 16]
    sh["wuqr"] = np.ascontiguousarray(wuqr.reshape(L, 2, 128, 384).transpose(0, 2, 1, 3).reshape(L, 128, 768))
    w_ukv = np.asarray(inp["w_ukv"], f32).reshape(L, 128, 4, 128)
    sh["wukvk"] = np.ascontiguousarray(w_ukv[:, :, :, :64].reshape(L, 128, 256))
    sh["wukvv"] = np.ascontiguousarray(w_ukv[:, :, :, 64:].reshape(L, 128, 256))
    rpb = np.asarray(inp["rpb"], f32)
    jq = np.arange(64)[:, None]; jk = np.arange(64)[None, :]
    dc = np.clip(jk - jq, -15, 15) + 15
    g = rpb[:, :, :, dc]
    sh["rpbT"] = np.ascontiguousarray(g.transpose(0, 3, 1, 2, 4).reshape(L, 64, 3840))
    wo_all = np.concatenate([np.asarray(inp["w_o_na"], f32), np.asarray(inp["w_o_gqa"], f32), np.asarray(inp["w_o_mla"], f32)], axis=1)
    sh["wo"] = np.ascontiguousarray(wo_all.reshape(L, 8, 128, D).transpose(0, 2, 1, 3).reshape(L, 128, 8 * D))
    sh["wout"] = np.ascontiguousarray(np.asarray(inp["w_out"], f32).reshape(L, KD, 128, D).transpose(0, 2, 1, 3).reshape(L, 128, KD * D))

    def gu_layout(w):
        lead = w.shape[:-2]
        gte = w[..., :F].reshape(*lead, KD, 128, KF, 128)
        up = w[..., F:].reshape(*lead, KD, 128, KF, 128)
        cat = np.stack([gte, up], axis=-2)
        nl = len(lead)
        perm = list(range(nl)) + [nl + 2, nl + 1, nl + 0, nl + 3, nl + 4]
        return np.ascontiguousarray(cat.transpose(perm).reshape(*lead, KF, 128, KD * 256))

    def dn_layout(w):
        lead = w.shape[:-2]
        nl = len(lead)
        a = w.reshape(*lead, KF, 128, KD, 128)
        perm = list(range(nl)) + [nl + 2, nl + 1, nl + 0, nl + 3]
        return np.ascontiguousarray(a.transpose(perm).reshape(*lead, KD, 128, KF * 128))
    sh["wgu"] = gu_layout(np.asarray(inp["w_ffn_gu"], f32))
    sh["wdn"] = dn_layout(np.asarray(inp["w_ffn_dn"], f32))
    if cfg.NM > 0:
        sh["wgum"] = gu_layout(np.asarray(inp["w_moe_gu"], f32)).reshape(cfg.NM * E * KF * 128, KD * 256)
        sh["wdnm"] = np.ascontiguousarray(np.asarray(inp["w_moe_dn"], f32).reshape(cfg.NM * E * F, D))
        sh["wr"] = np.ascontiguousarray(np.asarray(inp["w_router"], f32).reshape(cfg.NM, KD, 128, 8).transpose(0, 2, 1, 3).reshape(cfg.NM, 128, KD * 8))
    else:
        sh["wgum"] = np.zeros((E * KF * 128, KD * 256), f32)
        sh["wdnm"] = np.zeros((E * F, D), f32)
        sh["wr"] = np.zeros((1, 128, KD * 8), f32)
    pos = np.arange(S)
    rows = (pos // 64).astype(f32); cols = (pos % 64).astype(f32)

    def tables(half):
        nf = half // 2
        inv = np.power(10000.0, -np.arange(nf, dtype=f32) / nf).astype(f32)
        ang = np.concatenate([rows[:, None] * inv, cols[:, None] * inv], axis=-1)
        cos = np.concatenate([np.ones((C, half), f32), np.cos(ang)], 0).T
        sin = np.concatenate([np.zeros((C, half), f32), np.sin(ang)], 0).T
        return cos, sin
    cg, sg = tables(32)
    cosg = np.concatenate([cg, cg, cg, cg], 0)
    sing = np.concatenate([-sg, sg, -sg, sg], 0)
    cm, sm_ = tables(16)
    cosm = np.zeros((128, T), f32); sinm = np.zeros((128, T), f32)
    cosm[64:96] = np.concatenate([cm, cm], 0)
    sinm[64:96] = np.concatenate([-sm_, sm_], 0)
    col0 = np.clip(np.arange(64) - 8, 0, 48)
    inwin = (jk >= col0[:, None]) & (jk < col0[:, None] + 16)
    neg = np.where(inwin, 0.0, -30000.0).astype(f32)
    negm = np.tile(neg[:, None, :], (1, 60, 1)).reshape(64, 3840)
    ones = np.ones((128, 128), f32)
    bd = np.zeros((128, 128), f32); bd[:64, :64] = 1; bd[64:, 64:] = 1
    idb = np.zeros((128, 64), f32); idb[:64] = np.eye(64)
    ustr = np.triu(np.ones((128, 128), f32), 1)
    sh["cbf"] = np.ascontiguousarray(np.concatenate([ones, bd, idb, ustr, np.eye(128, dtype=f32)], 1).astype(BF))
    sh["ropet"] = np.ascontiguousarray(np.stack([cosg, sing, cosm, sinm], 1).astype(BF))
    sh["negm"] = np.ascontiguousarray(negm.astype(BF))
    sel64 = np.zeros((128, 128), f32); sel64[64, :64] = 1
    sele = np.zeros((128, E, 128), f32)
    for e in range(E):
        sele[e, e, :] = 1
    sh["cf32"] = np.ascontiguousarray(np.concatenate([np.eye(128, dtype=f32), sel64, sele.reshape(128, E * 128), np.full((128, 1), 1e-6, f32),
        np.tile((np.arange(10, dtype=f32) * 512)[None, :], (128, 1)), np.tile(np.arange((2 * T + E * 511) // 512, dtype=f32)[None, :], (128, 1)),
        (np.arange(KF, dtype=f32)[None, :] * 128 + np.arange(128, dtype=f32)[:, None])], 1))
    NV = L * NVL + 3 * KD
    per = []
    xin = np.asarray(inp["x"], f32); ctx = np.asarray(inp["ctx"], f32); c = np.asarray(inp["c"], f32)
    B = xin.shape[0]
    vbase = np.zeros((128, NV), f32)
    for l in range(L):
        o = l * NVL
        vbase[:, o:o + KD] = fm(inp["norm_mix"][l])
        vbase[:, o + KD:o + 2 * KD] = fm(inp["norm_ffn"][l])
        vbase[:, o + 2 * KD:o + 2 * KD + 48] = fm(inp["b_ada"][l])
        qg = np.asarray(inp["q_norm_gqa"][l], f32); kg = np.asarray(inp["k_norm_gqa"][l], f32)
        vbase[:, o + 2 * KD + 48] = np.concatenate([qg, qg])
        vbase[:, o + 2 * KD + 49] = np.concatenate([rot64(qg), rot64(qg)])
        vbase[:, o + 2 * KD + 50] = np.concatenate([kg, kg])
        vbase[:, o + 2 * KD + 51] = np.concatenate([rot64(kg), rot64(kg)])
        vbase[:, o + 2 * KD + 52:o + 2 * KD + 54] = fm(inp["q_lora_norm"][l])
        vbase[:, o + 2 * KD + 54] = np.asarray(inp["kv_lora_norm"][l], f32)
    go = L * NVL
    vbase[:, go:go + KD] = fm(inp["norm_final"])
    vbase[:, go + 2 * KD:go + 3 * KD] = fm(inp["c_ctx"])
    for b in range(B):
        m = dict(sh)
        v = vbase.copy()
        v[:, go + KD:go + 2 * KD] = fm(c[b])
        m["vecs"] = v
        m["xT"] = np.ascontiguousarray(np.concatenate([ctx[b], xin[b]], 0).T)
        per.append(m)
    return per


_CACHE = {}


def kernel(**inputs):
    cfg = Cfg()
    if "nc" not in _CACHE:
        _CACHE["nc"] = build(cfg)
    nc = _CACHE["nc"]
    in_maps = host_prep(cfg, inputs)
    res = run_bass_kernel_spmd(nc, in_maps, core_ids=list(range(len(in_maps))))
    out = np.stack([np.ascontiguousarray(r["outT"].T) for r in res.results], 0)
    return out.astype(np.float32)
```
